# Optimizing a Trainium2 kernel written in Bass

```python
import jax
import jax.numpy as jnp
from jax import lax
import numpy as np

D_MODEL = 1024
BATCH = 4
SEQ = 4096
DEPTH = 4

HG_HEADS = 8
HG_DK = 128
HG_DV = 128
HG_WIDTH = HG_HEADS * HG_DK
HG_VWIDTH = HG_HEADS * HG_DV
HG_CHUNK = 64
NORM_EPS = 1e-5

RW_HEAD = 64
RW_HEADS = D_MODEL // RW_HEAD
RW_WIDTH = RW_HEADS * RW_HEAD
DECAY_LORA = 64
AAA_LORA = 64
MV_LORA = 32
GATE_LORA = 128
RW_LN_EPS = 64e-5
RW_SPLITS = (RW_WIDTH, RW_WIDTH, RW_WIDTH, DECAY_LORA, DECAY_LORA, AAA_LORA, GATE_LORA)
RW_COLS = 3 * RW_WIDTH + 2 * DECAY_LORA + AAA_LORA + GATE_LORA

IN_SPLITS = (HG_WIDTH, HG_WIDTH, HG_WIDTH, HG_VWIDTH, HG_VWIDTH, RW_COLS, D_MODEL, D_MODEL)
IN_COLS = 3 * HG_WIDTH + 2 * HG_VWIDTH + RW_COLS + 2 * D_MODEL

N_EXPERTS = 32
TOP_K = 4
D_EXPERT = D_MODEL
SWIGLU_ALPHA = 1.702
SWIGLU_LIMIT = 7.0
MOE_BLOCK = 256

DN_ALPHA = (2 * DEPTH) ** 0.25
DN_BETA = (8 * DEPTH) ** -0.25

kernel_name = 'hgrn2_rwkv7_gated_moe_deepnorm_encoder'


def _split(t, sizes):
    idx = np.cumsum(sizes)[:-1].tolist()
    return jnp.split(t, idx, axis=-1)


def _layernorm(x, g, b):
    xf = x.astype(jnp.float32)
    xc = xf - jnp.mean(xf, axis=-1, keepdims=True)
    var = jnp.mean(xc * xc, axis=-1, keepdims=True)
    return (xc * lax.rsqrt(var + NORM_EPS) * g + b).astype(x.dtype)


def _centred_shift(p, mu):
    pp = jnp.pad(p, ((0, 0), (1, 1), (0, 0)))
    nb = 0.5 * (pp[:, :-2] + pp[:, 2:])
    return p + mu * (nb - p)


def _gla_chunked(q, lf, k, v):
    n, s, dk = q.shape
    dv = v.shape[-1]
    c = HG_CHUNK
    nc = s // c

    def chunks(t):
        return t.reshape(n, nc, c, t.shape[-1]).transpose(1, 0, 2, 3)

    lower = jnp.tril(jnp.ones((c, c), dtype=bool))

    def step(state, inp):
        qc, lfc, kc, vc = inp
        b = jnp.cumsum(lfc, axis=1)
        inter = jnp.einsum('nck,nkv->ncv', qc * jnp.exp(b), state)
        diff = b[:, :, None, :] - b[:, None, :, :]
        dec = jnp.exp(jnp.where(lower[None, :, :, None], diff, -jnp.inf))
        scores = jnp.einsum('ntk,ntsk,nsk->nts', qc, dec, kc)
        intra = jnp.einsum('nts,nsv->ntv', scores, vc)
        b_last = b[:, -1]
        new_state = jnp.exp(b_last)[:, :, None] * state + jnp.einsum(
            'nsk,nsv->nkv', kc * jnp.exp(b_last[:, None, :] - b), vc)
        return new_state, inter + intra

    s0 = jnp.zeros((n, dk, dv), jnp.float32)
    _, o = lax.scan(step, s0, (chunks(q), chunks(lf), chunks(k), chunks(v)))
    return o.transpose(1, 0, 2, 3).reshape(n, s, dv)


def _hgrn2_branch(q, f_fwd, f_bwd, i, og, lb, norm_w):
    bsz, s, _ = q.shape

    def heads(t):
        return t.astype(jnp.float32).reshape(bsz, s, HG_HEADS, -1).transpose(0, 2, 1, 3)

    def forget(z):
        f = lb + (1.0 - lb) * jax.nn.sigmoid(z.astype(jnp.float32))
        return heads(jnp.log(f)), heads(1.0 - f)

    lf_f, k_f = forget(f_fwd)
    lf_b, k_b = forget(f_bwd)
    qh, vh = heads(q), heads(i)

    def flip(t):
        return jnp.flip(t, axis=2)

    def both(fw, bw):
        return jnp.stack([fw, flip(bw)]).reshape(2 * bsz * HG_HEADS, s, -1)

    o = _gla_chunked(both(qh, qh), both(lf_f, lf_b), both(k_f, k_b), both(vh, vh))
    o = o.reshape(2, bsz, HG_HEADS, s, HG_DV)
    o = o[0] + flip(o[1])
    o = o * lax.rsqrt(jnp.mean(o * o, axis=-1, keepdims=True) + NORM_EPS) * norm_w
    o = o.transpose(0, 2, 1, 3).reshape(bsz, s, HG_VWIDTH)
    return (o * jax.nn.silu(og.astype(jnp.float32))).astype(q.dtype)


def _rwkv7_branch(prw, x, v_first, v_mix, mu, w0, w_up, a0, a_up, g_up, k_k, k_a, r_k,
                  lnx_w, lnx_b):
    bsz, s, _ = x.shape
    p = _centred_shift(prw.astype(jnp.float32), mu)
    r, k, v, wl_f, wl_b, a_lo, g_lo = _split(p, RW_SPLITS)
    decays = []
    for d, wl in enumerate((wl_f, wl_b)):
        w = -jax.nn.softplus(-(w0[d] + jnp.tanh(wl) @ w_up[d])) - 0.5
        decays.append(jnp.exp(-jnp.exp(w)))
    a = jax.nn.sigmoid(a0 + a_lo @ a_up)
    g = jax.nn.sigmoid(g_lo) @ g_up
    v_raw = v
    if v_mix is not None:
        v_down, v_up, v0 = v_mix
        v = v + (v_first - v) * jax.nn.sigmoid(v0 + (x.astype(jnp.float32) @ v_down) @ v_up)

    def heads(t):
        return t.reshape(bsz, s, RW_HEADS, RW_HEAD)

    kk = heads(k * k_k)
    kk = kk / jnp.maximum(jnp.sqrt(jnp.sum(kk * kk, axis=-1, keepdims=True)), 1e-12)
    k = heads(k * (1.0 + (a - 1.0) * k_a))
    r, v, a = heads(r), heads(v), heads(a)

    def tm(fw, bw):
        return jnp.stack([fw, jnp.flip(bw, axis=1)], axis=0).transpose(2, 0, 1, 3, 4)

    xs = (tm(r, r), tm(heads(decays[0]), heads(decays[1])), tm(k, k), tm(v, v),
          tm(-kk, -kk), tm(kk * a, kk * a))

    def step(st, inp):
        r_t, w_t, k_t, v_t, a_t, b_t = inp
        sa = jnp.einsum('...vk,...k->...v', st, a_t)
        st = (st * w_t[..., None, :] + sa[..., :, None] * b_t[..., None, :]
              + v_t[..., :, None] * k_t[..., None, :])
        return st, jnp.einsum('...vk,...k->...v', st, r_t)

    s0 = jnp.zeros((2, bsz, RW_HEADS, RW_HEAD, RW_HEAD), jnp.float32)
    _, y = lax.scan(step, s0, xs)
    y = (y[:, 0] + jnp.flip(y[:, 1], axis=0)).transpose(1, 0, 2, 3)
    yc = y - jnp.mean(y, axis=-1, keepdims=True)
    yn = yc * lax.rsqrt(jnp.mean(yc * yc, axis=-1, keepdims=True) + RW_LN_EPS)
    yn = yn * lnx_w.reshape(RW_HEADS, RW_HEAD) + lnx_b.reshape(RW_HEADS, RW_HEAD)
    bonus = jnp.sum(r * k * r_k, axis=-1, keepdims=True) * v
    out = (yn + bonus).reshape(bsz, s, RW_WIDTH) * g
    return out.astype(x.dtype), v_raw


def _moe(x2, router_w, router_b, w1, b1, w2, b2):
    t, d = x2.shape
    n_assign = t * TOP_K
    n_blocks = -(-n_assign // MOE_BLOCK) + N_EXPERTS
    n_slots = n_blocks * MOE_BLOCK
    logits = (x2 @ router_w + router_b).astype(jnp.float32)
    top_val, top_idx = lax.top_k(logits, TOP_K)
    gates = jax.nn.softmax(top_val, axis=-1)
    e_flat = top_idx.reshape(-1).astype(jnp.int32)
    tok_flat = jnp.repeat(jnp.arange(t, dtype=jnp.int32), TOP_K)
    g_flat = gates.reshape(-1)
    order = jnp.argsort(e_flat)
    e_s, tok_s, g_s = e_flat[order], tok_flat[order], g_flat[order]
    counts = jnp.bincount(e_flat, length=N_EXPERTS)
    starts = jnp.cumsum(counts) - counts
    padded = (counts + MOE_BLOCK - 1) // MOE_BLOCK * MOE_BLOCK
    pad_ends = jnp.cumsum(padded)
    pad_starts = pad_ends - padded
    dest = pad_starts[e_s] + jnp.arange(n_assign, dtype=jnp.int32) - starts[e_s]
    slot_tok = jnp.zeros((n_slots,), jnp.int32).at[dest].set(tok_s)
    slot_gate = jnp.zeros((n_slots,), jnp.float32).at[dest].set(g_s)
    block_start = jnp.arange(n_blocks, dtype=jnp.int32) * MOE_BLOCK
    block_exp = jnp.minimum(jnp.searchsorted(pad_ends, block_start, side='right'), N_EXPERTS - 1)

    def expert_block(args):
        tok, e = args
        h = x2[tok] @ w1[e] + b1[e]
        glu = jnp.minimum(h[:, ::2], SWIGLU_LIMIT)
        lin = jnp.clip(h[:, 1::2], -SWIGLU_LIMIT, SWIGLU_LIMIT)
        act = glu * jax.nn.sigmoid(SWIGLU_ALPHA * glu) * (lin + 1.0)
        return act @ w2[e] + b2[e]

    y = lax.map(expert_block, (slot_tok.reshape(n_blocks, MOE_BLOCK), block_exp))
    y = y.reshape(n_slots, d).astype(jnp.float32) * slot_gate[:, None]
    return jnp.zeros((t, d), jnp.float32).at[slot_tok].add(y).astype(x2.dtype)


def setup_inputs(seed: int = 0) -> dict:
    key = jax.random.key(seed)
    ks = list(jax.random.split(key, 40))
    L, D = DEPTH, D_MODEL

    def nrm(i, shape, scale):
        return scale * jax.random.normal(ks[i], shape, jnp.float32)

    ramp = -6.5 + 5.0 * jnp.linspace(0.0, 1.0, RW_WIDTH, dtype=jnp.float32) ** 0.85
    return {
        'x': nrm(0, (BATCH, SEQ, D), 1.0),
        'w_in': nrm(1, (L, D, IN_COLS), D ** -0.5),
        'hg_lb_logits': nrm(2, (L, HG_WIDTH), 0.1),
        'hg_norm_w': 1.0 + nrm(3, (L, HG_DV), 0.05),
        'rw_mu': jax.random.uniform(ks[4], (L, RW_COLS), jnp.float32, 0.2, 0.8),
        'rw_w0': ramp + nrm(5, (L, 2, RW_WIDTH), 0.1),
        'rw_w_up': nrm(6, (L, 2, DECAY_LORA, RW_WIDTH), DECAY_LORA ** -0.5),
        'rw_a0': nrm(7, (L, RW_WIDTH), 0.1),
        'rw_a_up': nrm(8, (L, AAA_LORA, RW_WIDTH), AAA_LORA ** -0.5),
        'rw_g_up': nrm(9, (L, GATE_LORA, RW_WIDTH), GATE_LORA ** -0.5),
        'rw_k_k': 0.85 + nrm(10, (L, RW_WIDTH), 0.05),
        'rw_k_a': 1.0 + nrm(11, (L, RW_WIDTH), 0.05),
        'rw_r_k': nrm(12, (L, RW_HEADS, RW_HEAD), 0.1),
        'rw_lnx_w': 1.0 + nrm(13, (L, RW_WIDTH), 0.05),
        'rw_lnx_b': nrm(14, (L, RW_WIDTH), 0.01),
        'rw_v_down': nrm(15, (L - 1, D, MV_LORA), D ** -0.5),
        'rw_v_up': nrm(16, (L - 1, MV_LORA, RW_WIDTH), MV_LORA ** -0.5),
        'rw_v0': nrm(17, (L - 1, RW_WIDTH), 0.1),
        'proj_a': nrm(18, (L, HG_VWIDTH, D), HG_VWIDTH ** -0.5),
        'proj_b': nrm(19, (L, RW_WIDTH, D), RW_WIDTH ** -0.5),
        'w_out': nrm(20, (L, D, D), DN_BETA * D ** -0.5),
        'ln1_g': 1.0 + nrm(21, (L, D), 0.05),
        'ln1_b': nrm(22, (L, D), 0.01),
        'router_w': nrm(23, (L, D, N_EXPERTS), D ** -0.5),
        'router_b': nrm(24, (L, N_EXPERTS), 0.01),
        'moe_w1': nrm(25, (L, N_EXPERTS, D, 2 * D_EXPERT), D ** -0.5),
        'moe_b1': nrm(26, (L, N_EXPERTS, 2 * D_EXPERT), 0.01),
        'moe_w2': nrm(27, (L, N_EXPERTS, D_EXPERT, D), DN_BETA * D_EXPERT ** -0.5),
        'moe_b2': nrm(28, (L, N_EXPERTS, D), 0.01),
        'ln2_g': 1.0 + nrm(29, (L, D), 0.05),
        'ln2_b': nrm(30, (L, D), 0.01),
    }


def reference(x, w_in, hg_lb_logits, hg_norm_w, rw_mu, rw_w0, rw_w_up, rw_a0, rw_a_up,
              rw_g_up, rw_k_k, rw_k_a, rw_r_k, rw_lnx_w, rw_lnx_b, rw_v_down, rw_v_up, rw_v0,
              proj_a, proj_b, w_out, ln1_g, ln1_b, router_w, router_b, moe_w1, moe_b1,
              moe_w2, moe_b2, ln2_g, ln2_b):
    bsz, s, d = x.shape
    lb_all = jnp.cumsum(jax.nn.softmax(hg_lb_logits.astype(jnp.float32), axis=0), axis=0)
    lb_all = lb_all - lb_all[0:1]
    v_first = None
    for l in range(DEPTH):
        p = x @ w_in[l]
        hq, hf_f, hf_b, hi, hog, prw, gate_a, gate_b = _split(p, IN_SPLITS)
        o_a = _hgrn2_branch(hq, hf_f, hf_b, hi, hog, lb_all[l], hg_norm_w[l])
        v_mix = None if l == 0 else (rw_v_down[l - 1], rw_v_up[l - 1], rw_v0[l - 1])
        o_b, v_raw = _rwkv7_branch(prw, x, v_first, v_mix, rw_mu[l], rw_w0[l], rw_w_up[l],
                                   rw_a0[l], rw_a_up[l], rw_g_up[l], rw_k_k[l], rw_k_a[l],
                                   rw_r_k[l], rw_lnx_w[l], rw_lnx_b[l])
        if l == 0:
            v_first = v_raw
        merged = (jax.nn.sigmoid(gate_a) * (o_a @ proj_a[l])
                  + jax.nn.sigmoid(gate_b) * (o_b @ proj_b[l]))
        x = _layernorm(DN_ALPHA * x + merged @ w_out[l], ln1_g[l], ln1_b[l])
        moe_out = _moe(x.reshape(bsz * s, d), router_w[l], router_b[l], moe_w1[l], moe_b1[l],
                       moe_w2[l], moe_b2[l]).reshape(bsz, s, d)
        x = _layernorm(DN_ALPHA * x + moe_out, ln2_g[l], ln2_b[l])
    return x
```

```python
import contextlib
import os
import numpy as np
import concourse.bass as bass
import concourse.mybir as mybir
from concourse.bass_utils import run_bass_kernel_spmd

F32 = mybir.dt.float32
BF16 = mybir.dt.bfloat16
AF = mybir.ActivationFunctionType
ALU = mybir.AluOpType

L_ALL = 4
D = 1024
T = 4096
NB = 8
NCOLS = 10560
RWB = 5120
GAB = RWB + 3392
GBB = GAB + 1024
DN_ALPHA = (2 * L_ALL) ** 0.25
NORM_EPS = 1e-5
RW_LN_EPS = 64e-5
NE = 32
C_W = float(np.exp(-0.5))
SWIGLU_ALPHA = 1.702
SWIGLU_LIMIT = 7.0

FM_MU = 0
FM_W0 = 27
FM_A0 = 43
FM_KK = 51
FM_KA = 59
FM_RK = 67
FM_LNW = 75
FM_LNB = 83
FM_V0 = 91
FM_LBL = 99
FM_NW = 131
FM_MUB = 132
NFM = 133
C_ID = 0
C_FI = 128
C_FS = 640
C_BI = 1152
C_BS = 1664
C_RESET = 2176
C_BD = 2688
C_ONES = 2816
C_I8 = 2944
C_SH1 = 3456
NCONST = 3584


class Buf:
    __slots__ = ("w", "r")

    def __init__(self):
        self.w = {}
        self.r = {}


class Prog:
    NDMA = 8

    def __init__(self):
        self.engs = ("pe", "dve", "act", "pool", "sp")
        self.q = {e: [] for e in self.engs}
        self.cnt = {}
        self.seen = {e: {} for e in self.engs}
        self.dma_slot = {e: 0 for e in self.engs}
        self.sem_keys = ["pe", "dve", "act", "pool"]
        for e in ("sp", "pool", "act"):
            for i in range(self.NDMA):
                self.sem_keys.append(f"d_{e}_{i}")
        for k in self.sem_keys:
            self.cnt[k] = 0

    def _deps(self, eng, reads, writes):
        need = {}
        for b in reads:
            for k, v in b.w.items():
                if need.get(k, 0) < v:
                    need[k] = v
        for b in writes:
            for k, v in b.w.items():
                if need.get(k, 0) < v:
                    need[k] = v
            for k, v in b.r.items():
                if need.get(k, 0) < v:
                    need[k] = v
        waits = []
        seen = self.seen[eng]
        for k, v in need.items():
            if k == "pe" and eng == "pe":
                continue
            if seen.get(k, 0) < v:
                seen[k] = v
                waits.append((k, v))
        return waits

    @staticmethod
    def _mark(ev, reads, writes):
        k, v = ev
        for b in writes:
            b.w[k] = v
        for b in reads:
            b.r[k] = v

    def op(self, eng, fn, reads=(), writes=()):
        self.total = getattr(self, "total", 0) + 1
        if self.total > int(os.environ.get("OPLIM", "1000000000")):
            return None
        waits = self._deps(eng, reads, writes)
        self.cnt[eng] += 1
        ev = (eng, self.cnt[eng])
        self.q[eng].append((fn, waits, (eng, 1)))
        self._mark(ev, reads, writes)
        return ev

    def dma(self, eng, fn, reads=(), writes=()):
        self.total = getattr(self, "total", 0) + 1
        if self.total > int(os.environ.get("OPLIM", "1000000000")):
            return None
        slot = self.dma_slot[eng]
        self.dma_slot[eng] = (slot + 1) % self.NDMA
        key = f"d_{eng}_{slot}"
        waits = self._deps(eng, reads, writes)
        prev = self.cnt[key]
        if prev > 0 and self.seen[eng].get(key, 0) < prev:
            self.seen[eng][key] = prev
            waits.append((key, prev))
        self.cnt[key] += 16
        ev = (key, self.cnt[key])
        self.q[eng].append((fn, waits, (key, 16)))
        self._mark(ev, reads, writes)
        return ev

    def barrier(self):
        snap = dict(self.cnt)
        for e in self.engs:
            waits = []
            for k, v in snap.items():
                if v > 0 and self.seen[e].get(k, 0) < v and not (k == e == "pe"):
                    self.seen[e][k] = v
                    waits.append((k, v))
            if waits:
                self.q[e].append((None, waits, None))

    def emit(self, block, sems):
        decos = {"pe": block.tensor, "dve": block.vector, "act": block.scalar,
                 "pool": block.gpsimd, "sp": block.sync}

        def make(engname):
            ops = self.q[engname]

            def body(e):
                for fn, waits, inc in ops:
                    for k, v in waits:
                        e.wait_ge(sems[k], v)
                    if fn is not None:
                        fn(e).then_inc(sems[inc[0]], inc[1])
            return body
        for engname, deco in decos.items():
            deco(make(engname))


class V:
    __slots__ = ("t", "ap")

    def __init__(self, t, ap):
        self.t = t
        self.ap = ap


class TT:
    def __init__(self, h):
        self.h = h
        self.b = Buf()

    def __getitem__(self, idx):
        return V(self, self.h[idx])


class Builder:
    def __init__(self, n_layers=L_ALL, stop=None, dbg=()):
        self.n_layers = n_layers
        self.stop = stop
        self.dbg = dbg
        self.nc = bass.Bass("TRN2", target_bir_lowering=False)
        self.P = Prog()
        self.es = contextlib.ExitStack()
        self.psi = 0
        self.dq = 0
        self.out_evs = []

    def sb(self, name, shape, dt):
        self.uid = getattr(self, "uid", 0) + 1
        return TT(self.es.enter_context(self.nc.sbuf_tensor(f"{name}_{self.uid}", list(shape), dt)))

    def dram_in(self, name, shape, dt=F32):
        return TT(self.nc.dram_tensor(name, list(shape), dt, kind="ExternalInput").ap())

    def dram_out(self, name, shape, dt=F32):
        return TT(self.nc.dram_tensor(name, list(shape), dt, kind="ExternalOutput").ap())

    def dram_tmp(self, name, shape, dt):
        return TT(self.nc.dram_tensor(name, list(shape), dt).ap())

    def ps(self, hold=False):
        while True:
            t = self.psb[self.psi]
            self.psi = (self.psi + 1) % len(self.psb)
            if not getattr(t, "held", False):
                break
        t.held = hold
        return t

    def mm(self, out, lhsT, rhs, start=True, stop=True):
        o, a, b = out.ap, lhsT.ap, rhs.ap
        self.P.op("pe", lambda e: e.matmul(o, lhsT=a, rhs=b, start=start, stop=stop),
                  reads=[lhsT.t.b, rhs.t.b], writes=[out.t.b])

    def act(self, out, in_, func, bias=None, scale=1.0, accum=None):
        o, i = out.ap, in_.ap
        reads = [in_.t.b]
        kw = {}
        if isinstance(bias, V):
            reads.append(bias.t.b)
            kw["bias"] = bias.ap
        elif bias is not None:
            kw["bias"] = bias
        if isinstance(scale, V):
            reads.append(scale.t.b)
            kw["scale"] = scale.ap
        else:
            kw["scale"] = scale
        writes = [out.t.b]
        if accum is not None:
            kw["accum_out"] = accum.ap
            writes.append(accum.t.b)
        self.P.op("act", lambda e: e.activation(out=o, in_=i, func=func, **kw), reads=reads, writes=writes)

    def ts(self, eng, out, in0, s1, s2=None, op0=ALU.mult, op1=None, accum=None):
        o, i = out.ap, in0.ap
        reads = [in0.t.b]
        a1 = s1
        if isinstance(s1, V):
            reads.append(s1.t.b)
            a1 = s1.ap
        a2 = s2
        if isinstance(s2, V):
            reads.append(s2.t.b)
            a2 = s2.ap
        kw = {}
        if op1 is not None:
            kw["op1"] = op1
        writes = [out.t.b]
        if accum is not None:
            kw["accum_out"] = accum.ap
            writes.append(accum.t.b)
        self.P.op(eng, lambda e: e.tensor_scalar(out=o, in0=i, scalar1=a1, scalar2=a2, op0=op0, **kw),
                  reads=reads, writes=writes)

    def tt(self, eng, out, in0, in1, op):
        o, a, b = out.ap, in0.ap, in1.ap
        self.P.op(eng, lambda e: e.tensor_tensor(out=o, in0=a, in1=b, op=op),
                  reads=[in0.t.b, in1.t.b], writes=[out.t.b])

    def stt(self, out, in0, scalar, in1, op0, op1):
        o, a, b = out.ap, in0.ap, in1.ap
        reads = [in0.t.b, in1.t.b]
        s = scalar
        if isinstance(scalar, V):
            reads.append(scalar.t.b)
            s = scalar.ap
        self.P.op("dve", lambda e: e.scalar_tensor_tensor(out=o, in0=a, scalar=s, in1=b, op0=op0, op1=op1),
                  reads=reads, writes=[out.t.b])

    def cp(self, eng, out, in_):
        o, i = out.ap, in_.ap
        if eng == "act":
            self.P.op("act", lambda e: e.activation(out=o, in_=i, func=AF.Copy), reads=[in_.t.b], writes=[out.t.b])
        else:
            self.P.op(eng, lambda e: e.tensor_copy(out=o, in_=i), reads=[in_.t.b], writes=[out.t.b])

    def memset(self, eng, out, val):
        o = out.ap
        self.P.op(eng, lambda e: e.memset(o, val), writes=[out.t.b])

    def scan(self, out, d0, d1):
        o, a, b = out.ap, d0.ap, d1.ap
        self.P.op("dve", lambda e: e.tensor_tensor_scan(out=o, data0=a, data1=b, initial=0.0, op0=ALU.mult, op1=ALU.add),
                  reads=[d0.t.b, d1.t.b], writes=[out.t.b])

    def recip(self, out, in_):
        o, i = out.ap, in_.ap
        self.P.op("dve", lambda e: e.reciprocal(out=o, in_=i), reads=[in_.t.b], writes=[out.t.b])

    def dma(self, out, in_, eng=None):
        if eng is None:
            eng = ("sp", "pool")[self.dq % 2]
            self.dq += 1
        o, i = out.ap, in_.ap
        return self.P.dma(eng, lambda e: e.dma_start(out=o, in_=i), reads=[in_.t.b], writes=[out.t.b])

    def build(self):
        nc = self.nc
        n_layers = self.n_layers
        self.x_in = self.dram_in("x", [T, D])
        self.w_in = self.dram_in("w_in", [L_ALL, D, NCOLS])
        self.w_up = self.dram_in("rw_w_up", [L_ALL, 128, D])
        self.a_up = self.dram_in("rw_a_up", [L_ALL, 64, D])
        self.g_up = self.dram_in("rw_g_up", [L_ALL, 128, D])
        self.v_down = self.dram_in("rw_v_down", [L_ALL - 1, D, 32])
        self.v_up = self.dram_in("rw_v_up", [L_ALL - 1, 32, D])
        self.proj_a = self.dram_in("proj_a", [L_ALL, D, D])
        self.proj_b = self.dram_in("proj_b", [L_ALL, D, D])
        self.w_out = self.dram_in("w_out", [L_ALL, D, D])
        self.router_w = self.dram_in("router_w", [L_ALL, D, NE])
        self.need_moe = self.stop is None or self.stop[1] == "moe"
        if self.need_moe:
            self.moe_w1 = self.dram_in("moe_w1", [L_ALL, NE, D, 2 * D])
            self.moe_w2 = self.dram_in("moe_w2", [L_ALL, NE, D, D])
            self.moe_b2 = self.dram_in("moe_b2", [L_ALL, NE, D])
        self.fm_pack = self.dram_in("fm_pack", [L_ALL, 128, NFM])
        self.bc_pack = self.dram_in("bc_pack", [L_ALL, 128, 4 * D + NE])
        self.b1_pack = self.dram_in("b1_pack", [L_ALL, 128, NE * 16])
        self.consts_d = self.dram_in("consts", [128, NCONST])
        self.y_out = self.dram_out("y", [T, D])
        self.xres = [self.dram_tmp("xres0", [T, D], F32), self.dram_tmp("xres1", [T, D], F32)]
        self.x1d = self.dram_tmp("x1d", [T, D], F32)
        self.oaT = self.dram_tmp("oaT", [D, T], BF16)
        self.obT = self.dram_tmp("obT", [D, T], BF16)
        self.mgT = self.dram_tmp("mgT", [D, T], BF16)
        self.vfT = self.dram_tmp("vfT", [D, T], F32)
        self.yfd = self.dram_tmp("yfd", [D, T], F32)
        self.twl_d = [self.dram_tmp("twlf_d", [64, T], BF16), self.dram_tmp("twlb_d", [64, T], BF16)]
        self.alo_d = self.dram_tmp("alo_d", [64, T], BF16)
        self.sigg_d = self.dram_tmp("sigg_d", [128, T], BF16)
        self.xvd_d = self.dram_tmp("xvd_d", [32, T], BF16)
        self.dbg_out = {}
        for name, shape, dt in self.dbg:
            self.dbg_out[name] = self.dram_out("dbg_" + name, shape, dt)

        with self.es:
            sems = {}
            for k in self.P.sem_keys:
                sems[k] = self.es.enter_context(nc.semaphore("s_" + k))
            self.psb = [TT(self.es.enter_context(nc.psum_tensor(f"ps{i}", [128, 512], F32))) for i in range(8)]
            self.XT = self.sb("XT", [128, 8, T], BF16)
            self.CST = self.sb("CST", [128, NCONST], F32)
            self.IDB = self.sb("IDB", [128, 128], BF16)
            self.BDB = self.sb("BDB", [128, 128], BF16)
            self.ONB = self.sb("ONB", [128, 128], BF16)
            self.FM = self.sb("FM", [128, NFM], F32)
            self.DER = self.sb("DER", [128, 96], F32)
            self.dma(self.CST[:, :], self.consts_d[:, :])
            self.cp("dve", self.IDB[:, :], self.CST[:, C_ID:C_ID + 128])
            self.cp("dve", self.BDB[:, :], self.CST[:, C_BD:C_BD + 128])
            self.cp("dve", self.ONB[:, :], self.CST[:, C_ONES:C_ONES + 128])

            for l in range(n_layers):
                src = self.x_in if l == 0 else self.xres[l % 2]
                dst = self.y_out if l == L_ALL - 1 else self.xres[(l + 1) % 2]
                self.layer(l, src, dst)
                if self.stop is not None and self.stop[0] == l:
                    break
            final_evs = []
            for name, (srcT, sl) in getattr(self, "dbg_src", {}).items():
                final_evs.append(self.dma(self.dbg_out[name][sl], srcT[sl], eng="sp"))
            self.P.barrier()
            with nc.Block() as block:
                self.P.emit(block, sems)
        return nc

    def params(self, l):
        self.dma(self.FM[:, :], self.fm_pack[l])
        DER, FM = self.DER, self.FM
        self.ts("dve", DER[:, 0:27], FM[:, FM_MU:FM_MU + 27], -1.0, 1.0, ALU.mult, ALU.add)
        self.ts("dve", DER[:, 27:54], FM[:, FM_MU:FM_MU + 27], 0.5, None, ALU.mult)
        self.ts("dve", DER[:, 54:62], FM[:, FM_KA:FM_KA + 8], -1.0, 1.0, ALU.mult, ALU.add)
        E = self.sb(f"lbE{l}", [128, 32], F32)
        self.act(E[:, :], FM[:, FM_LBL:FM_LBL + 32], AF.Exp)
        self.tt("dve", DER[:, 78:86], E[:, 0:8], E[:, 8:16], ALU.add)
        self.tt("dve", DER[:, 78:86], DER[:, 78:86], E[:, 16:24], ALU.add)
        self.tt("dve", DER[:, 78:86], DER[:, 78:86], E[:, 24:32], ALU.add)
        self.recip(DER[:, 78:86], DER[:, 78:86])
        self.memset("dve", DER[:, 62:70], 0.0)
        for i in range(1, l + 1):
            self.tt("dve", DER[:, 62:70], DER[:, 62:70], E[:, 8 * i:8 * i + 8], ALU.add)
        self.tt("dve", DER[:, 62:70], DER[:, 62:70], DER[:, 78:86], ALU.mult)
        self.ts("dve", DER[:, 70:78], DER[:, 62:70], -1.0, 1.0, ALU.mult, ALU.add)
        self.ts("dve", DER[:, 88:89], FM[:, FM_MUB:FM_MUB + 1], -1.0, 1.0, ALU.mult, ALU.add)
        self.ts("dve", DER[:, 89:90], FM[:, FM_MUB:FM_MUB + 1], 0.5, None, ALU.mult)
        self.memset("dve", DER[:, 86:87], NORM_EPS)
        self.memset("dve", DER[:, 87:88], RW_LN_EPS)

    def load_w(self, stage, wbf, src_tt, l, col0, n, dst_col, conv_eng):
        src = src_tt.h[l][:, col0:col0 + n].rearrange("(c p) n -> p c n", p=128)
        self.dma(stage[:, :, 0:n], V(src_tt, src))
        self.cp(conv_eng, wbf[:, :, dst_col:dst_col + n], stage[:, :, 0:n])

    def proj_fm(self, out, wbf, col0, m, t0, n, xt=None):
        xt = xt or self.XT
        for c in range(8):
            self.mm(out, wbf[:, c, col0:col0 + m], xt[:, c, t0:t0 + n], start=(c == 0), stop=(c == 7))

    def build_xt(self, src):
        XL = [self.sb(f"XL{i}_{id(src) % 1000}", [128, D], F32) for i in range(2)]
        for i in range(T // 128):
            xl = XL[i % 2]
            self.dma(xl[:, :], src[i * 128:(i + 1) * 128, :])
            for half in range(2):
                p = self.ps()
                for c in range(4):
                    cc = half * 4 + c
                    self.mm(p[:, c * 128:(c + 1) * 128], xl[:, cc * 128:(cc + 1) * 128], self.CST[:, C_ID:C_ID + 128])
                for c in range(4):
                    cc = half * 4 + c
                    self.cp("act" if c % 2 else "dve", self.XT[:, cc, i * 128:(i + 1) * 128], p[:, c * 128:(c + 1) * 128])

    def layer(self, l, src, dst):
        es_outer = self.es
        self.params(l)
        with contextlib.ExitStack() as es:
            self.es = es
            self.build_xt(src)
        self.es = es_outer
        self.P.barrier()
        if self.stop == (l, "xt"):
            return
        with contextlib.ExitStack() as es:
            self.es = es
            self.hgrn(l)
        self.es = es_outer
        self.P.barrier()
        if self.stop == (l, "hgrn"):
            return
        with contextlib.ExitStack() as es:
            self.es = es
            self.rwkv(l)
        self.es = es_outer
        self.P.barrier()
        if self.stop == (l, "rwkv"):
            return
        with contextlib.ExitStack() as es:
            self.es = es
            self.merge(l, src)
        self.es = es_outer
        self.P.barrier()
        if self.stop == (l, "merge"):
            return
        with contextlib.ExitStack() as es:
            self.es = es
            self.moe(l, dst)
        self.es = es_outer
        self.P.barrier()

    def hgrn(self, l):
        sb = self.sb
        CST, DER, FM, XT = self.CST, self.DER, self.FM, self.XT
        WS = [sb(f"h_ws{i}", [128, 8, 128], F32) for i in range(2)]
        WH = [sb(f"h_wh{i}", [128, 8, 640], BF16) for i in range(2)]
        OF = sb("h_of", [128, T], F32)
        S32 = sb("h_s32", [128, 128], F32)
        SBF = sb("h_sbf", [128, 128], BF16)
        Fm = sb("h_f", [128, 512], F32)
        LF = sb("h_lf", [128, 512], F32)
        KX = sb("h_kx", [128, 512], F32)
        G = sb("h_g", [128, 512], F32)
        D1 = sb("h_d1", [128, 512], F32)
        G2 = sb("h_g2", [128, 512], F32)
        EN = sb("h_en", [128, 512], F32)
        KT32 = sb("h_kt32", [128, 512], F32)
        KH = sb("h_kh", [128, 512], BF16)
        SOG = sb("h_sog", [128, 512], F32)
        O = sb("h_o", [128, 512], F32)
        SQ = sb("h_sq", [128, 512], BF16)
        RS = sb("h_rs", [128, 512], F32)
        OUTB = [sb(f"h_outb{i}", [128, 512], BF16) for i in range(2)]
        EP = [sb(f"h_ep{i}", [128, 512], F32) for i in range(2)]
        QT = [sb(f"h_qt{i}", [128, 512], BF16) for i in range(2)]
        KT = [sb(f"h_kt{i}", [128, 512], BF16) for i in range(2)]
        VT = [sb(f"h_vt{i}", [64, 1024], BF16) for i in range(2)]
        KHT = [sb(f"h_kht{i}", [64, 1024], BF16) for i in range(2)]
        AT = [sb(f"h_at{i}", [64, 512], BF16) for i in range(2)]
        cnt = 0
        for h in range(8):
            wh = WH[h % 2]
            for qi in range(5):
                self.load_w(WS[qi % 2], wh, self.w_in, l, qi * 1024 + h * 128, 128, qi * 128, "pool")
            for d in (0, 1):
                self.memset("dve", S32[:, :], 0.0)
                self.memset("dve", SBF[:, :], 0.0)
                for bi in range(NB):
                    blk = bi if d == 0 else NB - 1 - bi
                    t0 = blk * 512
                    k = cnt % 2
                    cnt += 1
                    ep, qt, kt, vt, kht, at = EP[k], QT[k], KT[k], VT[k], KHT[k], AT[k]
                    pq = self.ps()
                    self.proj_fm(pq[:, :], wh, 0, 128, t0, 512)
                    pf = self.ps()
                    self.proj_fm(pf[:, :], wh, 128 * (1 + d), 128, t0, 512)
                    for half in range(2):
                        pv = self.ps()
                        for cc in range(4):
                            c = half * 4 + cc
                            for kk in range(8):
                                self.mm(pv[0:64, cc * 128:(cc + 1) * 128], XT[:, kk, t0 + c * 64:t0 + c * 64 + 64],
                                        wh[:, kk, 384:512], start=(kk == 0), stop=(kk == 7))
                        self.cp("act", vt[0:64, half * 512:(half + 1) * 512], pv[0:64, :])
                    if d == 1:
                        pog = self.ps()
                        self.proj_fm(pog[:, :], wh, 512, 128, t0, 512)
                        self.act(SOG[:, :], pog[:, :], AF.Silu)
                    self.act(Fm[:, :], pf[:, :], AF.Sigmoid)
                    self.ts("dve", Fm[:, :], Fm[:, :], DER[:, 70 + h:71 + h], DER[:, 62 + h:63 + h], ALU.mult, ALU.add)
                    self.act(LF[:, :], Fm[:, :], AF.Ln)
                    self.ts("pool", KX[:, :], Fm[:, :], -1.0, 1.0, ALU.mult, ALU.add)
                    self.scan(G[:, :], CST[:, C_RESET:C_RESET + 512], LF[:, :])
                    g = G
                    if d == 1:
                        self.tt("pool", D1[:, :], LF[:, :], G[:, :], ALU.subtract)
                        for c in range(8):
                            self.ts("dve", G2[:, c * 64:(c + 1) * 64], D1[:, c * 64:(c + 1) * 64],
                                    G[:, c * 64 + 63:c * 64 + 64], None, ALU.add)
                        g = G2
                    self.act(ep[:, :], g[:, :], AF.Exp)
                    self.act(EN[:, :], g[:, :], AF.Exp, scale=-1.0)
                    self.tt("dve", qt[:, :], pq[:, :], ep[:, :], ALU.mult)
                    self.tt("pool", KT32[:, :], KX[:, :], EN[:, :], ALU.mult)
                    self.cp("pool", kt[:, :], KT32[:, :])
                    for c in range(8):
                        ec = c * 64 + 63 if d == 0 else c * 64
                        self.ts("dve" if c % 2 else "pool", KH[:, c * 64:(c + 1) * 64], KT32[:, c * 64:(c + 1) * 64],
                                ep[:, ec:ec + 1], None, ALU.mult)
                    for half in range(2):
                        pk = self.ps()
                        for cc in range(4):
                            c = half * 4 + cc
                            self.mm(pk[0:64, cc * 128:(cc + 1) * 128], KH[:, c * 64:(c + 1) * 64], self.IDB[:, :])
                        self.cp("act", kht[0:64, half * 512:(half + 1) * 512], pk[0:64, :])
                    psc = self.ps()
                    for c in range(8):
                        self.mm(psc[0:64, c * 64:(c + 1) * 64], kt[:, c * 64:(c + 1) * 64], qt[:, c * 64:(c + 1) * 64])
                    mcol = C_FI if d == 0 else C_BI
                    self.tt("dve", at[0:64, :], psc[0:64, :], CST[0:64, mcol:mcol + 512], ALU.mult)
                    po = self.ps(hold=True)
                    for ci in range(8):
                        c = ci if d == 0 else 7 - ci
                        ec = c * 64 + 63 if d == 0 else c * 64
                        self.mm(po[:, c * 64:(c + 1) * 64], vt[0:64, c * 128:(c + 1) * 128], at[0:64, c * 64:(c + 1) * 64],
                                start=True, stop=False)
                        self.mm(po[:, c * 64:(c + 1) * 64], SBF[:, :], qt[:, c * 64:(c + 1) * 64], start=False, stop=True)
                        pss = self.ps()
                        self.mm(pss[:, 0:128], kht[0:64, c * 128:(c + 1) * 128], vt[0:64, c * 128:(c + 1) * 128])
                        self.stt(S32[:, :], S32[:, :], ep[:, ec:ec + 1], pss[:, 0:128], ALU.mult, ALU.add)
                        self.cp("act", SBF[:, :], S32[:, :])
                    po.held = False
                    if d == 0:
                        self.cp("act", OF[:, t0:t0 + 512], po[:, :])
                    else:
                        ob = OUTB[bi % 2]
                        self.tt("dve", O[:, :], po[:, :], OF[:, t0:t0 + 512], ALU.add)
                        self.act(SQ[:, :], O[:, :], AF.Square)
                        pn = self.ps()
                        self.mm(pn[:, :], self.ONB[:, :], SQ[:, :])
                        self.act(RS[:, :], pn[:, :], AF.Sqrt, bias=DER[:, 86:87], scale=1.0 / 128.0)
                        self.recip(RS[:, :], RS[:, :])
                        self.tt("dve", O[:, :], O[:, :], RS[:, :], ALU.mult)
                        self.stt(ob[:, :], O[:, :], FM[:, FM_NW:FM_NW + 1], SOG[:, :], ALU.mult, ALU.mult)
                        self.dma(self.oaT[h * 128:(h + 1) * 128, t0:t0 + 512], ob[:, :])
        self.dbg_src = getattr(self, "dbg_src", {})
        if "oaT" in self.dbg_out:
            self.dbg_src["oaT"] = (self.oaT, (slice(None), slice(None)))
        if "OF" in self.dbg_out:
            self.dbg_src["OF"] = (OF, (slice(None), slice(None)))

    def shift_block(self, pm, hl, hr, rows, omm, hmu, PADB, TMPA, TMPB, OUT):
        R = slice(0, rows)
        self.cp("act", PADB[R, 1:513], pm)
        if hl is None:
            self.memset("dve", PADB[R, 0:1], 0.0)
        else:
            self.cp("dve", PADB[R, 0:1], hl)
        if hr is None:
            self.memset("dve", PADB[R, 513:514], 0.0)
        else:
            self.cp("dve", PADB[R, 513:514], hr)
        self.tt("dve", TMPA[R, :], PADB[R, 0:512], PADB[R, 2:514], ALU.add)
        self.act(TMPB[R, :], PADB[R, 1:513], AF.Identity, scale=omm)
        self.stt(OUT, TMPA[R, :], hmu, TMPB[R, :], ALU.mult, ALU.add)

    def proj_shift(self, wbf, col0, rows, blk, omm, hmu, PADB, TMPA, TMPB, OUT):
        XT = self.XT
        t0 = blk * 512
        pm = self.ps()
        self.proj_fm(pm[0:rows, :], wbf, col0, rows, t0, 512)
        ph = self.ps()
        hl = hr = None
        if blk > 0:
            for c in range(8):
                self.mm(ph[0:rows, 0:1], wbf[:, c, col0:col0 + rows], XT[:, c, t0 - 1:t0], start=(c == 0), stop=(c == 7))
            hl = ph[0:rows, 0:1]
        if blk < NB - 1:
            for c in range(8):
                self.mm(ph[0:rows, 1:2], wbf[:, c, col0:col0 + rows], XT[:, c, t0 + 512:t0 + 513], start=(c == 0), stop=(c == 7))
            hr = ph[0:rows, 1:2]
        self.shift_block(pm[0:rows, :], hl, hr, rows, omm, hmu, PADB, TMPA, TMPB, OUT)

    def rwkv(self, l):
        sb = self.sb
        CST, DER, FM, XT = self.CST, self.DER, self.FM, self.XT
        f32t = lambda n: sb(n, [128, 512], F32)
        bft = lambda n: sb(n, [128, 512], BF16)
        PADB = sb("r_padb", [128, 514], F32)
        TMPA, TMPB = f32t("r_tmpa"), f32t("r_tmpb")
        WS = [sb(f"r_ws{i}", [128, 8, 128], F32) for i in range(2)]
        WUP = sb("r_wup", [128, D], BF16)
        AUP = sb("r_aup", [64, D], BF16)
        GUP = sb("r_gup", [128, D], BF16)
        WUPB = sb("r_wupb", [64, D], BF16)
        LOB = sb("r_lob", [128, 512], BF16)
        TWLt = sb("r_twlt", [64, 512], BF16)
        ALOt = sb("r_alot", [64, 512], BF16)
        SIGGt = sb("r_siggt", [128, 512], BF16)
        if l > 0:
            VUP = sb("r_vup", [32, D], BF16)
            XVDt = sb("r_xvdt", [32, 512], BF16)
        es_keep = self.es
        with contextlib.ExitStack() as es0:
            self.es = es0
            WL = sb("r_wl", [128, 8, 320], BF16)
            self.load_w(WS[0], WL, self.w_in, l, RWB + 3072, 128, 0, "pool")
            self.load_w(WS[1], WL, self.w_in, l, RWB + 3200, 64, 128, "pool")
            self.load_w(WS[0], WL, self.w_in, l, RWB + 3264, 128, 192, "pool")
            LST = sb("r_lst", [128, D], F32)
            self.dma(LST[:, :], self.w_up[l])
            self.cp("dve", WUP[:, :], LST[:, :])
            self.dma(LST[0:64, :], V(self.w_up, self.w_up.h[l][64:128, :]))
            self.cp("dve", WUPB[0:64, :], LST[0:64, :])
            self.dma(LST[0:64, :], self.a_up[l])
            self.cp("dve", AUP[0:64, :], LST[0:64, :])
            self.dma(LST[:, :], self.g_up[l])
            self.cp("dve", GUP[:, :], LST[:, :])
            if l > 0:
                self.dma(LST[0:32, :], self.v_up[l - 1])
                self.cp("dve", VUP[0:32, :], LST[0:32, :])
                VDS = sb("r_vds", [128, 8, 32], F32)
                VD = sb("r_vd", [128, 8, 32], BF16)
                src = self.v_down.h[l - 1].rearrange("(c p) n -> p c n", p=128)
                self.dma(VDS[:, :, :], V(self.v_down, src))
                self.cp("dve", VD[:, :, :], VDS[:, :, :])
            SHT = f32t("r_sht")
            for blk in range(NB):
                t0 = blk * 512
                self.proj_shift(WL, 0, 64, blk, DER[0:64, 24:25], DER[0:64, 51:52], PADB, TMPA, TMPB, SHT[0:64, :])
                self.act(LOB[0:64, :], SHT[0:64, :], AF.Tanh)
                self.dma(self.twl_d[0][:, t0:t0 + 512], LOB[0:64, :])
                self.proj_shift(WL, 64, 64, blk, DER[0:64, 88:89], DER[0:64, 89:90], PADB, TMPA, TMPB, SHT[0:64, :])
                self.act(LOB[0:64, :], SHT[0:64, :], AF.Tanh)
                self.dma(self.twl_d[1][:, t0:t0 + 512], LOB[0:64, :])
                self.proj_shift(WL, 128, 64, blk, DER[0:64, 25:26], DER[0:64, 52:53], PADB, TMPA, TMPB, SHT[0:64, :])
                self.cp("act", LOB[0:64, :], SHT[0:64, :])
                self.dma(self.alo_d[:, t0:t0 + 512], LOB[0:64, :])
                self.proj_shift(WL, 192, 128, blk, DER[:, 26:27], DER[:, 53:54], PADB, TMPA, TMPB, SHT[:, :])
                self.act(LOB[:, :], SHT[:, :], AF.Sigmoid)
                self.dma(self.sigg_d[:, t0:t0 + 512], LOB[:, :])
                if l > 0:
                    pm = self.ps()
                    self.proj_fm(pm[0:32, :], VD, 0, 32, t0, 512)
                    self.cp("act", LOB[0:32, :], pm[0:32, :])
                    self.dma(self.xvd_d[:, t0:t0 + 512], LOB[0:32, :])
        self.es = es_keep
        self.P.barrier()
        import os
        RS = int(os.environ.get("RW_STOP", "99"))
        if RS == 0:
            return
        WP = [sb("r_wp0", [128, 8, 384], BF16)] * 2
        SH = [f32t(f"r_sh{q}") for q in range(3)]
        LW, A, VF, VV, KKN, KFIN, BV = [f32t("r_" + n) for n in ("lw", "a", "vf", "vv", "kkn", "kfin", "bv")]
        G, G2, EP, EN, EX, BT32, KT32, YB, YC = [f32t("r_" + n) for n in ("g", "g2", "ep", "en", "ex", "bt32", "kt32", "yb", "yc")]
        KSQ, ATt, BTt, KTt, RTt, BH, KHh, VB = [bft("r_" + n) for n in ("ksq", "at", "bt", "kt", "rt", "bh", "khh", "vb")]
        OB = [bft(f"r_ob{i}") for i in range(2)]
        h64 = lambda n: sb(n, [64, 1024], BF16)
        VTK, BHT, KHT, MAK, MRB, MRK, TTm = [h64("r_" + n) for n in ("vtk", "bht", "kht", "mak", "mrb", "mrk", "ttm")]
        XA, XtA, XB_, XtB = [h64("r_" + n) for n in ("xa", "xta", "xb", "xtb")]
        U = sb("r_u", [64, 128], BF16)
        UM = sb("r_um", [64, 1024], BF16)
        Pm = sb("r_pm", [64, 128], BF16)
        S32 = sb("r_s32", [64, 128], F32)
        SBF = sb("r_sbf", [64, 128], BF16)
        AT1, BT1, KT1, RT1 = [sb("r_" + n, [64, 512], BF16) for n in ("at1", "bt1", "kt1", "rt1")]
        EP1 = sb("r_ep1", [64, 512], F32)
        YH = sb("r_yh", [64, 2, 512], F32)
        SH0 = CST[0:64, C_ID:C_ID + 128]
        SH1 = CST[0:64, C_SH1:C_SH1 + 128]
        BDF = CST[:, C_BD:C_BD + 128]
        for j in range(8):
            wp = WP[j % 2]
            for q in range(3):
                self.load_w(WS[q % 2], wp, self.w_in, l, RWB + q * 1024 + 128 * j, 128, q * 128, "pool")
            jc = slice(128 * j, 128 * j + 128)
            for d in (0, 1):
                self.memset("dve", S32[:, :], 0.0)
                self.memset("dve", SBF[:, :], 0.0)
                for bi in range(NB):
                    blk = bi if d == 0 else NB - 1 - bi
                    t0 = blk * 512
                    tb = slice(t0, t0 + 512)
                    for q in range(3):
                        mc = q * 8 + j
                        self.proj_shift(wp, q * 128, 128, blk, DER[:, mc:mc + 1], DER[:, 27 + mc:28 + mc], PADB, TMPA, TMPB, SH[q][:, :])
                    if RS == 1:
                        return
                    pz = self.ps()
                    self.dma(TWLt[0:64, :], self.twl_d[d][:, tb])
                    self.mm(pz[:, :], (WUP if d == 0 else WUPB)[0:64, jc], TWLt[0:64, :])
                    self.act(LW[:, :], pz[:, :], AF.Sigmoid, bias=FM[:, FM_W0 + d * 8 + j:FM_W0 + d * 8 + j + 1])
                    self.ts("pool", LW[:, :], LW[:, :], -C_W, None, ALU.mult)
                    pa = self.ps()
                    self.dma(ALOt[0:64, :], self.alo_d[:, tb])
                    self.mm(pa[:, :], AUP[0:64, jc], ALOt[0:64, :])
                    self.act(A[:, :], pa[:, :], AF.Sigmoid, bias=FM[:, FM_A0 + j:FM_A0 + j + 1])
                    if l == 0:
                        if d == 0:
                            self.dma(self.vfT[jc, tb], SH[2][:, :])
                        Vv = SH[2]
                    else:
                        pv = self.ps()
                        self.dma(XVDt[0:32, :], self.xvd_d[:, tb])
                        self.mm(pv[:, :], VUP[0:32, jc], XVDt[0:32, :])
                        self.act(TMPA[:, :], pv[:, :], AF.Sigmoid, bias=FM[:, FM_V0 + j:FM_V0 + j + 1])
                        self.dma(VF[:, :], self.vfT[jc, tb])
                        self.tt("pool", TMPB[:, :], VF[:, :], SH[2][:, :], ALU.subtract)
                        self.tt("dve", TMPB[:, :], TMPB[:, :], TMPA[:, :], ALU.mult)
                        self.tt("dve", VV[:, :], SH[2][:, :], TMPB[:, :], ALU.add)
                        Vv = VV
                    if RS == 2:
                        return
                    self.ts("dve", TMPB[:, :], SH[1][:, :], FM[:, FM_KK + j:FM_KK + j + 1], None, ALU.mult)
                    self.act(KSQ[:, :], TMPB[:, :], AF.Square)
                    pss = self.ps()
                    self.mm(pss[:, :], self.BDB[:, :], KSQ[:, :])
                    self.act(TMPA[:, :], pss[:, :], AF.Sqrt)
                    self.ts("dve", TMPA[:, :], TMPA[:, :], 1e-12, None, ALU.max)
                    self.recip(TMPA[:, :], TMPA[:, :])
                    self.tt("dve", KKN[:, :], TMPB[:, :], TMPA[:, :], ALU.mult)
                    self.ts("dve", TMPA[:, :], A[:, :], FM[:, FM_KA + j:FM_KA + j + 1], DER[:, 54 + j:55 + j], ALU.mult, ALU.add)
                    self.tt("dve", KFIN[:, :], TMPA[:, :], SH[1][:, :], ALU.mult)
                    self.tt("pool", BV[:, :], KKN[:, :], A[:, :], ALU.mult)
                    if RS == 3:
                        return
                    self.scan(G[:, :], CST[:, C_RESET:C_RESET + 512], LW[:, :])
                    g = G
                    if d == 1:
                        self.tt("pool", TMPA[:, :], LW[:, :], G[:, :], ALU.subtract)
                        for c in range(8):
                            self.ts("dve", G2[:, c * 64:(c + 1) * 64], TMPA[:, c * 64:(c + 1) * 64],
                                    G[:, c * 64 + 63:c * 64 + 64], None, ALU.add)
                        g = G2
                    self.tt("pool", TMPB[:, :], g[:, :], LW[:, :], ALU.subtract)
                    self.act(EP[:, :], g[:, :], AF.Exp)
                    self.act(EN[:, :], g[:, :], AF.Exp, scale=-1.0)
                    self.act(EX[:, :], TMPB[:, :], AF.Exp)
                    self.stt(ATt[:, :], KKN[:, :], -1.0, EX[:, :], ALU.mult, ALU.mult)
                    self.tt("dve", BT32[:, :], BV[:, :], EN[:, :], ALU.mult)
                    self.cp("pool", BTt[:, :], BT32[:, :])
                    self.tt("dve", KT32[:, :], KFIN[:, :], EN[:, :], ALU.mult)
                    self.cp("pool", KTt[:, :], KT32[:, :])
                    self.tt("dve", RTt[:, :], SH[0][:, :], EP[:, :], ALU.mult)
                    for c in range(8):
                        ec = c * 64 + 63 if d == 0 else c * 64
                        cs = slice(c * 64, c * 64 + 64)
                        self.ts("dve", BH[:, cs], BT32[:, cs], EP[:, ec:ec + 1], None, ALU.mult)
                        self.ts("pool", KHh[:, cs], KT32[:, cs], EP[:, ec:ec + 1], None, ALU.mult)
                    self.cp("act", VB[:, :], Vv[:, :])
                    self.dma(AT1[0:64, :], ATt[64:128, :])
                    self.dma(BT1[0:64, :], BTt[64:128, :])
                    self.dma(KT1[0:64, :], KTt[64:128, :])
                    self.dma(RT1[0:64, :], RTt[64:128, :])
                    self.dma(EP1[0:64, :], EP[64:128, :])
                    HA = (ATt, AT1)
                    HB = (BTt, BT1)
                    HK = (KTt, KT1)
                    HR = (RTt, RT1)
                    HEP = (EP, EP1)
                    if RS == 4:
                        return
                    for srcb, dstb in ((VB, VTK), (BH, BHT), (KHh, KHT)):
                        for half in range(2):
                            pk = self.ps()
                            for cc in range(4):
                                c = half * 4 + cc
                                self.mm(pk[0:64, cc * 128:(cc + 1) * 128], srcb[:, c * 64:(c + 1) * 64], self.IDB[:, :])
                            self.cp("act" if half else "dve", dstb[0:64, half * 512:(half + 1) * 512], pk[0:64, :])
                    m_strict = C_FS if d == 0 else C_BS
                    m_strict_T = C_BS if d == 0 else C_FS
                    m_incl = C_FI if d == 0 else C_BI
                    for half in range(2):
                        banks = [self.ps() for _ in range(5)]
                        for cc in range(4):
                            c = half * 4 + cc
                            cs = slice(c * 64, c * 64 + 64)
                            for e in range(2):
                                R = slice(64 * e, 64 * e + 64)
                                co = slice((cc * 2 + e) * 64, (cc * 2 + e) * 64 + 64)
                                Z = slice(0, 64)
                                self.mm(banks[0][0:64, co], HB[e][Z, cs], HA[e][Z, cs])
                                self.mm(banks[1][0:64, co], HA[e][Z, cs], HB[e][Z, cs])
                                self.mm(banks[2][0:64, co], HK[e][Z, cs], HA[e][Z, cs])
                                self.mm(banks[3][0:64, co], HB[e][Z, cs], HR[e][Z, cs])
                                self.mm(banks[4][0:64, co], HK[e][Z, cs], HR[e][Z, cs])
                        hs = slice(half * 512, half * 512 + 512)
                        self.tt("dve", XA[0:64, hs], banks[0][0:64, :], CST[0:64, m_strict:m_strict + 512], ALU.mult)
                        self.tt("dve", XtA[0:64, hs], banks[1][0:64, :], CST[0:64, m_strict_T:m_strict_T + 512], ALU.mult)
                        self.tt("dve", MAK[0:64, hs], banks[2][0:64, :], CST[0:64, m_strict:m_strict + 512], ALU.mult)
                        self.tt("dve", MRB[0:64, hs], banks[3][0:64, :], CST[0:64, m_incl:m_incl + 512], ALU.mult)
                        self.tt("dve", MRK[0:64, hs], banks[4][0:64, :], CST[0:64, m_incl:m_incl + 512], ALU.mult)
                        self.tt("pool", TTm[0:64, hs], XA[0:64, hs], CST[0:64, C_I8:C_I8 + 512], ALU.add)
                    if RS == 5:
                        return
                    Xc, Xtc, Xn, Xtn = XA, XtA, XB_, XtB
                    for lev in range(5):
                        for half in range(2):
                            hs = slice(half * 512, half * 512 + 512)
                            p2 = self.ps()
                            for qq in range(8):
                                co = slice(qq * 64, qq * 64 + 64)
                                sc = slice(half * 512 + qq * 64, half * 512 + qq * 64 + 64)
                                self.mm(p2[0:64, co], Xc[0:64, sc], Xtc[0:64, sc])
                            self.cp("act", Xtn[0:64, hs], p2[0:64, :])
                            if lev < 4:
                                p1 = self.ps()
                                for qq in range(8):
                                    co = slice(qq * 64, qq * 64 + 64)
                                    sc = slice(half * 512 + qq * 64, half * 512 + qq * 64 + 64)
                                    self.mm(p1[0:64, co], Xtc[0:64, sc], Xc[0:64, sc])
                                self.cp("act", Xn[0:64, hs], p1[0:64, :])
                            p3 = self.ps()
                            for qq in range(8):
                                co = slice(qq * 64, qq * 64 + 64)
                                sc = slice(half * 512 + qq * 64, half * 512 + qq * 64 + 64)
                                self.mm(p3[0:64, co], Xtn[0:64, sc], TTm[0:64, sc])
                            self.tt("dve", TTm[0:64, hs], TTm[0:64, hs], p3[0:64, :], ALU.add)
                        Xc, Xtc, Xn, Xtn = Xn, Xtn, Xc, Xtc
                    if RS == 6:
                        return
                    for half in range(2):
                        pum = self.ps()
                        for cc in range(4):
                            c = half * 4 + cc
                            for e in range(2):
                                co = slice((c * 2 + e) * 64, (c * 2 + e) * 64 + 64)
                                self.mm(pum[0:64, cc * 128 + 64 * e:cc * 128 + 64 * e + 64], MAK[0:64, co],
                                        VTK[0:64, c * 128 + 64 * e:c * 128 + 64 * e + 64])
                        self.cp("act", UM[0:64, half * 512:(half + 1) * 512], pum[0:64, :])
                    if d == 1:
                        self.dma(YC[:, :], self.yfd[jc, tb])
                    for ci in range(8):
                        c = ci if d == 0 else 7 - ci
                        ec = c * 64 + 63 if d == 0 else c * 64
                        cs = slice(c * 64, c * 64 + 64)
                        Z = slice(0, 64)
                        pu = self.ps()
                        for e in range(2):
                            E = slice(64 * e, 64 * e + 64)
                            self.mm(pu[Z, E], HA[e][Z, cs], SBF[Z, E], start=True, stop=True)
                        self.cp("act", U[Z, :], pu[Z, 0:128])
                        pp = self.ps()
                        for e in range(2):
                            E = slice(64 * e, 64 * e + 64)
                            co = slice((c * 2 + e) * 64, (c * 2 + e) * 64 + 64)
                            ve = slice(c * 128 + 64 * e, c * 128 + 64 * e + 64)
                            self.mm(pp[Z, E], TTm[Z, co], U[Z, E], start=True, stop=False)
                            self.mm(pp[Z, E], TTm[Z, co], UM[Z, ve], start=False, stop=True)
                        self.cp("dve", Pm[Z, :], pp[Z, 0:128])
                        py = self.ps()
                        for e in range(2):
                            E = slice(64 * e, 64 * e + 64)
                            co = slice((c * 2 + e) * 64, (c * 2 + e) * 64 + 64)
                            ve = slice(c * 128 + 64 * e, c * 128 + 64 * e + 64)
                            self.mm(py[Z, E], SBF[Z, E], HR[e][Z, cs], start=True, stop=False)
                            self.mm(py[Z, E], Pm[Z, E], MRB[Z, co], start=False, stop=False)
                            self.mm(py[Z, E], VTK[Z, ve], MRK[Z, co], start=False, stop=True)
                        for e in range(2):
                            E = slice(64 * e, 64 * e + 64)
                            self.cp("act" if e else "dve", YH[Z, e, cs], py[Z, E])
                        pst = self.ps()
                        for e in range(2):
                            E = slice(64 * e, 64 * e + 64)
                            ve = slice(c * 128 + 64 * e, c * 128 + 64 * e + 64)
                            self.mm(pst[Z, E], BHT[Z, ve], Pm[Z, E], start=True, stop=False)
                            self.mm(pst[Z, E], KHT[Z, ve], VTK[Z, ve], start=False, stop=True)
                        for e in range(2):
                            E = slice(64 * e, 64 * e + 64)
                            self.stt(S32[Z, E], S32[Z, E], HEP[e][Z, ec:ec + 1], pst[Z, E], ALU.mult, ALU.add)
                        self.cp("act", SBF[Z, :], S32[Z, :])
                    pyb = self.ps()
                    for n4 in range(4):
                        ns = slice(n4 * 128, n4 * 128 + 128)
                        self.mm(pyb[:, ns], SH0, YH[0:64, 0, ns], start=True, stop=False)
                        self.mm(pyb[:, ns], SH1, YH[0:64, 1, ns], start=False, stop=True)
                    if d == 0:
                        self.cp("act", YB[:, :], pyb[:, :])
                    else:
                        self.tt("dve", YB[:, :], pyb[:, :], YC[:, :], ALU.add)
                    if RS == 7:
                        return
                    if d == 0:
                        self.dma(self.yfd[jc, tb], YB[:, :])
                    else:
                        ob = OB[bi % 2]
                        pm = self.ps()
                        for n4 in range(4):
                            self.mm(pm[:, n4 * 128:n4 * 128 + 128], BDF, YB[:, n4 * 128:n4 * 128 + 128])
                        self.stt(YC[:, :], pm[:, :], -1.0 / 64.0, YB[:, :], ALU.mult, ALU.add)
                        self.act(EN[:, :], YC[:, :], AF.Square)
                        pv2 = self.ps()
                        for n4 in range(4):
                            self.mm(pv2[:, n4 * 128:n4 * 128 + 128], BDF, EN[:, n4 * 128:n4 * 128 + 128])
                        self.act(EX[:, :], pv2[:, :], AF.Sqrt, bias=DER[:, 87:88], scale=1.0 / 64.0)
                        self.recip(EX[:, :], EX[:, :])
                        self.tt("dve", YC[:, :], YC[:, :], EX[:, :], ALU.mult)
                        self.ts("dve", YC[:, :], YC[:, :], FM[:, FM_LNW + j:FM_LNW + j + 1], FM[:, FM_LNB + j:FM_LNB + j + 1], ALU.mult, ALU.add)
                        self.stt(BT32[:, :], SH[0][:, :], FM[:, FM_RK + j:FM_RK + j + 1], KFIN[:, :], ALU.mult, ALU.mult)
                        pb = self.ps()
                        for n4 in range(4):
                            self.mm(pb[:, n4 * 128:n4 * 128 + 128], BDF, BT32[:, n4 * 128:n4 * 128 + 128])
                        self.tt("dve", KT32[:, :], pb[:, :], Vv[:, :], ALU.mult)
                        self.tt("pool", YC[:, :], YC[:, :], KT32[:, :], ALU.add)
                        pg = self.ps()
                        self.dma(SIGGt[:, :], self.sigg_d[:, tb])
                        self.mm(pg[:, :], GUP[:, jc], SIGGt[:, :])
                        self.tt("dve", ob[:, :], YC[:, :], pg[:, :], ALU.mult)
                        self.dma(self.obT[jc, tb], ob[:, :])
        self.dbg_src = getattr(self, "dbg_src", {})
        if "obT" in self.dbg_out:
            self.dbg_src["obT"] = (self.obT, (slice(None), slice(None)))


    def layernorm_tile(self, H, g, b, OUT, SUM, NM, JUNK):
        self.act(JUNK[:, :], H[:, :], AF.Identity, accum=SUM[:, 0:1])
        self.ts("dve", NM[:, 0:1], SUM[:, 0:1], -1.0 / D, None, ALU.mult)
        self.act(H[:, :], H[:, :], AF.Identity, bias=NM[:, 0:1])
        self.act(JUNK[:, :], H[:, :], AF.Square, accum=SUM[:, 1:2])
        self.act(NM[:, 1:2], SUM[:, 1:2], AF.Sqrt, bias=self.DER[:, 86:87], scale=1.0 / D)
        self.recip(NM[:, 1:2], NM[:, 1:2])
        self.stt(OUT[:, :], H[:, :], NM[:, 1:2], g, ALU.mult, ALU.mult)
        self.tt("pool", OUT[:, :], OUT[:, :], b, ALU.add)

    def merge(self, l, src):
        sb = self.sb
        CST, XT = self.CST, self.XT
        es_keep = self.es
        with contextlib.ExitStack() as es1:
            self.es = es1
            WS = [sb(f"m_ws{i}", [128, 8, 256], F32) for i in range(2)]
            PA = sb("m_pa", [128, 8, D], BF16)
            PB = sb("m_pb", [128, 8, D], BF16)
            WG = sb("m_wg", [128, 8, 2 * D], BF16)
            k = 0
            for q in range(4):
                self.load_w(WS[k % 2], PA, self.proj_a, l, q * 256, 256, q * 256, "pool" if k % 2 else "dve"); k += 1
                self.load_w(WS[k % 2], PB, self.proj_b, l, q * 256, 256, q * 256, "pool" if k % 2 else "dve"); k += 1
            for q in range(8):
                self.load_w(WS[k % 2], WG, self.w_in, l, GAB + q * 256, 256, q * 256, "pool" if k % 2 else "dve"); k += 1
            OA = sb("m_oa", [128, 8, 512], BF16)
            OBt = sb("m_ob", [128, 8, 512], BF16)
            SA, SB_, M1t, M2t = [sb("m_" + n, [128, 512], F32) for n in ("sa", "sb", "m1", "m2")]
            MG = [sb(f"m_mg{i}", [128, 512], BF16) for i in range(2)]
            for blk in range(NB):
                t0 = blk * 512
                self.dma(OA[:, :, :], V(self.oaT, self.oaT.h[:, t0:t0 + 512].rearrange("(c p) t -> p c t", p=128)))
                self.dma(OBt[:, :, :], V(self.obT, self.obT.h[:, t0:t0 + 512].rearrange("(c p) t -> p c t", p=128)))
                for j in range(8):
                    js = slice(j * 128, j * 128 + 128)
                    pA = self.ps()
                    for c in range(8):
                        self.mm(pA[:, :], PA[:, c, js], OA[:, c, :], start=(c == 0), stop=(c == 7))
                    pB = self.ps()
                    for c in range(8):
                        self.mm(pB[:, :], PB[:, c, js], OBt[:, c, :], start=(c == 0), stop=(c == 7))
                    pga = self.ps()
                    self.proj_fm(pga[:, :], WG, j * 128, 128, t0, 512)
                    pgb = self.ps()
                    self.proj_fm(pgb[:, :], WG, D + j * 128, 128, t0, 512)
                    self.act(SA[:, :], pga[:, :], AF.Sigmoid)
                    self.act(SB_[:, :], pgb[:, :], AF.Sigmoid)
                    self.tt("dve", M1t[:, :], pA[:, :], SA[:, :], ALU.mult)
                    self.tt("dve", M2t[:, :], pB[:, :], SB_[:, :], ALU.mult)
                    mg = MG[j % 2]
                    self.tt("pool", mg[:, :], M1t[:, :], M2t[:, :], ALU.add)
                    self.dma(self.mgT[js, t0:t0 + 512], mg[:, :])
        self.es = es_keep
        self.P.barrier()
        with contextlib.ExitStack() as es2:
            self.es = es2
            WS = [sb(f"m2_ws{i}", [128, 8, 256], F32) for i in range(2)]
            WO = sb("m2_wo", [128, 8, D], BF16)
            for q in range(4):
                self.load_w(WS[q % 2], WO, self.w_out, l, q * 256, 256, q * 256, "pool" if q % 2 else "dve")
            BC = sb("m2_bc", [128, 2 * D], F32)
            self.dma(BC[:, :], self.bc_pack.h[l][:, 0:2 * D] if False else V(self.bc_pack, self.bc_pack.h[l][:, 0:2 * D]))
            MGt = [sb(f"m2_mg{i}", [128, 8, 128], BF16) for i in range(2)]
            XR = [sb(f"m2_xr{i}", [128, D], F32) for i in range(2)]
            H = sb("m2_h", [128, D], F32)
            JUNK = sb("m2_junk", [128, D], F32)
            X1 = [sb(f"m2_x1{i}", [128, D], F32) for i in range(2)]
            SUM = sb("m2_sum", [128, 2], F32)
            NM = sb("m2_nm", [128, 2], F32)
            for i in range(T // 128):
                ts_ = slice(i * 128, i * 128 + 128)
                mgt, xr, x1 = MGt[i % 2], XR[i % 2], X1[i % 2]
                self.dma(mgt[:, :, :], V(self.mgT, self.mgT.h[:, ts_].rearrange("(c p) t -> p c t", p=128)))
                self.dma(xr[:, :], src[ts_, :])
                for half in range(2):
                    hs = slice(half * 512, half * 512 + 512)
                    ph = self.ps()
                    for c in range(8):
                        self.mm(ph[:, :], mgt[:, c, :], WO[:, c, hs], start=(c == 0), stop=(c == 7))
                    self.stt(H[:, hs], xr[:, hs], DN_ALPHA, ph[:, :], ALU.mult, ALU.add)
                self.layernorm_tile(H, BC[:, 0:D], BC[:, D:2 * D], x1, SUM, NM, JUNK)
                self.dma(self.x1d[ts_, :], x1[:, :])
                for half in range(2):
                    p = self.ps()
                    for c in range(4):
                        cc = half * 4 + c
                        self.mm(p[:, c * 128:(c + 1) * 128], x1[:, cc * 128:(cc + 1) * 128], CST[:, C_ID:C_ID + 128])
                    for c in range(4):
                        cc = half * 4 + c
                        self.cp("act" if c % 2 else "dve", XT[:, cc, ts_], p[:, c * 128:(c + 1) * 128])
        self.es = es_keep
        self.dbg_src = getattr(self, "dbg_src", {})
        if "x1" in self.dbg_out:
            self.dbg_src["x1"] = (self.x1d, (slice(None), slice(None)))

    def moe(self, l, dst):
        sb = self.sb
        CST, XT = self.CST, self.XT
        TP = 1024
        NTB = TP // 512
        NTL = TP // 128
        RWS = sb("e_rws", [128, 8, NE], F32)
        RW = sb("e_rw", [128, 8, NE], BF16)
        self.dma(RWS[:, :, :], V(self.router_w, self.router_w.h[l].rearrange("(c p) n -> p c n", p=128)))
        self.cp("dve", RW[:, :, :], RWS[:, :, :])
        BC = sb("e_bc", [128, 2 * D + NE], F32)
        self.dma(BC[:, :], V(self.bc_pack, self.bc_pack.h[l][:, 2 * D:4 * D + NE]))
        B1 = sb("e_b1", [128, NE * 16], F32)
        self.dma(B1[:, :], self.b1_pack[l])
        B2S = sb("e_b2s", [NE, D], F32)
        B2 = sb("e_b2", [NE, D], BF16)
        self.dma(B2S[:, :], self.moe_b2[l])
        self.cp("dve", B2[:, :], B2S[:, :])
        GT = sb("e_gt", [128, T // 128, NE], F32)
        GTT = sb("e_gtt", [NE, T], BF16)
        LG, EXv, MSK = [sb("e_" + n, [128, NE], F32) for n in ("lg", "ex", "msk")]
        M8 = sb("e_m8", [128, 8], F32)
        SS = sb("e_ss", [128, 2], F32)
        for i in range(T // 128):
            ts_ = slice(i * 128, i * 128 + 128)
            pl = self.ps()
            for c in range(8):
                self.mm(pl[:, 0:NE], XT[:, c, ts_], RW[:, c, :], start=(c == 0), stop=(c == 7))
            self.tt("dve", LG[:, :], pl[:, 0:NE], BC[:, 2 * D:2 * D + NE], ALU.add)
            lg_ap, m8_ap = LG[:, :].ap, M8[:, :].ap
            self.P.op("dve", lambda e, o=m8_ap, i_=lg_ap: e.max(out=o, in_=i_), reads=[LG.b], writes=[M8.b])
            self.ts("dve", SS[:, 0:1], M8[:, 0:1], -1.0, None, ALU.mult)
            self.act(EXv[:, :], LG[:, :], AF.Exp, bias=SS[:, 0:1])
            self.ts("dve", MSK[:, :], LG[:, :], M8[:, 3:4], None, ALU.is_ge)
            self.tt("dve", EXv[:, :], EXv[:, :], MSK[:, :], ALU.mult)
            self.act(MSK[:, :], EXv[:, :], AF.Identity, accum=SS[:, 1:2])
            self.recip(SS[:, 1:2], SS[:, 1:2])
            self.ts("dve", GT[:, i, :], EXv[:, :], SS[:, 1:2], None, ALU.mult)
            pt = self.ps()
            self.mm(pt[0:NE, 0:128], GT[:, i, :], CST[:, C_ID:C_ID + 128])
            self.cp("act", GTT[0:NE, ts_], pt[0:NE, 0:128])
        ACC = sb("e_acc", [128, NTL, D], F32)
        ACTT = sb("e_actt", [128, 8, TP], BF16)
        W1S = [sb("e_w1s0", [128, 8, 256], F32)] * 2
        W1B = [sb(f"e_w1b{i}", [128, 8, 256], BF16) for i in range(2)]
        W2S = [sb(f"e_w2s{i}", [128, 512], F32) for i in range(2)]
        W2B = sb("e_w2b", [128, 8, 512], BF16)
        GLU, SIG, LIN = [sb("e_" + n, [128, 512], F32) for n in ("glu", "sig", "lin")]
        XR = sb("e_xr", [128, D], F32)
        H = sb("e_h", [128, D], F32)
        JUNK = XR
        SUM = sb("e_sum", [128, 2], F32)
        NM = sb("e_nm", [128, 2], F32)
        wk = 0
        for ps_i in range(T // TP):
            tp0 = ps_i * TP
            for tl in range(NTL):
                tsl = slice(tp0 + tl * 128, tp0 + tl * 128 + 128)
                for half in range(2):
                    hs = slice(half * 512, half * 512 + 512)
                    pb = self.ps()
                    self.mm(pb[:, :], GTT[0:NE, tsl], B2[0:NE, hs])
                    self.cp("act", ACC[:, tl, hs], pb[:, :])
            for e in range(NE):
                for ft in range(8):
                    w1s, w1b = W1S[wk % 2], W1B[wk % 2]
                    wk += 1
                    src_ap = self.moe_w1.h[l][e][:, ft * 256:(ft + 1) * 256].rearrange("(c p) n -> p c n", p=128)
                    self.dma(w1s[:, :, :], V(self.moe_w1, src_ap))
                    de = w1s.h[:, :, :].rearrange("p c (f two) -> p c f two", two=2)
                    self.cp("pool", w1b[:, :, 0:128], V(w1s, de[:, :, :, 0]))
                    self.cp("pool" if ft % 2 else "dve", w1b[:, :, 128:256], V(w1s, de[:, :, :, 1]))
                    for tb in range(NTB):
                        t0 = tp0 + tb * 512
                        pg = self.ps()
                        self.proj_fm(pg[:, :], w1b, 0, 128, t0, 512)
                        pl = self.ps()
                        self.proj_fm(pl[:, :], w1b, 128, 128, t0, 512)
                        bg = B1[:, e * 16 + ft:e * 16 + ft + 1]
                        bl = B1[:, e * 16 + 8 + ft:e * 16 + 8 + ft + 1]
                        self.ts("dve", GLU[:, :], pg[:, :], bg, SWIGLU_LIMIT, ALU.add, ALU.min)
                        self.act(SIG[:, :], GLU[:, :], AF.Sigmoid, scale=SWIGLU_ALPHA)
                        self.ts("dve", LIN[:, :], pl[:, :], bl, SWIGLU_LIMIT, ALU.add, ALU.min)
                        self.ts("pool", LIN[:, :], LIN[:, :], -SWIGLU_LIMIT, 1.0, ALU.max, ALU.add)
                        self.tt("pool", GLU[:, :], GLU[:, :], SIG[:, :], ALU.mult)
                        self.tt("dve", ACTT[:, ft, tb * 512:(tb + 1) * 512], GLU[:, :], LIN[:, :], ALU.mult)
                for half in range(2):
                    hs = slice(half * 512, half * 512 + 512)
                    for ft in range(8):
                        w2s = W2S[ft % 2]
                        self.dma(w2s[:, :], V(self.moe_w2, self.moe_w2.h[l][e][ft * 128:(ft + 1) * 128, hs]))
                        self.cp("act" if ft % 2 else "pool", W2B[:, ft, :], w2s[:, :])
                    for tl in range(NTL):
                        gi = (tp0 // 128) + tl
                        py = self.ps()
                        for ft in range(8):
                            self.mm(py[:, :], ACTT[:, ft, tl * 128:(tl + 1) * 128], W2B[:, ft, :], start=(ft == 0), stop=(ft == 7))
                        self.stt(ACC[:, tl, hs], py[:, :], GT[:, gi, e:e + 1], ACC[:, tl, hs], ALU.mult, ALU.add)
            for tl in range(NTL):
                tsl = slice(tp0 + tl * 128, tp0 + tl * 128 + 128)
                self.dma(XR[:, :], self.x1d[tsl, :])
                self.stt(H[:, :], XR[:, :], DN_ALPHA, ACC[:, tl, :], ALU.mult, ALU.add)
                self.layernorm_tile(H, BC[:, 0:D], BC[:, D:2 * D], XR, SUM, NM, JUNK)
                self.out_evs.append(self.dma(dst[tsl, :], XR[:, :]))
        self.dbg_src = getattr(self, "dbg_src", {})
        if "x2" in self.dbg_out:
            self.dbg_src["x2"] = (dst, (slice(None), slice(None)))


def make_consts():
    c = np.zeros((128, NCONST), np.float32)
    c[:, C_ID:C_ID + 128] = np.eye(128, dtype=np.float32)
    i = np.arange(64)
    fi = (i[:, None] <= i[None, :]).astype(np.float32)
    fs = (i[:, None] < i[None, :]).astype(np.float32)
    bi = (i[:, None] >= i[None, :]).astype(np.float32)
    bs = (i[:, None] > i[None, :]).astype(np.float32)
    for base, m in ((C_FI, fi), (C_FS, fs), (C_BI, bi), (C_BS, bs)):
        c[0:64, base:base + 512] = np.tile(m, (1, 8))
    r = np.ones(512, np.float32)
    r[::64] = 0.0
    c[:, C_RESET:C_RESET + 512] = r[None, :]
    bd = np.zeros((128, 128), np.float32)
    bd[0:64, 0:64] = 1.0
    bd[64:128, 64:128] = 1.0
    c[:, C_BD:C_BD + 128] = bd
    c[:, C_ONES:C_ONES + 128] = 1.0
    c[0:64, C_I8:C_I8 + 512] = np.tile(np.eye(64, dtype=np.float32), (1, 8))
    c[0:64, C_SH1 + 64:C_SH1 + 128] = np.eye(64, dtype=np.float32)
    return c


def fmcols(v):
    v = np.asarray(v, np.float32).reshape(-1)
    n = (v.size + 127) // 128
    p = np.zeros(n * 128, np.float32)
    p[:v.size] = v
    return p.reshape(n, 128).T


def make_packs(inp):
    fm = np.zeros((L_ALL, 128, NFM), np.float32)
    bc = np.zeros((L_ALL, 128, 4 * D + NE), np.float32)
    b1 = np.zeros((L_ALL, 128, NE * 16), np.float32)
    for l in range(L_ALL):
        mu = np.asarray(inp["rw_mu"][l], np.float32)
        fm[l, :, FM_MU:FM_MU + 24] = fmcols(mu[0:3072])
        fm[l, :, FM_MU + 24] = mu[3072:3200]
        fm[l, 0:64, FM_MUB] = mu[3136:3200]
        fm[l, 0:64, FM_MU + 25] = mu[3200:3264]
        fm[l, :, FM_MU + 26] = mu[3264:3392]
        fm[l, :, FM_W0:FM_W0 + 16] = fmcols(inp["rw_w0"][l])
        fm[l, :, FM_A0:FM_A0 + 8] = fmcols(inp["rw_a0"][l])
        fm[l, :, FM_KK:FM_KK + 8] = fmcols(inp["rw_k_k"][l])
        fm[l, :, FM_KA:FM_KA + 8] = fmcols(inp["rw_k_a"][l])
        fm[l, :, FM_RK:FM_RK + 8] = fmcols(inp["rw_r_k"][l])
        fm[l, :, FM_LNW:FM_LNW + 8] = fmcols(inp["rw_lnx_w"][l])
        fm[l, :, FM_LNB:FM_LNB + 8] = fmcols(inp["rw_lnx_b"][l])
        if l > 0:
            fm[l, :, FM_V0:FM_V0 + 8] = fmcols(inp["rw_v0"][l - 1])
        for i in range(L_ALL):
            fm[l, :, FM_LBL + 8 * i:FM_LBL + 8 * i + 8] = fmcols(inp["hg_lb_logits"][i])
        fm[l, :, FM_NW] = inp["hg_norm_w"][l]
        bc[l, :, 0:D] = inp["ln1_g"][l][None, :]
        bc[l, :, D:2 * D] = inp["ln1_b"][l][None, :]
        bc[l, :, 2 * D:3 * D] = inp["ln2_g"][l][None, :]
        bc[l, :, 3 * D:4 * D] = inp["ln2_b"][l][None, :]
        bc[l, :, 4 * D:] = inp["router_b"][l][None, :]
        bb = np.asarray(inp["moe_b1"][l], np.float32)
        glu = bb[:, 0::2].reshape(NE, 8, 128)
        lin = bb[:, 1::2].reshape(NE, 8, 128)
        pk = np.concatenate([glu, lin], axis=1)
        b1[l] = pk.transpose(2, 0, 1).reshape(128, NE * 16)
    return fm, bc, b1


_CACHE = {}


def run(inputs, n_layers=L_ALL, stop=None, dbg=(), n_cores=4):
    key = (n_layers, stop, tuple(dbg))
    bld = Builder(n_layers=n_layers, stop=stop, dbg=dbg)
    nc = bld.build()
    inp = {k: np.asarray(v) for k, v in inputs.items()}
    fm, bc, b1 = make_packs(inp)
    shared = {
        "w_in": np.ascontiguousarray(inp["w_in"], np.float32),
        "rw_w_up": np.ascontiguousarray(inp["rw_w_up"], np.float32).reshape(L_ALL, 128, D),
        "rw_a_up": np.ascontiguousarray(inp["rw_a_up"], np.float32),
        "rw_g_up": np.ascontiguousarray(inp["rw_g_up"], np.float32),
        "rw_v_down": np.ascontiguousarray(inp["rw_v_down"], np.float32),
        "rw_v_up": np.ascontiguousarray(inp["rw_v_up"], np.float32),
        "proj_a": np.ascontiguousarray(inp["proj_a"], np.float32),
        "proj_b": np.ascontiguousarray(inp["proj_b"], np.float32),
        "w_out": np.ascontiguousarray(inp["w_out"], np.float32),
        "router_w": np.ascontiguousarray(inp["router_w"], np.float32),
        "fm_pack": fm, "bc_pack": bc, "b1_pack": b1, "consts": make_consts(),
    }
    if bld.need_moe:
        for k in ("moe_w1", "moe_w2", "moe_b2"):
            shared[k] = np.ascontiguousarray(inp[k], np.float32)
    in_maps = []
    for c in range(n_cores):
        m = dict(shared)
        m["x"] = np.ascontiguousarray(inp["x"][c % 4], np.float32)
        in_maps.append(m)
    res = run_bass_kernel_spmd(nc, in_maps, core_ids=list(range(n_cores)))
    return res


def kernel(**inputs):
    res = run(inputs)
    out = np.stack([np.asarray(res.results[c]["y"], np.float32) for c in range(4)], axis=0)
    return out
```

```python
import contextlib
import os
import numpy as np
import concourse.bass as bass
import concourse.mybir as mybir
from concourse.bass_utils import run_bass_kernel_spmd

F32 = mybir.dt.float32
BF16 = mybir.dt.bfloat16
AF = mybir.ActivationFunctionType
ALU = mybir.AluOpType

L_ALL = 4
D = 1024
T = 4096
NB = 8
NCOLS = 10560
RWB = 5120
GAB = RWB + 3392
GBB = GAB + 1024
DN_ALPHA = (2 * L_ALL) ** 0.25
NORM_EPS = 1e-5
RW_LN_EPS = 64e-5
NE = 32
C_W = float(np.exp(-0.5))
SWIGLU_ALPHA = 1.702
SWIGLU_LIMIT = 7.0

FM_MU = 0
FM_W0 = 27
FM_A0 = 43
FM_KK = 51
FM_KA = 59
FM_RK = 67
FM_LNW = 75
FM_LNB = 83
FM_V0 = 91
FM_LBL = 99
FM_NW = 131
FM_MUB = 132
NFM = 133
C_ID = 0
C_FI = 128
C_FS = 640
C_BI = 1152
C_BS = 1664
C_RESET = 2176
C_BD = 2688
C_ONES = 2816
C_I8 = 2944
C_SH1 = 3456
NCONST = 3584


class Buf:
    __slots__ = ("w", "r")

    def __init__(self):
        self.w = {}
        self.r = {}


class Prog:
    NDMA = 8

    def __init__(self):
        self.engs = ("pe", "dve", "act", "pool", "sp")
        self.q = {e: [] for e in self.engs}
        self.cnt = {}
        self.seen = {e: {} for e in self.engs}
        self.dma_slot = {e: 0 for e in self.engs}
        self.sem_keys = ["pe", "dve", "act", "pool"]
        for e in ("sp", "pool", "act"):
            for i in range(self.NDMA):
                self.sem_keys.append(f"d_{e}_{i}")
        for k in self.sem_keys:
            self.cnt[k] = 0

    def _deps(self, eng, reads, writes):
        need = {}
        for b in reads:
            for k, v in b.w.items():
                if need.get(k, 0) < v:
                    need[k] = v
        for b in writes:
            for k, v in b.w.items():
                if need.get(k, 0) < v:
                    need[k] = v
            for k, v in b.r.items():
                if need.get(k, 0) < v:
                    need[k] = v
        waits = []
        seen = self.seen[eng]
        for k, v in need.items():
            if k == "pe" and eng == "pe":
                continue
            if seen.get(k, 0) < v:
                seen[k] = v
                waits.append((k, v))
        return waits

    @staticmethod
    def _mark(ev, reads, writes):
        k, v = ev
        for b in writes:
            b.w[k] = v
        for b in reads:
            b.r[k] = v

    def op(self, eng, fn, reads=(), writes=()):
        self.total = getattr(self, "total", 0) + 1
        if self.total > int(os.environ.get("OPLIM", "1000000000")):
            return None
        waits = self._deps(eng, reads, writes)
        self.cnt[eng] += 1
        ev = (eng, self.cnt[eng])
        self.q[eng].append((fn, waits, (eng, 1)))
        self._mark(ev, reads, writes)
        return ev

    def dma(self, eng, fn, reads=(), writes=()):
        self.total = getattr(self, "total", 0) + 1
        if self.total > int(os.environ.get("OPLIM", "1000000000")):
            return None
        slot = self.dma_slot[eng]
        self.dma_slot[eng] = (slot + 1) % self.NDMA
        key = f"d_{eng}_{slot}"
        waits = self._deps(eng, reads, writes)
        prev = self.cnt[key]
        if prev > 0 and self.seen[eng].get(key, 0) < prev:
            self.seen[eng][key] = prev
            waits.append((key, prev))
        self.cnt[key] += 16
        ev = (key, self.cnt[key])
        self.q[eng].append((fn, waits, (key, 16)))
        self._mark(ev, reads, writes)
        return ev

    def barrier(self):
        snap = dict(self.cnt)
        for e in self.engs:
            waits = []
            for k, v in snap.items():
                if v > 0 and self.seen[e].get(k, 0) < v and not (k == e == "pe"):
                    self.seen[e][k] = v
                    waits.append((k, v))
            if waits:
                self.q[e].append((None, waits, None))

    def emit(self, block, sems):
        decos = {"pe": block.tensor, "dve": block.vector, "act": block.scalar,
                 "pool": block.gpsimd, "sp": block.sync}

        def make(engname):
            ops = self.q[engname]

            def body(e):
                for fn, waits, inc in ops:
                    for k, v in waits:
                        e.wait_ge(sems[k], v)
                    if fn is not None:
                        fn(e).then_inc(sems[inc[0]], inc[1])
            return body
        for engname, deco in decos.items():
            deco(make(engname))


class V:
    __slots__ = ("t", "ap")

    def __init__(self, t, ap):
        self.t = t
        self.ap = ap


class TT:
    def __init__(self, h):
        self.h = h
        self.b = Buf()

    def __getitem__(self, idx):
        return V(self, self.h[idx])


class _Half:
    __slots__ = ("b",)

    def __init__(self):
        self.b = Buf()


class TTH:
    def __init__(self, h):
        self.h = h
        self.halves = [_Half(), _Half()]

    def __getitem__(self, idx):
        cols = idx[1]
        start = cols.start or 0
        stop = cols.stop
        half = start // 512
        assert (stop - 1) // 512 == half, (start, stop)
        return V(self.halves[half], self.h[idx])


class Builder:
    def __init__(self, n_layers=L_ALL, stop=None, dbg=()):
        self.n_layers = n_layers
        self.stop = stop
        self.dbg = dbg
        self.nc = bass.Bass("TRN2", target_bir_lowering=False)
        self.P = Prog()
        self.es = contextlib.ExitStack()
        self.psi = 0
        self.dq = 0
        self.out_evs = []

    def sb(self, name, shape, dt):
        self.uid = getattr(self, "uid", 0) + 1
        return TT(self.es.enter_context(self.nc.sbuf_tensor(f"{name}_{self.uid}", list(shape), dt)))

    def dram_in(self, name, shape, dt=F32):
        return TT(self.nc.dram_tensor(name, list(shape), dt, kind="ExternalInput").ap())

    def dram_out(self, name, shape, dt=F32):
        return TT(self.nc.dram_tensor(name, list(shape), dt, kind="ExternalOutput").ap())

    def dram_tmp(self, name, shape, dt):
        return TT(self.nc.dram_tensor(name, list(shape), dt).ap())

    def ps(self, hold=False):
        while True:
            t = self.psb[self.psi]
            self.psi = (self.psi + 1) % len(self.psb)
            if not getattr(t, "held", False):
                break
        t.held = hold
        return t

    def mm(self, out, lhsT, rhs, start=True, stop=True):
        o, a, b = out.ap, lhsT.ap, rhs.ap
        self.P.op("pe", lambda e: e.matmul(o, lhsT=a, rhs=b, start=start, stop=stop),
                  reads=[lhsT.t.b, rhs.t.b], writes=[out.t.b])

    def act(self, out, in_, func, bias=None, scale=1.0, accum=None):
        o, i = out.ap, in_.ap
        reads = [in_.t.b]
        kw = {}
        if isinstance(bias, V):
            reads.append(bias.t.b)
            kw["bias"] = bias.ap
        elif bias is not None:
            kw["bias"] = bias
        if isinstance(scale, V):
            reads.append(scale.t.b)
            kw["scale"] = scale.ap
        else:
            kw["scale"] = scale
        writes = [out.t.b]
        if accum is not None:
            kw["accum_out"] = accum.ap
            writes.append(accum.t.b)
        self.P.op("act", lambda e: e.activation(out=o, in_=i, func=func, **kw), reads=reads, writes=writes)

    def ts(self, eng, out, in0, s1, s2=None, op0=ALU.mult, op1=None, accum=None):
        o, i = out.ap, in0.ap
        reads = [in0.t.b]
        a1 = s1
        if isinstance(s1, V):
            reads.append(s1.t.b)
            a1 = s1.ap
        a2 = s2
        if isinstance(s2, V):
            reads.append(s2.t.b)
            a2 = s2.ap
        kw = {}
        if op1 is not None:
            kw["op1"] = op1
        writes = [out.t.b]
        if accum is not None:
            kw["accum_out"] = accum.ap
            writes.append(accum.t.b)
        self.P.op(eng, lambda e: e.tensor_scalar(out=o, in0=i, scalar1=a1, scalar2=a2, op0=op0, **kw),
                  reads=reads, writes=writes)

    def tt(self, eng, out, in0, in1, op):
        o, a, b = out.ap, in0.ap, in1.ap
        self.P.op(eng, lambda e: e.tensor_tensor(out=o, in0=a, in1=b, op=op),
                  reads=[in0.t.b, in1.t.b], writes=[out.t.b])

    def stt(self, out, in0, scalar, in1, op0, op1):
        o, a, b = out.ap, in0.ap, in1.ap
        reads = [in0.t.b, in1.t.b]
        s = scalar
        if isinstance(scalar, V):
            reads.append(scalar.t.b)
            s = scalar.ap
        self.P.op("dve", lambda e: e.scalar_tensor_tensor(out=o, in0=a, scalar=s, in1=b, op0=op0, op1=op1),
                  reads=reads, writes=[out.t.b])

    def cp(self, eng, out, in_):
        o, i = out.ap, in_.ap
        if eng == "act":
            self.P.op("act", lambda e: e.activation(out=o, in_=i, func=AF.Copy), reads=[in_.t.b], writes=[out.t.b])
        else:
            self.P.op(eng, lambda e: e.tensor_copy(out=o, in_=i), reads=[in_.t.b], writes=[out.t.b])

    def memset(self, eng, out, val):
        o = out.ap
        self.P.op(eng, lambda e: e.memset(o, val), writes=[out.t.b])

    def scan(self, out, d0, d1):
        o, a, b = out.ap, d0.ap, d1.ap
        self.P.op("dve", lambda e: e.tensor_tensor_scan(out=o, data0=a, data1=b, initial=0.0, op0=ALU.mult, op1=ALU.add),
                  reads=[d0.t.b, d1.t.b], writes=[out.t.b])

    def recip(self, out, in_):
        o, i = out.ap, in_.ap
        self.P.op("dve", lambda e: e.reciprocal(out=o, in_=i), reads=[in_.t.b], writes=[out.t.b])

    def dma(self, out, in_, eng=None):
        if eng is None:
            eng = ("sp", "pool")[self.dq % 2]
            self.dq += 1
        o, i = out.ap, in_.ap
        return self.P.dma(eng, lambda e: e.dma_start(out=o, in_=i), reads=[in_.t.b], writes=[out.t.b])

    def build(self):
        nc = self.nc
        n_layers = self.n_layers
        self.x_in = self.dram_in("x", [T, D])
        self.w_in = self.dram_in("w_in", [L_ALL, D, NCOLS])
        self.w_up = self.dram_in("rw_w_up", [L_ALL, 128, D])
        self.a_up = self.dram_in("rw_a_up", [L_ALL, 64, D])
        self.g_up = self.dram_in("rw_g_up", [L_ALL, 128, D])
        self.v_down = self.dram_in("rw_v_down", [L_ALL - 1, D, 32])
        self.v_up = self.dram_in("rw_v_up", [L_ALL - 1, 32, D])
        self.proj_a = self.dram_in("proj_a", [L_ALL, D, D])
        self.proj_b = self.dram_in("proj_b", [L_ALL, D, D])
        self.w_out = self.dram_in("w_out", [L_ALL, D, D])
        self.router_w = self.dram_in("router_w", [L_ALL, D, NE])
        self.need_moe = self.stop is None or self.stop[1] == "moe"
        if self.need_moe:
            self.moe_w1 = self.dram_in("moe_w1", [L_ALL, NE, D, 2 * D])
            self.moe_w2 = self.dram_in("moe_w2", [L_ALL, NE, D, D])
            self.moe_b2 = self.dram_in("moe_b2", [L_ALL, NE, D])
        self.fm_pack = self.dram_in("fm_pack", [L_ALL, 128, NFM])
        self.bc_pack = self.dram_in("bc_pack", [L_ALL, 128, 4 * D + NE])
        self.b1_pack = self.dram_in("b1_pack", [L_ALL, 128, NE * 16])
        self.consts_d = self.dram_in("consts", [128, NCONST])
        self.y_out = self.dram_out("y", [T, D])
        self.xres = [self.dram_tmp("xres0", [T, D], F32), self.dram_tmp("xres1", [T, D], F32)]
        self.x1d = self.dram_tmp("x1d", [T, D], F32)
        self.oaT = self.dram_tmp("oaT", [D, T], BF16)
        self.obT = self.dram_tmp("obT", [D, T], BF16)
        self.mgT = self.dram_tmp("mgT", [D, T], BF16)
        self.vfT = self.dram_tmp("vfT", [D, T], F32)
        self.yfd = self.dram_tmp("yfd", [D, T], F32)
        self.twl_d = [self.dram_tmp("twlf_d", [64, T], BF16), self.dram_tmp("twlb_d", [64, T], BF16)]
        self.alo_d = self.dram_tmp("alo_d", [64, T], BF16)
        self.sigg_d = self.dram_tmp("sigg_d", [128, T], BF16)
        self.xvd_d = self.dram_tmp("xvd_d", [32, T], BF16)
        self.dbg_out = {}
        for name, shape, dt in self.dbg:
            self.dbg_out[name] = self.dram_out("dbg_" + name, shape, dt)

        with self.es:
            sems = {}
            for k in self.P.sem_keys:
                sems[k] = self.es.enter_context(nc.semaphore("s_" + k))
            self.psb = [TT(self.es.enter_context(nc.psum_tensor(f"ps{i}", [128, 512], F32))) for i in range(8)]
            self.XT = self.sb("XT", [128, 8, T], BF16)
            self.CST = self.sb("CST", [128, NCONST], F32)
            self.IDB = self.sb("IDB", [128, 128], BF16)
            self.BDB = self.sb("BDB", [128, 128], BF16)
            self.ONB = self.sb("ONB", [128, 128], BF16)
            self.FM = self.sb("FM", [128, NFM], F32)
            self.DER = self.sb("DER", [128, 96], F32)
            self.dma(self.CST[:, :], self.consts_d[:, :])
            self.cp("dve", self.IDB[:, :], self.CST[:, C_ID:C_ID + 128])
            self.cp("dve", self.BDB[:, :], self.CST[:, C_BD:C_BD + 128])
            self.cp("dve", self.ONB[:, :], self.CST[:, C_ONES:C_ONES + 128])

            for l in range(n_layers):
                src = self.x_in if l == 0 else self.xres[l % 2]
                dst = self.y_out if l == L_ALL - 1 else self.xres[(l + 1) % 2]
                self.layer(l, src, dst)
                if self.stop is not None and self.stop[0] == l:
                    break
            final_evs = []
            for name, (srcT, sl) in getattr(self, "dbg_src", {}).items():
                final_evs.append(self.dma(self.dbg_out[name][sl], srcT[sl], eng="sp"))
            self.P.barrier()
            with nc.Block() as block:
                self.P.emit(block, sems)
        return nc

    def params(self, l):
        self.dma(self.FM[:, :], self.fm_pack[l])
        DER, FM = self.DER, self.FM
        self.ts("dve", DER[:, 0:27], FM[:, FM_MU:FM_MU + 27], -1.0, 1.0, ALU.mult, ALU.add)
        self.ts("dve", DER[:, 27:54], FM[:, FM_MU:FM_MU + 27], 0.5, None, ALU.mult)
        self.ts("dve", DER[:, 54:62], FM[:, FM_KA:FM_KA + 8], -1.0, 1.0, ALU.mult, ALU.add)
        E = self.sb(f"lbE{l}", [128, 32], F32)
        self.act(E[:, :], FM[:, FM_LBL:FM_LBL + 32], AF.Exp)
        self.tt("dve", DER[:, 78:86], E[:, 0:8], E[:, 8:16], ALU.add)
        self.tt("dve", DER[:, 78:86], DER[:, 78:86], E[:, 16:24], ALU.add)
        self.tt("dve", DER[:, 78:86], DER[:, 78:86], E[:, 24:32], ALU.add)
        self.recip(DER[:, 78:86], DER[:, 78:86])
        self.memset("dve", DER[:, 62:70], 0.0)
        for i in range(1, l + 1):
            self.tt("dve", DER[:, 62:70], DER[:, 62:70], E[:, 8 * i:8 * i + 8], ALU.add)
        self.tt("dve", DER[:, 62:70], DER[:, 62:70], DER[:, 78:86], ALU.mult)
        self.ts("dve", DER[:, 70:78], DER[:, 62:70], -1.0, 1.0, ALU.mult, ALU.add)
        self.ts("dve", DER[:, 88:89], FM[:, FM_MUB:FM_MUB + 1], -1.0, 1.0, ALU.mult, ALU.add)
        self.ts("dve", DER[:, 89:90], FM[:, FM_MUB:FM_MUB + 1], 0.5, None, ALU.mult)
        self.memset("dve", DER[:, 86:87], NORM_EPS)
        self.memset("dve", DER[:, 87:88], RW_LN_EPS)

    def load_w(self, stage, wbf, src_tt, l, col0, n, dst_col, conv_eng):
        src = src_tt.h[l][:, col0:col0 + n].rearrange("(c p) n -> p c n", p=128)
        self.dma(stage[:, :, 0:n], V(src_tt, src))
        self.cp(conv_eng, wbf[:, :, dst_col:dst_col + n], stage[:, :, 0:n])

    def proj_fm(self, out, wbf, col0, m, t0, n, xt=None):
        xt = xt or self.XT
        for c in range(8):
            self.mm(out, wbf[:, c, col0:col0 + m], xt[:, c, t0:t0 + n], start=(c == 0), stop=(c == 7))

    def build_xt(self, src):
        XL = [self.sb(f"XL{i}_{id(src) % 1000}", [128, D], F32) for i in range(2)]
        for i in range(T // 128):
            xl = XL[i % 2]
            self.dma(xl[:, :], src[i * 128:(i + 1) * 128, :])
            for half in range(2):
                p = self.ps()
                for c in range(4):
                    cc = half * 4 + c
                    self.mm(p[:, c * 128:(c + 1) * 128], xl[:, cc * 128:(cc + 1) * 128], self.CST[:, C_ID:C_ID + 128])
                for c in range(4):
                    cc = half * 4 + c
                    self.cp("act" if c % 2 else "dve", self.XT[:, cc, i * 128:(i + 1) * 128], p[:, c * 128:(c + 1) * 128])

    def layer(self, l, src, dst):
        es_outer = self.es
        self.params(l)
        with contextlib.ExitStack() as es:
            self.es = es
            self.build_xt(src)
        self.es = es_outer
        self.P.barrier()
        if self.stop == (l, "xt"):
            return
        with contextlib.ExitStack() as es:
            self.es = es
            self.hgrn(l)
        self.es = es_outer
        self.P.barrier()
        if self.stop == (l, "hgrn"):
            return
        with contextlib.ExitStack() as es:
            self.es = es
            self.rwkv(l)
        self.es = es_outer
        self.P.barrier()
        if self.stop == (l, "rwkv"):
            return
        with contextlib.ExitStack() as es:
            self.es = es
            self.merge(l, src)
        self.es = es_outer
        self.P.barrier()
        if self.stop == (l, "merge"):
            return
        with contextlib.ExitStack() as es:
            self.es = es
            self.moe(l, dst)
        self.es = es_outer
        self.P.barrier()

    def hgrn(self, l):
        sb = self.sb
        CST, DER, FM, XT = self.CST, self.DER, self.FM, self.XT
        WS = [sb(f"h_ws{i}", [128, 8, 128], F32) for i in range(2)]
        WH = [sb(f"h_wh{i}", [128, 8, 640], BF16) for i in range(2)]
        OF = sb("h_of", [128, T], F32)
        S32 = sb("h_s32", [128, 128], F32)
        SBF = sb("h_sbf", [128, 128], BF16)
        Fm = sb("h_f", [128, 512], F32)
        LF = sb("h_lf", [128, 512], F32)
        KX = sb("h_kx", [128, 512], F32)
        G = sb("h_g", [128, 512], F32)
        D1 = sb("h_d1", [128, 512], F32)
        G2 = sb("h_g2", [128, 512], F32)
        EN = sb("h_en", [128, 512], F32)
        KT32 = sb("h_kt32", [128, 512], F32)
        KH = sb("h_kh", [128, 512], BF16)
        SOG = sb("h_sog", [128, 512], F32)
        O = sb("h_o", [128, 512], F32)
        SQ = sb("h_sq", [128, 512], BF16)
        RS = sb("h_rs", [128, 512], F32)
        OUTB = [sb(f"h_outb{i}", [128, 512], BF16) for i in range(2)]
        EP = [sb(f"h_ep{i}", [128, 512], F32) for i in range(2)]
        QT = [sb(f"h_qt{i}", [128, 512], BF16) for i in range(2)]
        KT = [sb(f"h_kt{i}", [128, 512], BF16) for i in range(2)]
        VT = [sb(f"h_vt{i}", [64, 1024], BF16) for i in range(2)]
        KHT = [sb(f"h_kht{i}", [64, 1024], BF16) for i in range(2)]
        AT = [sb(f"h_at{i}", [64, 512], BF16) for i in range(2)]
        cnt = 0
        for h in range(8):
            wh = WH[h % 2]
            for qi in range(5):
                self.load_w(WS[qi % 2], wh, self.w_in, l, qi * 1024 + h * 128, 128, qi * 128, "pool")
            for d in (0, 1):
                self.memset("dve", S32[:, :], 0.0)
                self.memset("dve", SBF[:, :], 0.0)
                for bi in range(NB):
                    blk = bi if d == 0 else NB - 1 - bi
                    t0 = blk * 512
                    k = cnt % 2
                    cnt += 1
                    ep, qt, kt, vt, kht, at = EP[k], QT[k], KT[k], VT[k], KHT[k], AT[k]
                    pq = self.ps()
                    self.proj_fm(pq[:, :], wh, 0, 128, t0, 512)
                    pf = self.ps()
                    self.proj_fm(pf[:, :], wh, 128 * (1 + d), 128, t0, 512)
                    for half in range(2):
                        pv = self.ps()
                        for cc in range(4):
                            c = half * 4 + cc
                            for kk in range(8):
                                self.mm(pv[0:64, cc * 128:(cc + 1) * 128], XT[:, kk, t0 + c * 64:t0 + c * 64 + 64],
                                        wh[:, kk, 384:512], start=(kk == 0), stop=(kk == 7))
                        self.cp("act", vt[0:64, half * 512:(half + 1) * 512], pv[0:64, :])
                    if d == 1:
                        pog = self.ps()
                        self.proj_fm(pog[:, :], wh, 512, 128, t0, 512)
                        self.act(SOG[:, :], pog[:, :], AF.Silu)
                    self.act(Fm[:, :], pf[:, :], AF.Sigmoid)
                    self.ts("dve", Fm[:, :], Fm[:, :], DER[:, 70 + h:71 + h], DER[:, 62 + h:63 + h], ALU.mult, ALU.add)
                    self.act(LF[:, :], Fm[:, :], AF.Ln)
                    self.ts("dve", KX[:, :], Fm[:, :], -1.0, 1.0, ALU.mult, ALU.add)
                    self.scan(G[:, :], CST[:, C_RESET:C_RESET + 512], LF[:, :])
                    g = G
                    if d == 1:
                        self.tt("dve", D1[:, :], LF[:, :], G[:, :], ALU.subtract)
                        for c in range(8):
                            self.ts("dve", G2[:, c * 64:(c + 1) * 64], D1[:, c * 64:(c + 1) * 64],
                                    G[:, c * 64 + 63:c * 64 + 64], None, ALU.add)
                        g = G2
                    self.act(ep[:, :], g[:, :], AF.Exp)
                    self.act(EN[:, :], g[:, :], AF.Exp, scale=-1.0)
                    self.tt("dve", qt[:, :], pq[:, :], ep[:, :], ALU.mult)
                    self.tt("dve", KT32[:, :], KX[:, :], EN[:, :], ALU.mult)
                    self.cp("dve", kt[:, :], KT32[:, :])
                    for c in range(8):
                        ec = c * 64 + 63 if d == 0 else c * 64
                        self.ts("dve" if c % 2 else "pool", KH[:, c * 64:(c + 1) * 64], KT32[:, c * 64:(c + 1) * 64],
                                ep[:, ec:ec + 1], None, ALU.mult)
                    for half in range(2):
                        pk = self.ps()
                        for cc in range(4):
                            c = half * 4 + cc
                            self.mm(pk[0:64, cc * 128:(cc + 1) * 128], KH[:, c * 64:(c + 1) * 64], self.IDB[:, :])
                        self.cp("act", kht[0:64, half * 512:(half + 1) * 512], pk[0:64, :])
                    psc = self.ps()
                    for c in range(8):
                        self.mm(psc[0:64, c * 64:(c + 1) * 64], kt[:, c * 64:(c + 1) * 64], qt[:, c * 64:(c + 1) * 64])
                    mcol = C_FI if d == 0 else C_BI
                    self.tt("dve", at[0:64, :], psc[0:64, :], CST[0:64, mcol:mcol + 512], ALU.mult)
                    po = self.ps(hold=True)
                    for ci in range(8):
                        c = ci if d == 0 else 7 - ci
                        ec = c * 64 + 63 if d == 0 else c * 64
                        self.mm(po[:, c * 64:(c + 1) * 64], vt[0:64, c * 128:(c + 1) * 128], at[0:64, c * 64:(c + 1) * 64],
                                start=True, stop=False)
                        self.mm(po[:, c * 64:(c + 1) * 64], SBF[:, :], qt[:, c * 64:(c + 1) * 64], start=False, stop=True)
                        pss = self.ps()
                        self.mm(pss[:, 0:128], kht[0:64, c * 128:(c + 1) * 128], vt[0:64, c * 128:(c + 1) * 128])
                        self.stt(S32[:, :], S32[:, :], ep[:, ec:ec + 1], pss[:, 0:128], ALU.mult, ALU.add)
                        self.cp("act", SBF[:, :], S32[:, :])
                    po.held = False
                    if d == 0:
                        self.cp("act", OF[:, t0:t0 + 512], po[:, :])
                    else:
                        ob = OUTB[bi % 2]
                        self.tt("dve", O[:, :], po[:, :], OF[:, t0:t0 + 512], ALU.add)
                        self.act(SQ[:, :], O[:, :], AF.Square)
                        pn = self.ps()
                        self.mm(pn[:, :], self.ONB[:, :], SQ[:, :])
                        self.act(RS[:, :], pn[:, :], AF.Sqrt, bias=DER[:, 86:87], scale=1.0 / 128.0)
                        self.recip(RS[:, :], RS[:, :])
                        self.tt("dve", O[:, :], O[:, :], RS[:, :], ALU.mult)
                        self.stt(ob[:, :], O[:, :], FM[:, FM_NW:FM_NW + 1], SOG[:, :], ALU.mult, ALU.mult)
                        self.dma(self.oaT[h * 128:(h + 1) * 128, t0:t0 + 512], ob[:, :])
        self.dbg_src = getattr(self, "dbg_src", {})
        if "oaT" in self.dbg_out:
            self.dbg_src["oaT"] = (self.oaT, (slice(None), slice(None)))
        if "OF" in self.dbg_out:
            self.dbg_src["OF"] = (OF, (slice(None), slice(None)))

    def shift_block(self, pm, hl, hr, rows, omm, hmu, PADB, TMPA, TMPB, OUT):
        R = slice(0, rows)
        self.cp("act", PADB[R, 1:513], pm)
        if hl is None:
            self.memset("dve", PADB[R, 0:1], 0.0)
        else:
            self.cp("dve", PADB[R, 0:1], hl)
        if hr is None:
            self.memset("dve", PADB[R, 513:514], 0.0)
        else:
            self.cp("dve", PADB[R, 513:514], hr)
        self.tt("dve", TMPA[R, :], PADB[R, 0:512], PADB[R, 2:514], ALU.add)
        self.act(TMPB[R, :], PADB[R, 1:513], AF.Identity, scale=omm)
        self.stt(OUT, TMPA[R, :], hmu, TMPB[R, :], ALU.mult, ALU.add)

    def proj_shift(self, wbf, col0, rows, blk, omm, hmu, PADB, TMPA, TMPB, OUT):
        XT = self.XT
        t0 = blk * 512
        pm = self.ps()
        self.proj_fm(pm[0:rows, :], wbf, col0, rows, t0, 512)
        ph = self.ps()
        hl = hr = None
        if blk > 0:
            for c in range(8):
                self.mm(ph[0:rows, 0:1], wbf[:, c, col0:col0 + rows], XT[:, c, t0 - 1:t0], start=(c == 0), stop=(c == 7))
            hl = ph[0:rows, 0:1]
        if blk < NB - 1:
            for c in range(8):
                self.mm(ph[0:rows, 1:2], wbf[:, c, col0:col0 + rows], XT[:, c, t0 + 512:t0 + 513], start=(c == 0), stop=(c == 7))
            hr = ph[0:rows, 1:2]
        self.shift_block(pm[0:rows, :], hl, hr, rows, omm, hmu, PADB, TMPA, TMPB, OUT)

    def rwkv(self, l):
        sb = self.sb
        CST, DER, FM, XT = self.CST, self.DER, self.FM, self.XT
        f32t = lambda n: sb(n, [128, 512], F32)
        bft = lambda n: sb(n, [128, 512], BF16)
        PADB = sb("r_padb", [128, 514], F32)
        TMPA, TMPB = f32t("r_tmpa"), f32t("r_tmpb")
        WS = [sb(f"r_ws{i}", [128, 8, 128], F32) for i in range(2)]
        WUP = sb("r_wup", [128, D], BF16)
        AUP = sb("r_aup", [64, D], BF16)
        GUP = sb("r_gup", [128, D], BF16)
        WUPB = sb("r_wupb", [64, D], BF16)
        LOB = sb("r_lob", [128, 512], BF16)
        TWLt = sb("r_twlt", [64, 512], BF16)
        ALOt = sb("r_alot", [64, 512], BF16)
        SIGGt = sb("r_siggt", [128, 512], BF16)
        if l > 0:
            VUP = sb("r_vup", [32, D], BF16)
            XVDt = sb("r_xvdt", [32, 512], BF16)
        es_keep = self.es
        with contextlib.ExitStack() as es0:
            self.es = es0
            WL = sb("r_wl", [128, 8, 320], BF16)
            self.load_w(WS[0], WL, self.w_in, l, RWB + 3072, 128, 0, "pool")
            self.load_w(WS[1], WL, self.w_in, l, RWB + 3200, 64, 128, "pool")
            self.load_w(WS[0], WL, self.w_in, l, RWB + 3264, 128, 192, "pool")
            LST = sb("r_lst", [128, D], F32)
            self.dma(LST[:, :], self.w_up[l])
            self.cp("dve", WUP[:, :], LST[:, :])
            self.dma(LST[0:64, :], V(self.w_up, self.w_up.h[l][64:128, :]))
            self.cp("dve", WUPB[0:64, :], LST[0:64, :])
            self.dma(LST[0:64, :], self.a_up[l])
            self.cp("dve", AUP[0:64, :], LST[0:64, :])
            self.dma(LST[:, :], self.g_up[l])
            self.cp("dve", GUP[:, :], LST[:, :])
            if l > 0:
                self.dma(LST[0:32, :], self.v_up[l - 1])
                self.cp("dve", VUP[0:32, :], LST[0:32, :])
                VDS = sb("r_vds", [128, 8, 32], F32)
                VD = sb("r_vd", [128, 8, 32], BF16)
                src = self.v_down.h[l - 1].rearrange("(c p) n -> p c n", p=128)
                self.dma(VDS[:, :, :], V(self.v_down, src))
                self.cp("dve", VD[:, :, :], VDS[:, :, :])
            SHT = f32t("r_sht")
            for blk in range(NB):
                t0 = blk * 512
                self.proj_shift(WL, 0, 64, blk, DER[0:64, 24:25], DER[0:64, 51:52], PADB, TMPA, TMPB, SHT[0:64, :])
                self.act(LOB[0:64, :], SHT[0:64, :], AF.Tanh)
                self.dma(self.twl_d[0][:, t0:t0 + 512], LOB[0:64, :])
                self.proj_shift(WL, 64, 64, blk, DER[0:64, 88:89], DER[0:64, 89:90], PADB, TMPA, TMPB, SHT[0:64, :])
                self.act(LOB[0:64, :], SHT[0:64, :], AF.Tanh)
                self.dma(self.twl_d[1][:, t0:t0 + 512], LOB[0:64, :])
                self.proj_shift(WL, 128, 64, blk, DER[0:64, 25:26], DER[0:64, 52:53], PADB, TMPA, TMPB, SHT[0:64, :])
                self.cp("act", LOB[0:64, :], SHT[0:64, :])
                self.dma(self.alo_d[:, t0:t0 + 512], LOB[0:64, :])
                self.proj_shift(WL, 192, 128, blk, DER[:, 26:27], DER[:, 53:54], PADB, TMPA, TMPB, SHT[:, :])
                self.act(LOB[:, :], SHT[:, :], AF.Sigmoid)
                self.dma(self.sigg_d[:, t0:t0 + 512], LOB[:, :])
                if l > 0:
                    pm = self.ps()
                    self.proj_fm(pm[0:32, :], VD, 0, 32, t0, 512)
                    self.cp("act", LOB[0:32, :], pm[0:32, :])
                    self.dma(self.xvd_d[:, t0:t0 + 512], LOB[0:32, :])
        self.es = es_keep
        self.P.barrier()
        import os
        RS = int(os.environ.get("RW_STOP", "99"))
        if RS == 0:
            return
        WP = [sb("r_wp0", [128, 8, 384], BF16)] * 2
        SH = [f32t(f"r_sh{q}") for q in range(3)]
        LW, A, VF, VV, KKN, KFIN, BV = [f32t("r_" + n) for n in ("lw", "a", "vf", "vv", "kkn", "kfin", "bv")]
        G, G2, EP, EN, EX, BT32, KT32, YB, YC = [f32t("r_" + n) for n in ("g", "g2", "ep", "en", "ex", "bt32", "kt32", "yb", "yc")]
        KSQ, ATt, BTt, KTt, RTt, BH, KHh, VB = [bft("r_" + n) for n in ("ksq", "at", "bt", "kt", "rt", "bh", "khh", "vb")]
        OB = [bft(f"r_ob{i}") for i in range(2)]
        h64 = lambda n: TTH(sb(n, [64, 1024], BF16).h)
        VTK, BHT, KHT, MAK, MRB, MRK, TTm = [h64("r_" + n) for n in ("vtk", "bht", "kht", "mak", "mrb", "mrk", "ttm")]
        XA, XtA, XB_, XtB = [h64("r_" + n) for n in ("xa", "xta", "xb", "xtb")]
        U = sb("r_u", [64, 128], BF16)
        UM = h64("r_um")
        Pm = sb("r_pm", [64, 128], BF16)
        S32 = sb("r_s32", [64, 128], F32)
        SBF = sb("r_sbf", [64, 128], BF16)
        AT1, BT1, KT1, RT1 = [sb("r_" + n, [64, 512], BF16) for n in ("at1", "bt1", "kt1", "rt1")]
        EP1 = sb("r_ep1", [64, 512], F32)
        YH = sb("r_yh", [64, 2, 512], F32)
        SH0 = CST[0:64, C_ID:C_ID + 128]
        SH1 = CST[0:64, C_SH1:C_SH1 + 128]
        BDF = CST[:, C_BD:C_BD + 128]
        for j in range(8):
            wp = WP[j % 2]
            for q in range(3):
                self.load_w(WS[q % 2], wp, self.w_in, l, RWB + q * 1024 + 128 * j, 128, q * 128, "pool")
            jc = slice(128 * j, 128 * j + 128)
            for d in (0, 1):
                self.memset("dve", S32[:, :], 0.0)
                self.memset("dve", SBF[:, :], 0.0)
                for bi in range(NB):
                    blk = bi if d == 0 else NB - 1 - bi
                    t0 = blk * 512
                    tb = slice(t0, t0 + 512)
                    for q in range(3):
                        mc = q * 8 + j
                        self.proj_shift(wp, q * 128, 128, blk, DER[:, mc:mc + 1], DER[:, 27 + mc:28 + mc], PADB, TMPA, TMPB, SH[q][:, :])
                    if RS == 1:
                        return
                    pz = self.ps()
                    self.dma(TWLt[0:64, :], self.twl_d[d][:, tb])
                    self.mm(pz[:, :], (WUP if d == 0 else WUPB)[0:64, jc], TWLt[0:64, :])
                    self.act(LW[:, :], pz[:, :], AF.Sigmoid, bias=FM[:, FM_W0 + d * 8 + j:FM_W0 + d * 8 + j + 1])
                    self.ts("dve", LW[:, :], LW[:, :], -C_W, None, ALU.mult)
                    pa = self.ps()
                    self.dma(ALOt[0:64, :], self.alo_d[:, tb])
                    self.mm(pa[:, :], AUP[0:64, jc], ALOt[0:64, :])
                    self.act(A[:, :], pa[:, :], AF.Sigmoid, bias=FM[:, FM_A0 + j:FM_A0 + j + 1])
                    if l == 0:
                        if d == 0:
                            self.dma(self.vfT[jc, tb], SH[2][:, :])
                        Vv = SH[2]
                    else:
                        pv = self.ps()
                        self.dma(XVDt[0:32, :], self.xvd_d[:, tb])
                        self.mm(pv[:, :], VUP[0:32, jc], XVDt[0:32, :])
                        self.act(TMPA[:, :], pv[:, :], AF.Sigmoid, bias=FM[:, FM_V0 + j:FM_V0 + j + 1])
                        self.dma(VF[:, :], self.vfT[jc, tb])
                        self.tt("dve", TMPB[:, :], VF[:, :], SH[2][:, :], ALU.subtract)
                        self.tt("dve", TMPB[:, :], TMPB[:, :], TMPA[:, :], ALU.mult)
                        self.tt("dve", VV[:, :], SH[2][:, :], TMPB[:, :], ALU.add)
                        Vv = VV
                    if RS == 2:
                        return
                    self.ts("dve", TMPB[:, :], SH[1][:, :], FM[:, FM_KK + j:FM_KK + j + 1], None, ALU.mult)
                    self.act(KSQ[:, :], TMPB[:, :], AF.Square)
                    pss = self.ps()
                    self.mm(pss[:, :], self.BDB[:, :], KSQ[:, :])
                    self.act(TMPA[:, :], pss[:, :], AF.Sqrt)
                    self.ts("dve", TMPA[:, :], TMPA[:, :], 1e-12, None, ALU.max)
                    self.recip(TMPA[:, :], TMPA[:, :])
                    self.tt("dve", KKN[:, :], TMPB[:, :], TMPA[:, :], ALU.mult)
                    self.ts("dve", TMPA[:, :], A[:, :], FM[:, FM_KA + j:FM_KA + j + 1], DER[:, 54 + j:55 + j], ALU.mult, ALU.add)
                    self.tt("dve", KFIN[:, :], TMPA[:, :], SH[1][:, :], ALU.mult)
                    self.tt("dve", BV[:, :], KKN[:, :], A[:, :], ALU.mult)
                    if RS == 3:
                        return
                    self.scan(G[:, :], CST[:, C_RESET:C_RESET + 512], LW[:, :])
                    g = G
                    if d == 1:
                        self.tt("dve", TMPA[:, :], LW[:, :], G[:, :], ALU.subtract)
                        for c in range(8):
                            self.ts("dve", G2[:, c * 64:(c + 1) * 64], TMPA[:, c * 64:(c + 1) * 64],
                                    G[:, c * 64 + 63:c * 64 + 64], None, ALU.add)
                        g = G2
                    self.tt("dve", TMPB[:, :], g[:, :], LW[:, :], ALU.subtract)
                    self.act(EP[:, :], g[:, :], AF.Exp)
                    self.act(EN[:, :], g[:, :], AF.Exp, scale=-1.0)
                    self.act(EX[:, :], TMPB[:, :], AF.Exp)
                    self.stt(ATt[:, :], KKN[:, :], -1.0, EX[:, :], ALU.mult, ALU.mult)
                    self.tt("dve", BT32[:, :], BV[:, :], EN[:, :], ALU.mult)
                    self.cp("dve", BTt[:, :], BT32[:, :])
                    self.tt("dve", KT32[:, :], KFIN[:, :], EN[:, :], ALU.mult)
                    self.cp("dve", KTt[:, :], KT32[:, :])
                    self.tt("dve", RTt[:, :], SH[0][:, :], EP[:, :], ALU.mult)
                    for c in range(8):
                        ec = c * 64 + 63 if d == 0 else c * 64
                        cs = slice(c * 64, c * 64 + 64)
                        self.ts("dve", BH[:, cs], BT32[:, cs], EP[:, ec:ec + 1], None, ALU.mult)
                        self.ts("pool", KHh[:, cs], KT32[:, cs], EP[:, ec:ec + 1], None, ALU.mult)
                    self.cp("act", VB[:, :], Vv[:, :])
                    self.dma(AT1[0:64, :], ATt[64:128, :])
                    self.dma(BT1[0:64, :], BTt[64:128, :])
                    self.dma(KT1[0:64, :], KTt[64:128, :])
                    self.dma(RT1[0:64, :], RTt[64:128, :])
                    self.dma(EP1[0:64, :], EP[64:128, :])
                    HA = (ATt, AT1)
                    HB = (BTt, BT1)
                    HK = (KTt, KT1)
                    HR = (RTt, RT1)
                    HEP = (EP, EP1)
                    if RS == 4:
                        return
                    for srcb, dstb in ((VB, VTK), (BH, BHT), (KHh, KHT)):
                        for half in range(2):
                            pk = self.ps()
                            for cc in range(4):
                                c = half * 4 + cc
                                self.mm(pk[0:64, cc * 128:(cc + 1) * 128], srcb[:, c * 64:(c + 1) * 64], self.IDB[:, :])
                            self.cp("act" if half else "dve", dstb[0:64, half * 512:(half + 1) * 512], pk[0:64, :])
                    m_strict = C_FS if d == 0 else C_BS
                    m_strict_T = C_BS if d == 0 else C_FS
                    m_incl = C_FI if d == 0 else C_BI
                    for half in range(2):
                        banks = [self.ps() for _ in range(5)]
                        for cc in range(4):
                            c = half * 4 + cc
                            cs = slice(c * 64, c * 64 + 64)
                            for e in range(2):
                                R = slice(64 * e, 64 * e + 64)
                                co = slice((cc * 2 + e) * 64, (cc * 2 + e) * 64 + 64)
                                Z = slice(0, 64)
                                self.mm(banks[0][0:64, co], HB[e][Z, cs], HA[e][Z, cs])
                                self.mm(banks[1][0:64, co], HA[e][Z, cs], HB[e][Z, cs])
                                self.mm(banks[2][0:64, co], HK[e][Z, cs], HA[e][Z, cs])
                                self.mm(banks[3][0:64, co], HB[e][Z, cs], HR[e][Z, cs])
                                self.mm(banks[4][0:64, co], HK[e][Z, cs], HR[e][Z, cs])
                        hs = slice(half * 512, half * 512 + 512)
                        self.tt("dve", XA[0:64, hs], banks[0][0:64, :], CST[0:64, m_strict:m_strict + 512], ALU.mult)
                        self.tt("dve", XtA[0:64, hs], banks[1][0:64, :], CST[0:64, m_strict_T:m_strict_T + 512], ALU.mult)
                        self.tt("dve", MAK[0:64, hs], banks[2][0:64, :], CST[0:64, m_strict:m_strict + 512], ALU.mult)
                        self.tt("dve", MRB[0:64, hs], banks[3][0:64, :], CST[0:64, m_incl:m_incl + 512], ALU.mult)
                        self.tt("dve", MRK[0:64, hs], banks[4][0:64, :], CST[0:64, m_incl:m_incl + 512], ALU.mult)
                        self.tt("dve", TTm[0:64, hs], XA[0:64, hs], CST[0:64, C_I8:C_I8 + 512], ALU.add)
                    if RS == 5:
                        return
                    Xc, Xtc, Xn, Xtn = XA, XtA, XB_, XtB
                    for lev in range(5):
                        for half in range(2):
                            hs = slice(half * 512, half * 512 + 512)
                            p2 = self.ps()
                            for qq in range(8):
                                co = slice(qq * 64, qq * 64 + 64)
                                sc = slice(half * 512 + qq * 64, half * 512 + qq * 64 + 64)
                                self.mm(p2[0:64, co], Xc[0:64, sc], Xtc[0:64, sc])
                            self.cp("act", Xtn[0:64, hs], p2[0:64, :])
                            if lev < 4:
                                p1 = self.ps()
                                for qq in range(8):
                                    co = slice(qq * 64, qq * 64 + 64)
                                    sc = slice(half * 512 + qq * 64, half * 512 + qq * 64 + 64)
                                    self.mm(p1[0:64, co], Xtc[0:64, sc], Xc[0:64, sc])
                                self.cp("act" if half else "dve", Xn[0:64, hs], p1[0:64, :])
                        for half in range(2):
                            hs = slice(half * 512, half * 512 + 512)
                            p3 = self.ps()
                            for qq in range(8):
                                co = slice(qq * 64, qq * 64 + 64)
                                sc = slice(half * 512 + qq * 64, half * 512 + qq * 64 + 64)
                                self.mm(p3[0:64, co], Xtn[0:64, sc], TTm[0:64, sc])
                            self.tt("dve", TTm[0:64, hs], TTm[0:64, hs], p3[0:64, :], ALU.add)
                        Xc, Xtc, Xn, Xtn = Xn, Xtn, Xc, Xtc
                    if RS == 6:
                        return
                    for half in range(2):
                        pum = self.ps()
                        for cc in range(4):
                            c = half * 4 + cc
                            for e in range(2):
                                co = slice((c * 2 + e) * 64, (c * 2 + e) * 64 + 64)
                                self.mm(pum[0:64, cc * 128 + 64 * e:cc * 128 + 64 * e + 64], MAK[0:64, co],
                                        VTK[0:64, c * 128 + 64 * e:c * 128 + 64 * e + 64])
                        self.cp("act", UM[0:64, half * 512:(half + 1) * 512], pum[0:64, :])
                    if d == 1:
                        self.dma(YC[:, :], self.yfd[jc, tb])
                    for ci in range(8):
                        c = ci if d == 0 else 7 - ci
                        ec = c * 64 + 63 if d == 0 else c * 64
                        cs = slice(c * 64, c * 64 + 64)
                        Z = slice(0, 64)
                        pu = self.ps()
                        for e in range(2):
                            E = slice(64 * e, 64 * e + 64)
                            self.mm(pu[Z, E], HA[e][Z, cs], SBF[Z, E], start=True, stop=True)
                        self.cp("act", U[Z, :], pu[Z, 0:128])
                        pp = self.ps()
                        for e in range(2):
                            E = slice(64 * e, 64 * e + 64)
                            co = slice((c * 2 + e) * 64, (c * 2 + e) * 64 + 64)
                            ve = slice(c * 128 + 64 * e, c * 128 + 64 * e + 64)
                            self.mm(pp[Z, E], TTm[Z, co], U[Z, E], start=True, stop=False)
                            self.mm(pp[Z, E], TTm[Z, co], UM[Z, ve], start=False, stop=True)
                        self.cp("dve", Pm[Z, :], pp[Z, 0:128])
                        py = self.ps()
                        for e in range(2):
                            E = slice(64 * e, 64 * e + 64)
                            co = slice((c * 2 + e) * 64, (c * 2 + e) * 64 + 64)
                            ve = slice(c * 128 + 64 * e, c * 128 + 64 * e + 64)
                            self.mm(py[Z, E], SBF[Z, E], HR[e][Z, cs], start=True, stop=False)
                            self.mm(py[Z, E], Pm[Z, E], MRB[Z, co], start=False, stop=False)
                            self.mm(py[Z, E], VTK[Z, ve], MRK[Z, co], start=False, stop=True)
                        for e in range(2):
                            E = slice(64 * e, 64 * e + 64)
                            self.cp("act" if e else "dve", YH[Z, e, cs], py[Z, E])
                        pst = self.ps()
                        for e in range(2):
                            E = slice(64 * e, 64 * e + 64)
                            ve = slice(c * 128 + 64 * e, c * 128 + 64 * e + 64)
                            self.mm(pst[Z, E], BHT[Z, ve], Pm[Z, E], start=True, stop=False)
                            self.mm(pst[Z, E], KHT[Z, ve], VTK[Z, ve], start=False, stop=True)
                        for e in range(2):
                            E = slice(64 * e, 64 * e + 64)
                            self.stt(S32[Z, E], S32[Z, E], HEP[e][Z, ec:ec + 1], pst[Z, E], ALU.mult, ALU.add)
                        self.cp("act", SBF[Z, :], S32[Z, :])
                    pyb = self.ps()
                    for n4 in range(4):
                        ns = slice(n4 * 128, n4 * 128 + 128)
                        self.mm(pyb[:, ns], SH0, YH[0:64, 0, ns], start=True, stop=False)
                        self.mm(pyb[:, ns], SH1, YH[0:64, 1, ns], start=False, stop=True)
                    if d == 0:
                        self.cp("act", YB[:, :], pyb[:, :])
                    else:
                        self.tt("dve", YB[:, :], pyb[:, :], YC[:, :], ALU.add)
                    if RS == 7:
                        return
                    if d == 0:
                        self.dma(self.yfd[jc, tb], YB[:, :])
                    else:
                        ob = OB[bi % 2]
                        pm = self.ps()
                        for n4 in range(4):
                            self.mm(pm[:, n4 * 128:n4 * 128 + 128], BDF, YB[:, n4 * 128:n4 * 128 + 128])
                        self.stt(YC[:, :], pm[:, :], -1.0 / 64.0, YB[:, :], ALU.mult, ALU.add)
                        self.act(EN[:, :], YC[:, :], AF.Square)
                        pv2 = self.ps()
                        for n4 in range(4):
                            self.mm(pv2[:, n4 * 128:n4 * 128 + 128], BDF, EN[:, n4 * 128:n4 * 128 + 128])
                        self.act(EX[:, :], pv2[:, :], AF.Sqrt, bias=DER[:, 87:88], scale=1.0 / 64.0)
                        self.recip(EX[:, :], EX[:, :])
                        self.tt("dve", YC[:, :], YC[:, :], EX[:, :], ALU.mult)
                        self.ts("dve", YC[:, :], YC[:, :], FM[:, FM_LNW + j:FM_LNW + j + 1], FM[:, FM_LNB + j:FM_LNB + j + 1], ALU.mult, ALU.add)
                        self.stt(BT32[:, :], SH[0][:, :], FM[:, FM_RK + j:FM_RK + j + 1], KFIN[:, :], ALU.mult, ALU.mult)
                        pb = self.ps()
                        for n4 in range(4):
                            self.mm(pb[:, n4 * 128:n4 * 128 + 128], BDF, BT32[:, n4 * 128:n4 * 128 + 128])
                        self.tt("dve", KT32[:, :], pb[:, :], Vv[:, :], ALU.mult)
                        self.tt("dve", YC[:, :], YC[:, :], KT32[:, :], ALU.add)
                        pg = self.ps()
                        self.dma(SIGGt[:, :], self.sigg_d[:, tb])
                        self.mm(pg[:, :], GUP[:, jc], SIGGt[:, :])
                        self.tt("dve", ob[:, :], YC[:, :], pg[:, :], ALU.mult)
                        self.dma(self.obT[jc, tb], ob[:, :])
        self.dbg_src = getattr(self, "dbg_src", {})
        if "obT" in self.dbg_out:
            self.dbg_src["obT"] = (self.obT, (slice(None), slice(None)))


    def layernorm_tile(self, H, g, b, OUT, SUM, NM, JUNK):
        self.act(JUNK[:, :], H[:, :], AF.Identity, accum=SUM[:, 0:1])
        self.ts("dve", NM[:, 0:1], SUM[:, 0:1], -1.0 / D, None, ALU.mult)
        self.act(H[:, :], H[:, :], AF.Identity, bias=NM[:, 0:1])
        self.act(JUNK[:, :], H[:, :], AF.Square, accum=SUM[:, 1:2])
        self.act(NM[:, 1:2], SUM[:, 1:2], AF.Sqrt, bias=self.DER[:, 86:87], scale=1.0 / D)
        self.recip(NM[:, 1:2], NM[:, 1:2])
        self.stt(OUT[:, :], H[:, :], NM[:, 1:2], g, ALU.mult, ALU.mult)
        self.tt("pool", OUT[:, :], OUT[:, :], b, ALU.add)

    def merge(self, l, src):
        sb = self.sb
        CST, XT = self.CST, self.XT
        es_keep = self.es
        with contextlib.ExitStack() as es1:
            self.es = es1
            WS = [sb(f"m_ws{i}", [128, 8, 256], F32) for i in range(2)]
            PA = sb("m_pa", [128, 8, D], BF16)
            PB = sb("m_pb", [128, 8, D], BF16)
            WG = sb("m_wg", [128, 8, 2 * D], BF16)
            k = 0
            for q in range(4):
                self.load_w(WS[k % 2], PA, self.proj_a, l, q * 256, 256, q * 256, "pool" if k % 2 else "dve"); k += 1
                self.load_w(WS[k % 2], PB, self.proj_b, l, q * 256, 256, q * 256, "pool" if k % 2 else "dve"); k += 1
            for q in range(8):
                self.load_w(WS[k % 2], WG, self.w_in, l, GAB + q * 256, 256, q * 256, "pool" if k % 2 else "dve"); k += 1
            OA = sb("m_oa", [128, 8, 512], BF16)
            OBt = sb("m_ob", [128, 8, 512], BF16)
            SA, SB_, M1t, M2t = [sb("m_" + n, [128, 512], F32) for n in ("sa", "sb", "m1", "m2")]
            MG = [sb(f"m_mg{i}", [128, 512], BF16) for i in range(2)]
            for blk in range(NB):
                t0 = blk * 512
                self.dma(OA[:, :, :], V(self.oaT, self.oaT.h[:, t0:t0 + 512].rearrange("(c p) t -> p c t", p=128)))
                self.dma(OBt[:, :, :], V(self.obT, self.obT.h[:, t0:t0 + 512].rearrange("(c p) t -> p c t", p=128)))
                for j in range(8):
                    js = slice(j * 128, j * 128 + 128)
                    pA = self.ps()
                    for c in range(8):
                        self.mm(pA[:, :], PA[:, c, js], OA[:, c, :], start=(c == 0), stop=(c == 7))
                    pB = self.ps()
                    for c in range(8):
                        self.mm(pB[:, :], PB[:, c, js], OBt[:, c, :], start=(c == 0), stop=(c == 7))
                    pga = self.ps()
                    self.proj_fm(pga[:, :], WG, j * 128, 128, t0, 512)
                    pgb = self.ps()
                    self.proj_fm(pgb[:, :], WG, D + j * 128, 128, t0, 512)
                    self.act(SA[:, :], pga[:, :], AF.Sigmoid)
                    self.act(SB_[:, :], pgb[:, :], AF.Sigmoid)
                    self.tt("dve", M1t[:, :], pA[:, :], SA[:, :], ALU.mult)
                    self.tt("dve", M2t[:, :], pB[:, :], SB_[:, :], ALU.mult)
                    mg = MG[j % 2]
                    self.tt("pool", mg[:, :], M1t[:, :], M2t[:, :], ALU.add)
                    self.dma(self.mgT[js, t0:t0 + 512], mg[:, :])
        self.es = es_keep
        self.P.barrier()
        with contextlib.ExitStack() as es2:
            self.es = es2
            WS = [sb(f"m2_ws{i}", [128, 8, 256], F32) for i in range(2)]
            WO = sb("m2_wo", [128, 8, D], BF16)
            for q in range(4):
                self.load_w(WS[q % 2], WO, self.w_out, l, q * 256, 256, q * 256, "pool" if q % 2 else "dve")
            BC = sb("m2_bc", [128, 2 * D], F32)
            self.dma(BC[:, :], self.bc_pack.h[l][:, 0:2 * D] if False else V(self.bc_pack, self.bc_pack.h[l][:, 0:2 * D]))
            MGt = [sb(f"m2_mg{i}", [128, 8, 128], BF16) for i in range(2)]
            XR = [sb(f"m2_xr{i}", [128, D], F32) for i in range(2)]
            H = sb("m2_h", [128, D], F32)
            JUNK = sb("m2_junk", [128, D], F32)
            X1 = [sb(f"m2_x1{i}", [128, D], F32) for i in range(2)]
            SUM = sb("m2_sum", [128, 2], F32)
            NM = sb("m2_nm", [128, 2], F32)
            for i in range(T // 128):
                ts_ = slice(i * 128, i * 128 + 128)
                mgt, xr, x1 = MGt[i % 2], XR[i % 2], X1[i % 2]
                self.dma(mgt[:, :, :], V(self.mgT, self.mgT.h[:, ts_].rearrange("(c p) t -> p c t", p=128)))
                self.dma(xr[:, :], src[ts_, :])
                for half in range(2):
                    hs = slice(half * 512, half * 512 + 512)
                    ph = self.ps()
                    for c in range(8):
                        self.mm(ph[:, :], mgt[:, c, :], WO[:, c, hs], start=(c == 0), stop=(c == 7))
                    self.stt(H[:, hs], xr[:, hs], DN_ALPHA, ph[:, :], ALU.mult, ALU.add)
                self.layernorm_tile(H, BC[:, 0:D], BC[:, D:2 * D], x1, SUM, NM, JUNK)
                self.dma(self.x1d[ts_, :], x1[:, :])
                for half in range(2):
                    p = self.ps()
                    for c in range(4):
                        cc = half * 4 + c
                        self.mm(p[:, c * 128:(c + 1) * 128], x1[:, cc * 128:(cc + 1) * 128], CST[:, C_ID:C_ID + 128])
                    for c in range(4):
                        cc = half * 4 + c
                        self.cp("act" if c % 2 else "dve", XT[:, cc, ts_], p[:, c * 128:(c + 1) * 128])
        self.es = es_keep
        self.dbg_src = getattr(self, "dbg_src", {})
        if "x1" in self.dbg_out:
            self.dbg_src["x1"] = (self.x1d, (slice(None), slice(None)))

    def moe(self, l, dst):
        sb = self.sb
        CST, XT = self.CST, self.XT
        TP = 1024
        NTB = TP // 512
        NTL = TP // 128
        RWS = sb("e_rws", [128, 8, NE], F32)
        RW = sb("e_rw", [128, 8, NE], BF16)
        self.dma(RWS[:, :, :], V(self.router_w, self.router_w.h[l].rearrange("(c p) n -> p c n", p=128)))
        self.cp("dve", RW[:, :, :], RWS[:, :, :])
        BC = sb("e_bc", [128, 2 * D + NE], F32)
        self.dma(BC[:, :], V(self.bc_pack, self.bc_pack.h[l][:, 2 * D:4 * D + NE]))
        B1 = sb("e_b1", [128, NE * 16], F32)
        self.dma(B1[:, :], self.b1_pack[l])
        B2S = sb("e_b2s", [NE, D], F32)
        B2 = sb("e_b2", [NE, D], BF16)
        self.dma(B2S[:, :], self.moe_b2[l])
        self.cp("dve", B2[:, :], B2S[:, :])
        GT = sb("e_gt", [128, T // 128, NE], F32)
        GTT = sb("e_gtt", [NE, T], BF16)
        LG, EXv, MSK = [sb("e_" + n, [128, NE], F32) for n in ("lg", "ex", "msk")]
        M8 = sb("e_m8", [128, 8], F32)
        SS = sb("e_ss", [128, 2], F32)
        for i in range(T // 128):
            ts_ = slice(i * 128, i * 128 + 128)
            pl = self.ps()
            for c in range(8):
                self.mm(pl[:, 0:NE], XT[:, c, ts_], RW[:, c, :], start=(c == 0), stop=(c == 7))
            self.tt("dve", LG[:, :], pl[:, 0:NE], BC[:, 2 * D:2 * D + NE], ALU.add)
            lg_ap, m8_ap = LG[:, :].ap, M8[:, :].ap
            self.P.op("dve", lambda e, o=m8_ap, i_=lg_ap: e.max(out=o, in_=i_), reads=[LG.b], writes=[M8.b])
            self.ts("dve", SS[:, 0:1], M8[:, 0:1], -1.0, None, ALU.mult)
            self.act(EXv[:, :], LG[:, :], AF.Exp, bias=SS[:, 0:1])
            self.ts("dve", MSK[:, :], LG[:, :], M8[:, 3:4], None, ALU.is_ge)
            self.tt("dve", EXv[:, :], EXv[:, :], MSK[:, :], ALU.mult)
            self.act(MSK[:, :], EXv[:, :], AF.Identity, accum=SS[:, 1:2])
            self.recip(SS[:, 1:2], SS[:, 1:2])
            self.ts("dve", GT[:, i, :], EXv[:, :], SS[:, 1:2], None, ALU.mult)
            pt = self.ps()
            self.mm(pt[0:NE, 0:128], GT[:, i, :], CST[:, C_ID:C_ID + 128])
            self.cp("act", GTT[0:NE, ts_], pt[0:NE, 0:128])
        ACC = sb("e_acc", [128, NTL, D], F32)
        ACTT = sb("e_actt", [128, 8, TP], BF16)
        W1S = [sb("e_w1s0", [128, 8, 256], F32)] * 2
        W1B = [sb(f"e_w1b{i}", [128, 8, 256], BF16) for i in range(2)]
        W2S = [sb(f"e_w2s{i}", [128, 512], F32) for i in range(2)]
        W2B = sb("e_w2b", [128, 8, 512], BF16)
        GLU, SIG, LIN = [sb("e_" + n, [128, 512], F32) for n in ("glu", "sig", "lin")]
        XR = sb("e_xr", [128, D], F32)
        H = sb("e_h", [128, D], F32)
        JUNK = XR
        SUM = sb("e_sum", [128, 2], F32)
        NM = sb("e_nm", [128, 2], F32)
        wk = 0
        for ps_i in range(T // TP):
            tp0 = ps_i * TP
            for tl in range(NTL):
                tsl = slice(tp0 + tl * 128, tp0 + tl * 128 + 128)
                for half in range(2):
                    hs = slice(half * 512, half * 512 + 512)
                    pb = self.ps()
                    self.mm(pb[:, :], GTT[0:NE, tsl], B2[0:NE, hs])
                    self.cp("act", ACC[:, tl, hs], pb[:, :])
            for e in range(NE):
                for ft in range(8):
                    w1s, w1b = W1S[wk % 2], W1B[wk % 2]
                    wk += 1
                    src_ap = self.moe_w1.h[l][e][:, ft * 256:(ft + 1) * 256].rearrange("(c p) n -> p c n", p=128)
                    self.dma(w1s[:, :, :], V(self.moe_w1, src_ap))
                    de = w1s.h[:, :, :].rearrange("p c (f two) -> p c f two", two=2)
                    self.cp("pool", w1b[:, :, 0:128], V(w1s, de[:, :, :, 0]))
                    self.cp("pool" if ft % 2 else "dve", w1b[:, :, 128:256], V(w1s, de[:, :, :, 1]))
                    for tb in range(NTB):
                        t0 = tp0 + tb * 512
                        pg = self.ps()
                        self.proj_fm(pg[:, :], w1b, 0, 128, t0, 512)
                        pl = self.ps()
                        self.proj_fm(pl[:, :], w1b, 128, 128, t0, 512)
                        bg = B1[:, e * 16 + ft:e * 16 + ft + 1]
                        bl = B1[:, e * 16 + 8 + ft:e * 16 + 8 + ft + 1]
                        self.ts("dve", GLU[:, :], pg[:, :], bg, SWIGLU_LIMIT, ALU.add, ALU.min)
                        self.act(SIG[:, :], GLU[:, :], AF.Sigmoid, scale=SWIGLU_ALPHA)
                        self.ts("dve", LIN[:, :], pl[:, :], bl, SWIGLU_LIMIT, ALU.add, ALU.min)
                        self.ts("pool", LIN[:, :], LIN[:, :], -SWIGLU_LIMIT, 1.0, ALU.max, ALU.add)
                        self.tt("pool", GLU[:, :], GLU[:, :], SIG[:, :], ALU.mult)
                        self.tt("dve", ACTT[:, ft, tb * 512:(tb + 1) * 512], GLU[:, :], LIN[:, :], ALU.mult)
                for half in range(2):
                    hs = slice(half * 512, half * 512 + 512)
                    for ft in range(8):
                        w2s = W2S[ft % 2]
                        self.dma(w2s[:, :], V(self.moe_w2, self.moe_w2.h[l][e][ft * 128:(ft + 1) * 128, hs]))
                        self.cp("act" if ft % 2 else "pool", W2B[:, ft, :], w2s[:, :])
                    for tl in range(NTL):
                        gi = (tp0 // 128) + tl
                        py = self.ps()
                        for ft in range(8):
                            self.mm(py[:, :], ACTT[:, ft, tl * 128:(tl + 1) * 128], W2B[:, ft, :], start=(ft == 0), stop=(ft == 7))
                        self.stt(ACC[:, tl, hs], py[:, :], GT[:, gi, e:e + 1], ACC[:, tl, hs], ALU.mult, ALU.add)
            for tl in range(NTL):
                tsl = slice(tp0 + tl * 128, tp0 + tl * 128 + 128)
                self.dma(XR[:, :], self.x1d[tsl, :])
                self.stt(H[:, :], XR[:, :], DN_ALPHA, ACC[:, tl, :], ALU.mult, ALU.add)
                self.layernorm_tile(H, BC[:, 0:D], BC[:, D:2 * D], XR, SUM, NM, JUNK)
                self.out_evs.append(self.dma(dst[tsl, :], XR[:, :]))
        self.dbg_src = getattr(self, "dbg_src", {})
        if "x2" in self.dbg_out:
            self.dbg_src["x2"] = (dst, (slice(None), slice(None)))


def make_consts():
    c = np.zeros((128, NCONST), np.float32)
    c[:, C_ID:C_ID + 128] = np.eye(128, dtype=np.float32)
    i = np.arange(64)
    fi = (i[:, None] <= i[None, :]).astype(np.float32)
    fs = (i[:, None] < i[None, :]).astype(np.float32)
    bi = (i[:, None] >= i[None, :]).astype(np.float32)
    bs = (i[:, None] > i[None, :]).astype(np.float32)
    for base, m in ((C_FI, fi), (C_FS, fs), (C_BI, bi), (C_BS, bs)):
        c[0:64, base:base + 512] = np.tile(m, (1, 8))
    r = np.ones(512, np.float32)
    r[::64] = 0.0
    c[:, C_RESET:C_RESET + 512] = r[None, :]
    bd = np.zeros((128, 128), np.float32)
    bd[0:64, 0:64] = 1.0
    bd[64:128, 64:128] = 1.0
    c[:, C_BD:C_BD + 128] = bd
    c[:, C_ONES:C_ONES + 128] = 1.0
    c[0:64, C_I8:C_I8 + 512] = np.tile(np.eye(64, dtype=np.float32), (1, 8))
    c[0:64, C_SH1 + 64:C_SH1 + 128] = np.eye(64, dtype=np.float32)
    return c


def fmcols(v):
    v = np.asarray(v, np.float32).reshape(-1)
    n = (v.size + 127) // 128
    p = np.zeros(n * 128, np.float32)
    p[:v.size] = v
    return p.reshape(n, 128).T


def make_packs(inp):
    fm = np.zeros((L_ALL, 128, NFM), np.float32)
    bc = np.zeros((L_ALL, 128, 4 * D + NE), np.float32)
    b1 = np.zeros((L_ALL, 128, NE * 16), np.float32)
    for l in range(L_ALL):
        mu = np.asarray(inp["rw_mu"][l], np.float32)
        fm[l, :, FM_MU:FM_MU + 24] = fmcols(mu[0:3072])
        fm[l, :, FM_MU + 24] = mu[3072:3200]
        fm[l, 0:64, FM_MUB] = mu[3136:3200]
        fm[l, 0:64, FM_MU + 25] = mu[3200:3264]
        fm[l, :, FM_MU + 26] = mu[3264:3392]
        fm[l, :, FM_W0:FM_W0 + 16] = fmcols(inp["rw_w0"][l])
        fm[l, :, FM_A0:FM_A0 + 8] = fmcols(inp["rw_a0"][l])
        fm[l, :, FM_KK:FM_KK + 8] = fmcols(inp["rw_k_k"][l])
        fm[l, :, FM_KA:FM_KA + 8] = fmcols(inp["rw_k_a"][l])
        fm[l, :, FM_RK:FM_RK + 8] = fmcols(inp["rw_r_k"][l])
        fm[l, :, FM_LNW:FM_LNW + 8] = fmcols(inp["rw_lnx_w"][l])
        fm[l, :, FM_LNB:FM_LNB + 8] = fmcols(inp["rw_lnx_b"][l])
        if l > 0:
            fm[l, :, FM_V0:FM_V0 + 8] = fmcols(inp["rw_v0"][l - 1])
        for i in range(L_ALL):
            fm[l, :, FM_LBL + 8 * i:FM_LBL + 8 * i + 8] = fmcols(inp["hg_lb_logits"][i])
        fm[l, :, FM_NW] = inp["hg_norm_w"][l]
        bc[l, :, 0:D] = inp["ln1_g"][l][None, :]
        bc[l, :, D:2 * D] = inp["ln1_b"][l][None, :]
        bc[l, :, 2 * D:3 * D] = inp["ln2_g"][l][None, :]
        bc[l, :, 3 * D:4 * D] = inp["ln2_b"][l][None, :]
        bc[l, :, 4 * D:] = inp["router_b"][l][None, :]
        bb = np.asarray(inp["moe_b1"][l], np.float32)
        glu = bb[:, 0::2].reshape(NE, 8, 128)
        lin = bb[:, 1::2].reshape(NE, 8, 128)
        pk = np.concatenate([glu, lin], axis=1)
        b1[l] = pk.transpose(2, 0, 1).reshape(128, NE * 16)
    return fm, bc, b1


_CACHE = {}


def run(inputs, n_layers=L_ALL, stop=None, dbg=(), n_cores=4):
    key = (n_layers, stop, tuple(dbg))
    bld = Builder(n_layers=n_layers, stop=stop, dbg=dbg)
    nc = bld.build()
    inp = {k: np.asarray(v) for k, v in inputs.items()}
    fm, bc, b1 = make_packs(inp)
    shared = {
        "w_in": np.ascontiguousarray(inp["w_in"], np.float32),
        "rw_w_up": np.ascontiguousarray(inp["rw_w_up"], np.float32).reshape(L_ALL, 128, D),
        "rw_a_up": np.ascontiguousarray(inp["rw_a_up"], np.float32),
        "rw_g_up": np.ascontiguousarray(inp["rw_g_up"], np.float32),
        "rw_v_down": np.ascontiguousarray(inp["rw_v_down"], np.float32),
        "rw_v_up": np.ascontiguousarray(inp["rw_v_up"], np.float32),
        "proj_a": np.ascontiguousarray(inp["proj_a"], np.float32),
        "proj_b": np.ascontiguousarray(inp["proj_b"], np.float32),
        "w_out": np.ascontiguousarray(inp["w_out"], np.float32),
        "router_w": np.ascontiguousarray(inp["router_w"], np.float32),
        "fm_pack": fm, "bc_pack": bc, "b1_pack": b1, "consts": make_consts(),
    }
    if bld.need_moe:
        for k in ("moe_w1", "moe_w2", "moe_b2"):
            shared[k] = np.ascontiguousarray(inp[k], np.float32)
    in_maps = []
    for c in range(n_cores):
        m = dict(shared)
        m["x"] = np.ascontiguousarray(inp["x"][c % 4], np.float32)
        in_maps.append(m)
    res = run_bass_kernel_spmd(nc, in_maps, core_ids=list(range(n_cores)))
    return res


def kernel(**inputs):
    res = run(inputs)
    out = np.stack([np.asarray(res.results[c]["y"], np.float32) for c in range(4)], axis=0)
    return out
```

```python
import contextlib
import os
import numpy as np
import concourse.bass as bass
import concourse.mybir as mybir
from concourse.bass_utils import run_bass_kernel_spmd

F32 = mybir.dt.float32
BF16 = mybir.dt.bfloat16
AF = mybir.ActivationFunctionType
ALU = mybir.AluOpType

L_ALL = 4
D = 1024
T = 4096
NB = 8
NCOLS = 10560
RWB = 5120
GAB = RWB + 3392
GBB = GAB + 1024
DN_ALPHA = (2 * L_ALL) ** 0.25
NORM_EPS = 1e-5
RW_LN_EPS = 64e-5
NE = 32
C_W = float(np.exp(-0.5))
SWIGLU_ALPHA = 1.702
SWIGLU_LIMIT = 7.0

FM_MU = 0
FM_W0 = 27
FM_A0 = 43
FM_KK = 51
FM_KA = 59
FM_RK = 67
FM_LNW = 75
FM_LNB = 83
FM_V0 = 91
FM_LBL = 99
FM_NW = 131
FM_MUB = 132
NFM = 133
C_ID = 0
C_FI = 128
C_FS = 640
C_BI = 1152
C_BS = 1664
C_RESET = 2176
C_BD = 2688
C_ONES = 2816
C_I8 = 2944
C_SH1 = 3456
NCONST = 3584


class Buf:
    __slots__ = ("w", "r")

    def __init__(self):
        self.w = {}
        self.r = {}


class Prog:
    NDMA = 8

    def __init__(self):
        self.engs = ("pe", "dve", "act", "pool", "sp")
        self.q = {e: [] for e in self.engs}
        self.cnt = {}
        self.seen = {e: {} for e in self.engs}
        self.dma_slot = {e: 0 for e in self.engs}
        self.sem_keys = ["pe", "dve", "act", "pool"]
        for e in ("sp", "pool", "act"):
            for i in range(self.NDMA):
                self.sem_keys.append(f"d_{e}_{i}")
        for k in self.sem_keys:
            self.cnt[k] = 0

    def _deps(self, eng, reads, writes):
        need = {}
        for b in reads:
            for k, v in b.w.items():
                if need.get(k, 0) < v:
                    need[k] = v
        for b in writes:
            for k, v in b.w.items():
                if need.get(k, 0) < v:
                    need[k] = v
            for k, v in b.r.items():
                if need.get(k, 0) < v:
                    need[k] = v
        waits = []
        seen = self.seen[eng]
        for k, v in need.items():
            if k == "pe" and eng == "pe":
                continue
            if seen.get(k, 0) < v:
                seen[k] = v
                waits.append((k, v))
        return waits

    @staticmethod
    def _mark(ev, reads, writes):
        k, v = ev
        for b in writes:
            b.w[k] = v
        for b in reads:
            b.r[k] = v

    def op(self, eng, fn, reads=(), writes=()):
        self.total = getattr(self, "total", 0) + 1
        if self.total > int(os.environ.get("OPLIM", "1000000000")):
            return None
        waits = self._deps(eng, reads, writes)
        self.cnt[eng] += 1
        ev = (eng, self.cnt[eng])
        self.q[eng].append((fn, waits, (eng, 1)))
        self._mark(ev, reads, writes)
        return ev

    def dma(self, eng, fn, reads=(), writes=()):
        self.total = getattr(self, "total", 0) + 1
        if self.total > int(os.environ.get("OPLIM", "1000000000")):
            return None
        slot = self.dma_slot[eng]
        self.dma_slot[eng] = (slot + 1) % self.NDMA
        key = f"d_{eng}_{slot}"
        waits = self._deps(eng, reads, writes)
        prev = self.cnt[key]
        if prev > 0 and self.seen[eng].get(key, 0) < prev:
            self.seen[eng][key] = prev
            waits.append((key, prev))
        self.cnt[key] += 16
        ev = (key, self.cnt[key])
        self.q[eng].append((fn, waits, (key, 16)))
        self._mark(ev, reads, writes)
        return ev

    def barrier(self):
        snap = dict(self.cnt)
        for e in self.engs:
            waits = []
            for k, v in snap.items():
                if v > 0 and self.seen[e].get(k, 0) < v and not (k == e == "pe"):
                    self.seen[e][k] = v
                    waits.append((k, v))
            if waits:
                self.q[e].append((None, waits, None))

    def emit(self, block, sems):
        decos = {"pe": block.tensor, "dve": block.vector, "act": block.scalar,
                 "pool": block.gpsimd, "sp": block.sync}

        def make(engname):
            ops = self.q[engname]

            def body(e):
                for fn, waits, inc in ops:
                    for k, v in waits:
                        e.wait_ge(sems[k], v)
                    if fn is not None:
                        fn(e).then_inc(sems[inc[0]], inc[1])
            return body
        for engname, deco in decos.items():
            deco(make(engname))


class V:
    __slots__ = ("t", "ap")

    def __init__(self, t, ap):
        self.t = t
        self.ap = ap


class TT:
    def __init__(self, h):
        self.h = h
        self.b = Buf()

    def __getitem__(self, idx):
        return V(self, self.h[idx])


class _Half:
    __slots__ = ("b",)

    def __init__(self):
        self.b = Buf()


class TTH:
    def __init__(self, h):
        self.h = h
        self.halves = [_Half(), _Half()]

    def __getitem__(self, idx):
        cols = idx[1]
        start = cols.start or 0
        stop = cols.stop
        half = start // 512
        assert (stop - 1) // 512 == half, (start, stop)
        return V(self.halves[half], self.h[idx])


class Builder:
    def __init__(self, n_layers=L_ALL, stop=None, dbg=()):
        self.n_layers = n_layers
        self.stop = stop
        self.dbg = dbg
        self.nc = bass.Bass("TRN2", target_bir_lowering=False)
        self.P = Prog()
        self.es = contextlib.ExitStack()
        self.psi = 0
        self.dq = 0
        self.out_evs = []

    def sb(self, name, shape, dt):
        self.uid = getattr(self, "uid", 0) + 1
        return TT(self.es.enter_context(self.nc.sbuf_tensor(f"{name}_{self.uid}", list(shape), dt)))

    def dram_in(self, name, shape, dt=F32):
        return TT(self.nc.dram_tensor(name, list(shape), dt, kind="ExternalInput").ap())

    def dram_out(self, name, shape, dt=F32):
        return TT(self.nc.dram_tensor(name, list(shape), dt, kind="ExternalOutput").ap())

    def dram_tmp(self, name, shape, dt):
        return TT(self.nc.dram_tensor(name, list(shape), dt).ap())

    def ps(self, hold=False):
        while True:
            t = self.psb[self.psi]
            self.psi = (self.psi + 1) % len(self.psb)
            if not getattr(t, "held", False):
                break
        t.held = hold
        return t

    def mm(self, out, lhsT, rhs, start=True, stop=True):
        o, a, b = out.ap, lhsT.ap, rhs.ap
        self.P.op("pe", lambda e: e.matmul(o, lhsT=a, rhs=b, start=start, stop=stop),
                  reads=[lhsT.t.b, rhs.t.b], writes=[out.t.b])

    def act(self, out, in_, func, bias=None, scale=1.0, accum=None):
        o, i = out.ap, in_.ap
        reads = [in_.t.b]
        kw = {}
        if isinstance(bias, V):
            reads.append(bias.t.b)
            kw["bias"] = bias.ap
        elif bias is not None:
            kw["bias"] = bias
        if isinstance(scale, V):
            reads.append(scale.t.b)
            kw["scale"] = scale.ap
        else:
            kw["scale"] = scale
        writes = [out.t.b]
        if accum is not None:
            kw["accum_out"] = accum.ap
            writes.append(accum.t.b)
        self.P.op("act", lambda e: e.activation(out=o, in_=i, func=func, **kw), reads=reads, writes=writes)

    def ts(self, eng, out, in0, s1, s2=None, op0=ALU.mult, op1=None, accum=None):
        o, i = out.ap, in0.ap
        reads = [in0.t.b]
        a1 = s1
        if isinstance(s1, V):
            reads.append(s1.t.b)
            a1 = s1.ap
        a2 = s2
        if isinstance(s2, V):
            reads.append(s2.t.b)
            a2 = s2.ap
        kw = {}
        if op1 is not None:
            kw["op1"] = op1
        writes = [out.t.b]
        if accum is not None:
            kw["accum_out"] = accum.ap
            writes.append(accum.t.b)
        self.P.op(eng, lambda e: e.tensor_scalar(out=o, in0=i, scalar1=a1, scalar2=a2, op0=op0, **kw),
                  reads=reads, writes=writes)

    def tt(self, eng, out, in0, in1, op):
        o, a, b = out.ap, in0.ap, in1.ap
        self.P.op(eng, lambda e: e.tensor_tensor(out=o, in0=a, in1=b, op=op),
                  reads=[in0.t.b, in1.t.b], writes=[out.t.b])

    def stt(self, out, in0, scalar, in1, op0, op1):
        o, a, b = out.ap, in0.ap, in1.ap
        reads = [in0.t.b, in1.t.b]
        s = scalar
        if isinstance(scalar, V):
            reads.append(scalar.t.b)
            s = scalar.ap
        self.P.op("dve", lambda e: e.scalar_tensor_tensor(out=o, in0=a, scalar=s, in1=b, op0=op0, op1=op1),
                  reads=reads, writes=[out.t.b])

    def cp(self, eng, out, in_):
        o, i = out.ap, in_.ap
        if eng == "act":
            self.P.op("act", lambda e: e.activation(out=o, in_=i, func=AF.Copy), reads=[in_.t.b], writes=[out.t.b])
        else:
            self.P.op(eng, lambda e: e.tensor_copy(out=o, in_=i), reads=[in_.t.b], writes=[out.t.b])

    def memset(self, eng, out, val):
        o = out.ap
        self.P.op(eng, lambda e: e.memset(o, val), writes=[out.t.b])

    def scan(self, out, d0, d1):
        o, a, b = out.ap, d0.ap, d1.ap
        self.P.op("dve", lambda e: e.tensor_tensor_scan(out=o, data0=a, data1=b, initial=0.0, op0=ALU.mult, op1=ALU.add),
                  reads=[d0.t.b, d1.t.b], writes=[out.t.b])

    def recip(self, out, in_):
        o, i = out.ap, in_.ap
        self.P.op("dve", lambda e: e.reciprocal(out=o, in_=i), reads=[in_.t.b], writes=[out.t.b])

    def dma(self, out, in_, eng=None):
        if eng is None:
            eng = ("sp", "pool")[self.dq % 2]
            self.dq += 1
        o, i = out.ap, in_.ap
        return self.P.dma(eng, lambda e: e.dma_start(out=o, in_=i), reads=[in_.t.b], writes=[out.t.b])

    def build(self):
        nc = self.nc
        n_layers = self.n_layers
        self.x_in = self.dram_in("x", [T, D])
        self.w_in = self.dram_in("w_in", [L_ALL, D, NCOLS])
        self.w_up = self.dram_in("rw_w_up", [L_ALL, 128, D])
        self.a_up = self.dram_in("rw_a_up", [L_ALL, 64, D])
        self.g_up = self.dram_in("rw_g_up", [L_ALL, 128, D])
        self.v_down = self.dram_in("rw_v_down", [L_ALL - 1, D, 32])
        self.v_up = self.dram_in("rw_v_up", [L_ALL - 1, 32, D])
        self.proj_a = self.dram_in("proj_a", [L_ALL, D, D])
        self.proj_b = self.dram_in("proj_b", [L_ALL, D, D])
        self.w_out = self.dram_in("w_out", [L_ALL, D, D])
        self.router_w = self.dram_in("router_w", [L_ALL, D, NE])
        self.need_moe = self.stop is None or self.stop[1] == "moe"
        if self.need_moe:
            self.moe_w1 = self.dram_in("moe_w1", [L_ALL, NE, D, 2 * D])
            self.moe_w2 = self.dram_in("moe_w2", [L_ALL, NE, D, D])
            self.moe_b2 = self.dram_in("moe_b2", [L_ALL, NE, D])
        self.fm_pack = self.dram_in("fm_pack", [L_ALL, 128, NFM])
        self.bc_pack = self.dram_in("bc_pack", [L_ALL, 128, 4 * D + NE])
        self.b1_pack = self.dram_in("b1_pack", [L_ALL, 128, NE * 16])
        self.consts_d = self.dram_in("consts", [128, NCONST])
        self.y_out = self.dram_out("y", [T, D])
        self.xres = [self.dram_tmp("xres0", [T, D], F32), self.dram_tmp("xres1", [T, D], F32)]
        self.x1d = self.dram_tmp("x1d", [T, D], F32)
        self.oaT = self.dram_tmp("oaT", [D, T], BF16)
        self.obT = self.dram_tmp("obT", [D, T], BF16)
        self.mgT = self.dram_tmp("mgT", [D, T], BF16)
        self.vfT = self.dram_tmp("vfT", [D, T], F32)
        self.yfd = self.dram_tmp("yfd", [D, T], F32)
        self.twl_d = [self.dram_tmp("twlf_d", [64, T], BF16), self.dram_tmp("twlb_d", [64, T], BF16)]
        self.alo_d = self.dram_tmp("alo_d", [64, T], BF16)
        self.sigg_d = self.dram_tmp("sigg_d", [128, T], BF16)
        self.xvd_d = self.dram_tmp("xvd_d", [32, T], BF16)
        self.dbg_out = {}
        for name, shape, dt in self.dbg:
            self.dbg_out[name] = self.dram_out("dbg_" + name, shape, dt)

        with self.es:
            sems = {}
            for k in self.P.sem_keys:
                sems[k] = self.es.enter_context(nc.semaphore("s_" + k))
            self.psb = [TT(self.es.enter_context(nc.psum_tensor(f"ps{i}", [128, 512], F32))) for i in range(8)]
            self.XT = self.sb("XT", [128, 8, T], BF16)
            self.CST = self.sb("CST", [128, NCONST], F32)
            self.IDB = self.sb("IDB", [128, 128], BF16)
            self.BDB = self.sb("BDB", [128, 128], BF16)
            self.ONB = self.sb("ONB", [128, 128], BF16)
            self.FM = self.sb("FM", [128, NFM], F32)
            self.DER = self.sb("DER", [128, 96], F32)
            self.dma(self.CST[:, :], self.consts_d[:, :])
            self.cp("dve", self.IDB[:, :], self.CST[:, C_ID:C_ID + 128])
            self.cp("dve", self.BDB[:, :], self.CST[:, C_BD:C_BD + 128])
            self.cp("dve", self.ONB[:, :], self.CST[:, C_ONES:C_ONES + 128])

            for l in range(n_layers):
                src = self.x_in if l == 0 else self.xres[l % 2]
                dst = self.y_out if l == L_ALL - 1 else self.xres[(l + 1) % 2]
                self.layer(l, src, dst)
                if self.stop is not None and self.stop[0] == l:
                    break
            final_evs = []
            for name, (srcT, sl) in getattr(self, "dbg_src", {}).items():
                final_evs.append(self.dma(self.dbg_out[name][sl], srcT[sl], eng="sp"))
            self.P.barrier()
            with nc.Block() as block:
                self.P.emit(block, sems)
        return nc

    def params(self, l):
        self.dma(self.FM[:, :], self.fm_pack[l])
        DER, FM = self.DER, self.FM
        self.ts("dve", DER[:, 0:27], FM[:, FM_MU:FM_MU + 27], -1.0, 1.0, ALU.mult, ALU.add)
        self.ts("dve", DER[:, 27:54], FM[:, FM_MU:FM_MU + 27], 0.5, None, ALU.mult)
        self.ts("dve", DER[:, 54:62], FM[:, FM_KA:FM_KA + 8], -1.0, 1.0, ALU.mult, ALU.add)
        E = self.sb(f"lbE{l}", [128, 32], F32)
        self.act(E[:, :], FM[:, FM_LBL:FM_LBL + 32], AF.Exp)
        self.tt("dve", DER[:, 78:86], E[:, 0:8], E[:, 8:16], ALU.add)
        self.tt("dve", DER[:, 78:86], DER[:, 78:86], E[:, 16:24], ALU.add)
        self.tt("dve", DER[:, 78:86], DER[:, 78:86], E[:, 24:32], ALU.add)
        self.recip(DER[:, 78:86], DER[:, 78:86])
        self.memset("dve", DER[:, 62:70], 0.0)
        for i in range(1, l + 1):
            self.tt("dve", DER[:, 62:70], DER[:, 62:70], E[:, 8 * i:8 * i + 8], ALU.add)
        self.tt("dve", DER[:, 62:70], DER[:, 62:70], DER[:, 78:86], ALU.mult)
        self.ts("dve", DER[:, 70:78], DER[:, 62:70], -1.0, 1.0, ALU.mult, ALU.add)
        self.ts("dve", DER[:, 88:89], FM[:, FM_MUB:FM_MUB + 1], -1.0, 1.0, ALU.mult, ALU.add)
        self.ts("dve", DER[:, 89:90], FM[:, FM_MUB:FM_MUB + 1], 0.5, None, ALU.mult)
        self.memset("dve", DER[:, 86:87], NORM_EPS)
        self.memset("dve", DER[:, 87:88], RW_LN_EPS)

    def load_w(self, stage, wbf, src_tt, l, col0, n, dst_col, conv_eng):
        src = src_tt.h[l][:, col0:col0 + n].rearrange("(c p) n -> p c n", p=128)
        self.dma(stage[:, :, 0:n], V(src_tt, src))
        self.cp(conv_eng, wbf[:, :, dst_col:dst_col + n], stage[:, :, 0:n])

    def proj_fm(self, out, wbf, col0, m, t0, n, xt=None):
        xt = xt or self.XT
        for c in range(8):
            self.mm(out, wbf[:, c, col0:col0 + m], xt[:, c, t0:t0 + n], start=(c == 0), stop=(c == 7))

    def build_xt(self, src):
        XL = [self.sb(f"XL{i}_{id(src) % 1000}", [128, D], F32) for i in range(2)]
        for i in range(T // 128):
            xl = XL[i % 2]
            self.dma(xl[:, :], src[i * 128:(i + 1) * 128, :])
            for half in range(2):
                p = self.ps()
                for c in range(4):
                    cc = half * 4 + c
                    self.mm(p[:, c * 128:(c + 1) * 128], xl[:, cc * 128:(cc + 1) * 128], self.CST[:, C_ID:C_ID + 128])
                for c in range(4):
                    cc = half * 4 + c
                    self.cp("act" if c % 2 else "dve", self.XT[:, cc, i * 128:(i + 1) * 128], p[:, c * 128:(c + 1) * 128])

    def layer(self, l, src, dst):
        es_outer = self.es
        self.params(l)
        with contextlib.ExitStack() as es:
            self.es = es
            self.build_xt(src)
        self.es = es_outer
        self.P.barrier()
        if self.stop == (l, "xt"):
            return
        with contextlib.ExitStack() as es:
            self.es = es
            self.hgrn(l)
        self.es = es_outer
        self.P.barrier()
        if self.stop == (l, "hgrn"):
            return
        with contextlib.ExitStack() as es:
            self.es = es
            self.rwkv(l)
        self.es = es_outer
        self.P.barrier()
        if self.stop == (l, "rwkv"):
            return
        with contextlib.ExitStack() as es:
            self.es = es
            self.merge(l, src)
        self.es = es_outer
        self.P.barrier()
        if self.stop == (l, "merge"):
            return
        with contextlib.ExitStack() as es:
            self.es = es
            self.moe(l, dst)
        self.es = es_outer
        self.P.barrier()

    def hgrn(self, l):
        sb = self.sb
        CST, DER, FM, XT = self.CST, self.DER, self.FM, self.XT
        WS = [sb(f"h_ws{i}", [128, 8, 128], F32) for i in range(2)]
        WH = [sb(f"h_wh{i}", [128, 8, 640], BF16) for i in range(2)]
        OF = sb("h_of", [128, T], F32)
        S32 = sb("h_s32", [128, 128], F32)
        SBF = sb("h_sbf", [128, 128], BF16)
        Fm = sb("h_f", [128, 512], F32)
        LF = sb("h_lf", [128, 512], F32)
        KX = sb("h_kx", [128, 512], F32)
        G = sb("h_g", [128, 512], F32)
        D1 = sb("h_d1", [128, 512], F32)
        G2 = sb("h_g2", [128, 512], F32)
        EN = sb("h_en", [128, 512], F32)
        KT32 = sb("h_kt32", [128, 512], F32)
        KH = sb("h_kh", [128, 512], BF16)
        SOG = sb("h_sog", [128, 512], F32)
        O = sb("h_o", [128, 512], F32)
        SQ = sb("h_sq", [128, 512], BF16)
        RS = sb("h_rs", [128, 512], F32)
        OUTB = [sb(f"h_outb{i}", [128, 512], BF16) for i in range(2)]
        EP = [sb(f"h_ep{i}", [128, 512], F32) for i in range(2)]
        QT = [sb(f"h_qt{i}", [128, 512], BF16) for i in range(2)]
        KT = [sb(f"h_kt{i}", [128, 512], BF16) for i in range(2)]
        VT = [sb(f"h_vt{i}", [64, 1024], BF16) for i in range(2)]
        KHT = [sb(f"h_kht{i}", [64, 1024], BF16) for i in range(2)]
        AT = [sb(f"h_at{i}", [64, 512], BF16) for i in range(2)]
        cnt = 0
        for h in range(8):
            wh = WH[h % 2]
            for qi in range(5):
                self.load_w(WS[qi % 2], wh, self.w_in, l, qi * 1024 + h * 128, 128, qi * 128, "pool")
            for d in (0, 1):
                self.memset("dve", S32[:, :], 0.0)
                self.memset("dve", SBF[:, :], 0.0)
                for bi in range(NB):
                    blk = bi if d == 0 else NB - 1 - bi
                    t0 = blk * 512
                    k = cnt % 2
                    cnt += 1
                    ep, qt, kt, vt, kht, at = EP[k], QT[k], KT[k], VT[k], KHT[k], AT[k]
                    pq = self.ps()
                    self.proj_fm(pq[:, :], wh, 0, 128, t0, 512)
                    pf = self.ps()
                    self.proj_fm(pf[:, :], wh, 128 * (1 + d), 128, t0, 512)
                    for half in range(2):
                        pv = self.ps()
                        for cc in range(4):
                            c = half * 4 + cc
                            for kk in range(8):
                                self.mm(pv[0:64, cc * 128:(cc + 1) * 128], XT[:, kk, t0 + c * 64:t0 + c * 64 + 64],
                                        wh[:, kk, 384:512], start=(kk == 0), stop=(kk == 7))
                        self.cp("act", vt[0:64, half * 512:(half + 1) * 512], pv[0:64, :])
                    if d == 1:
                        pog = self.ps()
                        self.proj_fm(pog[:, :], wh, 512, 128, t0, 512)
                        self.act(SOG[:, :], pog[:, :], AF.Silu)
                    self.act(Fm[:, :], pf[:, :], AF.Sigmoid)
                    self.ts("dve", Fm[:, :], Fm[:, :], DER[:, 70 + h:71 + h], DER[:, 62 + h:63 + h], ALU.mult, ALU.add)
                    self.act(LF[:, :], Fm[:, :], AF.Ln)
                    self.ts("dve", KX[:, :], Fm[:, :], -1.0, 1.0, ALU.mult, ALU.add)
                    self.scan(G[:, :], CST[:, C_RESET:C_RESET + 512], LF[:, :])
                    g = G
                    if d == 1:
                        self.tt("dve", D1[:, :], LF[:, :], G[:, :], ALU.subtract)
                        for c in range(8):
                            self.ts("dve", G2[:, c * 64:(c + 1) * 64], D1[:, c * 64:(c + 1) * 64],
                                    G[:, c * 64 + 63:c * 64 + 64], None, ALU.add)
                        g = G2
                    self.act(ep[:, :], g[:, :], AF.Exp)
                    self.act(EN[:, :], g[:, :], AF.Exp, scale=-1.0)
                    self.tt("dve", qt[:, :], pq[:, :], ep[:, :], ALU.mult)
                    self.tt("dve", KT32[:, :], KX[:, :], EN[:, :], ALU.mult)
                    self.cp("dve", kt[:, :], KT32[:, :])
                    for c in range(8):
                        ec = c * 64 + 63 if d == 0 else c * 64
                        self.ts("dve" if c % 2 else "pool", KH[:, c * 64:(c + 1) * 64], KT32[:, c * 64:(c + 1) * 64],
                                ep[:, ec:ec + 1], None, ALU.mult)
                    for half in range(2):
                        pk = self.ps()
                        for cc in range(4):
                            c = half * 4 + cc
                            self.mm(pk[0:64, cc * 128:(cc + 1) * 128], KH[:, c * 64:(c + 1) * 64], self.IDB[:, :])
                        self.cp("act", kht[0:64, half * 512:(half + 1) * 512], pk[0:64, :])
                    psc = self.ps()
                    for c in range(8):
                        self.mm(psc[0:64, c * 64:(c + 1) * 64], kt[:, c * 64:(c + 1) * 64], qt[:, c * 64:(c + 1) * 64])
                    mcol = C_FI if d == 0 else C_BI
                    self.tt("dve", at[0:64, :], psc[0:64, :], CST[0:64, mcol:mcol + 512], ALU.mult)
                    po = self.ps(hold=True)
                    for ci in range(8):
                        c = ci if d == 0 else 7 - ci
                        ec = c * 64 + 63 if d == 0 else c * 64
                        self.mm(po[:, c * 64:(c + 1) * 64], vt[0:64, c * 128:(c + 1) * 128], at[0:64, c * 64:(c + 1) * 64],
                                start=True, stop=False)
                        self.mm(po[:, c * 64:(c + 1) * 64], SBF[:, :], qt[:, c * 64:(c + 1) * 64], start=False, stop=True)
                        pss = self.ps()
                        self.mm(pss[:, 0:128], kht[0:64, c * 128:(c + 1) * 128], vt[0:64, c * 128:(c + 1) * 128])
                        self.stt(S32[:, :], S32[:, :], ep[:, ec:ec + 1], pss[:, 0:128], ALU.mult, ALU.add)
                        self.cp("act", SBF[:, :], S32[:, :])
                    po.held = False
                    if d == 0:
                        self.cp("act", OF[:, t0:t0 + 512], po[:, :])
                    else:
                        ob = OUTB[bi % 2]
                        self.tt("dve", O[:, :], po[:, :], OF[:, t0:t0 + 512], ALU.add)
                        self.act(SQ[:, :], O[:, :], AF.Square)
                        pn = self.ps()
                        self.mm(pn[:, :], self.ONB[:, :], SQ[:, :])
                        self.act(RS[:, :], pn[:, :], AF.Sqrt, bias=DER[:, 86:87], scale=1.0 / 128.0)
                        self.recip(RS[:, :], RS[:, :])
                        self.tt("dve", O[:, :], O[:, :], RS[:, :], ALU.mult)
                        self.stt(ob[:, :], O[:, :], FM[:, FM_NW:FM_NW + 1], SOG[:, :], ALU.mult, ALU.mult)
                        self.dma(self.oaT[h * 128:(h + 1) * 128, t0:t0 + 512], ob[:, :])
        self.dbg_src = getattr(self, "dbg_src", {})
        if "oaT" in self.dbg_out:
            self.dbg_src["oaT"] = (self.oaT, (slice(None), slice(None)))
        if "OF" in self.dbg_out:
            self.dbg_src["OF"] = (OF, (slice(None), slice(None)))

    def shift_block(self, pm, hl, hr, rows, omm, hmu, PADB, TMPA, TMPB, OUT):
        R = slice(0, rows)
        self.cp("act", PADB[R, 1:513], pm)
        if hl is None:
            self.memset("dve", PADB[R, 0:1], 0.0)
        else:
            self.cp("dve", PADB[R, 0:1], hl)
        if hr is None:
            self.memset("dve", PADB[R, 513:514], 0.0)
        else:
            self.cp("dve", PADB[R, 513:514], hr)
        self.tt("dve", TMPA[R, :], PADB[R, 0:512], PADB[R, 2:514], ALU.add)
        self.act(TMPB[R, :], PADB[R, 1:513], AF.Identity, scale=omm)
        self.stt(OUT, TMPA[R, :], hmu, TMPB[R, :], ALU.mult, ALU.add)

    def proj_shift(self, wbf, col0, rows, blk, omm, hmu, PADB, TMPA, TMPB, OUT):
        XT = self.XT
        t0 = blk * 512
        pm = self.ps()
        self.proj_fm(pm[0:rows, :], wbf, col0, rows, t0, 512)
        ph = self.ps()
        hl = hr = None
        if blk > 0:
            for c in range(8):
                self.mm(ph[0:rows, 0:1], wbf[:, c, col0:col0 + rows], XT[:, c, t0 - 1:t0], start=(c == 0), stop=(c == 7))
            hl = ph[0:rows, 0:1]
        if blk < NB - 1:
            for c in range(8):
                self.mm(ph[0:rows, 1:2], wbf[:, c, col0:col0 + rows], XT[:, c, t0 + 512:t0 + 513], start=(c == 0), stop=(c == 7))
            hr = ph[0:rows, 1:2]
        self.shift_block(pm[0:rows, :], hl, hr, rows, omm, hmu, PADB, TMPA, TMPB, OUT)

    def rwkv(self, l):
        sb = self.sb
        CST, DER, FM, XT = self.CST, self.DER, self.FM, self.XT
        f32t = lambda n: sb(n, [128, 512], F32)
        bft = lambda n: sb(n, [128, 512], BF16)
        PADB = sb("r_padb", [128, 514], F32)
        TMPA, TMPB = f32t("r_tmpa"), f32t("r_tmpb")
        PADB2 = sb("r_padb2", [128, 514], F32)
        TMPA2, TMPB2 = f32t("r_tmpa2"), f32t("r_tmpb2")
        WS = [sb(f"r_ws{i}", [128, 8, 128], F32) for i in range(2)]
        WUP = sb("r_wup", [128, D], BF16)
        AUP = sb("r_aup", [64, D], BF16)
        GUP = sb("r_gup", [128, D], BF16)
        WUPB = sb("r_wupb", [64, D], BF16)
        LOB = sb("r_lob", [128, 512], BF16)
        TWLt = sb("r_twlt", [64, 512], BF16)
        ALOt = sb("r_alot", [64, 512], BF16)
        SIGGt = sb("r_siggt", [128, 512], BF16)
        if l > 0:
            VUP = sb("r_vup", [32, D], BF16)
            XVDt = sb("r_xvdt", [32, 512], BF16)
        es_keep = self.es
        with contextlib.ExitStack() as es0:
            self.es = es0
            WL = sb("r_wl", [128, 8, 320], BF16)
            self.load_w(WS[0], WL, self.w_in, l, RWB + 3072, 128, 0, "pool")
            self.load_w(WS[1], WL, self.w_in, l, RWB + 3200, 64, 128, "pool")
            self.load_w(WS[0], WL, self.w_in, l, RWB + 3264, 128, 192, "pool")
            LST = sb("r_lst", [128, D], F32)
            self.dma(LST[:, :], self.w_up[l])
            self.cp("dve", WUP[:, :], LST[:, :])
            self.dma(LST[0:64, :], V(self.w_up, self.w_up.h[l][64:128, :]))
            self.cp("dve", WUPB[0:64, :], LST[0:64, :])
            self.dma(LST[0:64, :], self.a_up[l])
            self.cp("dve", AUP[0:64, :], LST[0:64, :])
            self.dma(LST[:, :], self.g_up[l])
            self.cp("dve", GUP[:, :], LST[:, :])
            if l > 0:
                self.dma(LST[0:32, :], self.v_up[l - 1])
                self.cp("dve", VUP[0:32, :], LST[0:32, :])
                VDS = sb("r_vds", [128, 8, 32], F32)
                VD = sb("r_vd", [128, 8, 32], BF16)
                src = self.v_down.h[l - 1].rearrange("(c p) n -> p c n", p=128)
                self.dma(VDS[:, :, :], V(self.v_down, src))
                self.cp("dve", VD[:, :, :], VDS[:, :, :])
            SHT = f32t("r_sht")
            for blk in range(NB):
                t0 = blk * 512
                self.proj_shift(WL, 0, 64, blk, DER[0:64, 24:25], DER[0:64, 51:52], PADB, TMPA, TMPB, SHT[0:64, :])
                self.act(LOB[0:64, :], SHT[0:64, :], AF.Tanh)
                self.dma(self.twl_d[0][:, t0:t0 + 512], LOB[0:64, :])
                self.proj_shift(WL, 64, 64, blk, DER[0:64, 88:89], DER[0:64, 89:90], PADB, TMPA, TMPB, SHT[0:64, :])
                self.act(LOB[0:64, :], SHT[0:64, :], AF.Tanh)
                self.dma(self.twl_d[1][:, t0:t0 + 512], LOB[0:64, :])
                self.proj_shift(WL, 128, 64, blk, DER[0:64, 25:26], DER[0:64, 52:53], PADB, TMPA, TMPB, SHT[0:64, :])
                self.cp("act", LOB[0:64, :], SHT[0:64, :])
                self.dma(self.alo_d[:, t0:t0 + 512], LOB[0:64, :])
                self.proj_shift(WL, 192, 128, blk, DER[:, 26:27], DER[:, 53:54], PADB, TMPA, TMPB, SHT[:, :])
                self.act(LOB[:, :], SHT[:, :], AF.Sigmoid)
                self.dma(self.sigg_d[:, t0:t0 + 512], LOB[:, :])
                if l > 0:
                    pm = self.ps()
                    self.proj_fm(pm[0:32, :], VD, 0, 32, t0, 512)
                    self.cp("act", LOB[0:32, :], pm[0:32, :])
                    self.dma(self.xvd_d[:, t0:t0 + 512], LOB[0:32, :])
        self.es = es_keep
        self.P.barrier()
        import os
        RS = int(os.environ.get("RW_STOP", "99"))
        if RS == 0:
            return
        WP = [sb("r_wp0", [128, 8, 384], BF16)] * 2
        SH = [f32t(f"r_sh{q}") for q in range(3)]
        LW, A, VF, VV, KKN, KFIN, BV = [f32t("r_" + n) for n in ("lw", "a", "vf", "vv", "kkn", "kfin", "bv")]
        G, G2, EP, EN, EX, BT32, KT32, YB, YC = [f32t("r_" + n) for n in ("g", "g2", "ep", "en", "ex", "bt32", "kt32", "yb", "yc")]
        KSQ, ATt, BTt, KTt, RTt, BH, KHh, VB = [bft("r_" + n) for n in ("ksq", "at", "bt", "kt", "rt", "bh", "khh", "vb")]
        OB = [bft(f"r_ob{i}") for i in range(2)]
        h64 = lambda n: TTH(sb(n, [64, 1024], BF16).h)
        VTK, BHT, KHT, MAK, MRB, MRK, TTm = [h64("r_" + n) for n in ("vtk", "bht", "kht", "mak", "mrb", "mrk", "ttm")]
        XA, XtA, XB_, XtB = [h64("r_" + n) for n in ("xa", "xta", "xb", "xtb")]
        U = sb("r_u", [64, 128], BF16)
        UM = h64("r_um")
        Pm = sb("r_pm", [64, 128], BF16)
        S32 = sb("r_s32", [64, 128], F32)
        SBF = sb("r_sbf", [64, 128], BF16)
        AT1, BT1, KT1, RT1 = [sb("r_" + n, [64, 512], BF16) for n in ("at1", "bt1", "kt1", "rt1")]
        EP1 = sb("r_ep1", [64, 512], F32)
        YH = sb("r_yh", [64, 2, 512], F32)
        SH0 = CST[0:64, C_ID:C_ID + 128]
        SH1 = CST[0:64, C_SH1:C_SH1 + 128]
        BDF = CST[:, C_BD:C_BD + 128]
        for j in range(8):
            wp = WP[j % 2]
            for q in range(3):
                self.load_w(WS[q % 2], wp, self.w_in, l, RWB + q * 1024 + 128 * j, 128, q * 128, "pool")
            jc = slice(128 * j, 128 * j + 128)
            for d in (0, 1):
                self.memset("dve", S32[:, :], 0.0)
                self.memset("dve", SBF[:, :], 0.0)
                for bi in range(NB):
                    blk = bi if d == 0 else NB - 1 - bi
                    t0 = blk * 512
                    tb = slice(t0, t0 + 512)
                    for q in range(3):
                        mc = q * 8 + j
                        if q == 1:
                            self.proj_shift(wp, q * 128, 128, blk, DER[:, mc:mc + 1], DER[:, 27 + mc:28 + mc], PADB2, TMPA2, TMPB2, SH[q][:, :])
                        else:
                            self.proj_shift(wp, q * 128, 128, blk, DER[:, mc:mc + 1], DER[:, 27 + mc:28 + mc], PADB, TMPA, TMPB, SH[q][:, :])
                    if RS == 1:
                        return
                    pz = self.ps()
                    self.dma(TWLt[0:64, :], self.twl_d[d][:, tb])
                    self.mm(pz[:, :], (WUP if d == 0 else WUPB)[0:64, jc], TWLt[0:64, :])
                    self.act(LW[:, :], pz[:, :], AF.Sigmoid, bias=FM[:, FM_W0 + d * 8 + j:FM_W0 + d * 8 + j + 1])
                    self.ts("dve", LW[:, :], LW[:, :], -C_W, None, ALU.mult)
                    pa = self.ps()
                    self.dma(ALOt[0:64, :], self.alo_d[:, tb])
                    self.mm(pa[:, :], AUP[0:64, jc], ALOt[0:64, :])
                    self.act(A[:, :], pa[:, :], AF.Sigmoid, bias=FM[:, FM_A0 + j:FM_A0 + j + 1])
                    if l == 0:
                        if d == 0:
                            self.dma(self.vfT[jc, tb], SH[2][:, :])
                        Vv = SH[2]
                    else:
                        pv = self.ps()
                        self.dma(XVDt[0:32, :], self.xvd_d[:, tb])
                        self.mm(pv[:, :], VUP[0:32, jc], XVDt[0:32, :])
                        self.act(TMPA[:, :], pv[:, :], AF.Sigmoid, bias=FM[:, FM_V0 + j:FM_V0 + j + 1])
                        self.dma(VF[:, :], self.vfT[jc, tb])
                        self.tt("dve", TMPB[:, :], VF[:, :], SH[2][:, :], ALU.subtract)
                        self.tt("dve", TMPB[:, :], TMPB[:, :], TMPA[:, :], ALU.mult)
                        self.tt("dve", VV[:, :], SH[2][:, :], TMPB[:, :], ALU.add)
                        Vv = VV
                    if RS == 2:
                        return
                    self.ts("dve", TMPB[:, :], SH[1][:, :], FM[:, FM_KK + j:FM_KK + j + 1], None, ALU.mult)
                    self.act(KSQ[:, :], TMPB[:, :], AF.Square)
                    pss = self.ps()
                    self.mm(pss[:, :], self.BDB[:, :], KSQ[:, :])
                    self.act(TMPA[:, :], pss[:, :], AF.Sqrt)
                    self.ts("dve", TMPA[:, :], TMPA[:, :], 1e-12, None, ALU.max)
                    self.recip(TMPA[:, :], TMPA[:, :])
                    self.tt("dve", KKN[:, :], TMPB[:, :], TMPA[:, :], ALU.mult)
                    self.ts("dve", TMPA[:, :], A[:, :], FM[:, FM_KA + j:FM_KA + j + 1], DER[:, 54 + j:55 + j], ALU.mult, ALU.add)
                    self.tt("dve", KFIN[:, :], TMPA[:, :], SH[1][:, :], ALU.mult)
                    self.tt("dve", BV[:, :], KKN[:, :], A[:, :], ALU.mult)
                    if RS == 3:
                        return
                    self.scan(G[:, :], CST[:, C_RESET:C_RESET + 512], LW[:, :])
                    g = G
                    if d == 1:
                        self.tt("dve", TMPA[:, :], LW[:, :], G[:, :], ALU.subtract)
                        for c in range(8):
                            self.ts("dve", G2[:, c * 64:(c + 1) * 64], TMPA[:, c * 64:(c + 1) * 64],
                                    G[:, c * 64 + 63:c * 64 + 64], None, ALU.add)
                        g = G2
                    self.tt("dve", TMPB[:, :], g[:, :], LW[:, :], ALU.subtract)
                    self.act(EP[:, :], g[:, :], AF.Exp)
                    self.act(EN[:, :], g[:, :], AF.Exp, scale=-1.0)
                    self.act(EX[:, :], TMPB[:, :], AF.Exp)
                    self.stt(ATt[:, :], KKN[:, :], -1.0, EX[:, :], ALU.mult, ALU.mult)
                    self.tt("dve", BT32[:, :], BV[:, :], EN[:, :], ALU.mult)
                    self.cp("dve", BTt[:, :], BT32[:, :])
                    self.tt("dve", KT32[:, :], KFIN[:, :], EN[:, :], ALU.mult)
                    self.cp("dve", KTt[:, :], KT32[:, :])
                    self.tt("dve", RTt[:, :], SH[0][:, :], EP[:, :], ALU.mult)
                    for c in range(8):
                        ec = c * 64 + 63 if d == 0 else c * 64
                        cs = slice(c * 64, c * 64 + 64)
                        self.ts("dve", BH[:, cs], BT32[:, cs], EP[:, ec:ec + 1], None, ALU.mult)
                        self.ts("pool", KHh[:, cs], KT32[:, cs], EP[:, ec:ec + 1], None, ALU.mult)
                    self.cp("act", VB[:, :], Vv[:, :])
                    self.dma(AT1[0:64, :], ATt[64:128, :])
                    self.dma(BT1[0:64, :], BTt[64:128, :])
                    self.dma(KT1[0:64, :], KTt[64:128, :])
                    self.dma(RT1[0:64, :], RTt[64:128, :])
                    self.dma(EP1[0:64, :], EP[64:128, :])
                    HA = (ATt, AT1)
                    HB = (BTt, BT1)
                    HK = (KTt, KT1)
                    HR = (RTt, RT1)
                    HEP = (EP, EP1)
                    if RS == 4:
                        return
                    for srcb, dstb in ((VB, VTK), (BH, BHT), (KHh, KHT)):
                        for half in range(2):
                            pk = self.ps()
                            for cc in range(4):
                                c = half * 4 + cc
                                self.mm(pk[0:64, cc * 128:(cc + 1) * 128], srcb[:, c * 64:(c + 1) * 64], self.IDB[:, :])
                            self.cp("act" if half else "dve", dstb[0:64, half * 512:(half + 1) * 512], pk[0:64, :])
                    m_strict = C_FS if d == 0 else C_BS
                    m_strict_T = C_BS if d == 0 else C_FS
                    m_incl = C_FI if d == 0 else C_BI
                    for half in range(2):
                        banks = [self.ps() for _ in range(5)]
                        for cc in range(4):
                            c = half * 4 + cc
                            cs = slice(c * 64, c * 64 + 64)
                            for e in range(2):
                                R = slice(64 * e, 64 * e + 64)
                                co = slice((cc * 2 + e) * 64, (cc * 2 + e) * 64 + 64)
                                Z = slice(0, 64)
                                self.mm(banks[0][0:64, co], HB[e][Z, cs], HA[e][Z, cs])
                                self.mm(banks[1][0:64, co], HA[e][Z, cs], HB[e][Z, cs])
                                self.mm(banks[2][0:64, co], HK[e][Z, cs], HA[e][Z, cs])
                                self.mm(banks[3][0:64, co], HB[e][Z, cs], HR[e][Z, cs])
                                self.mm(banks[4][0:64, co], HK[e][Z, cs], HR[e][Z, cs])
                        hs = slice(half * 512, half * 512 + 512)
                        self.tt("dve", XA[0:64, hs], banks[0][0:64, :], CST[0:64, m_strict:m_strict + 512], ALU.mult)
                        self.tt("dve", XtA[0:64, hs], banks[1][0:64, :], CST[0:64, m_strict_T:m_strict_T + 512], ALU.mult)
                        self.tt("dve", MAK[0:64, hs], banks[2][0:64, :], CST[0:64, m_strict:m_strict + 512], ALU.mult)
                        self.tt("dve", MRB[0:64, hs], banks[3][0:64, :], CST[0:64, m_incl:m_incl + 512], ALU.mult)
                        self.tt("dve", MRK[0:64, hs], banks[4][0:64, :], CST[0:64, m_incl:m_incl + 512], ALU.mult)
                        self.tt("dve", TTm[0:64, hs], XA[0:64, hs], CST[0:64, C_I8:C_I8 + 512], ALU.add)
                    if RS == 5:
                        return
                    Xc, Xtc, Xn, Xtn = XA, XtA, XB_, XtB
                    for lev in range(5):
                        for half in range(2):
                            hs = slice(half * 512, half * 512 + 512)
                            p2 = self.ps()
                            for qq in range(8):
                                co = slice(qq * 64, qq * 64 + 64)
                                sc = slice(half * 512 + qq * 64, half * 512 + qq * 64 + 64)
                                self.mm(p2[0:64, co], Xc[0:64, sc], Xtc[0:64, sc])
                            self.cp("act", Xtn[0:64, hs], p2[0:64, :])
                            if lev < 4:
                                p1 = self.ps()
                                for qq in range(8):
                                    co = slice(qq * 64, qq * 64 + 64)
                                    sc = slice(half * 512 + qq * 64, half * 512 + qq * 64 + 64)
                                    self.mm(p1[0:64, co], Xtc[0:64, sc], Xc[0:64, sc])
                                self.cp("act" if half else "dve", Xn[0:64, hs], p1[0:64, :])
                        for half in range(2):
                            hs = slice(half * 512, half * 512 + 512)
                            p3 = self.ps()
                            for qq in range(8):
                                co = slice(qq * 64, qq * 64 + 64)
                                sc = slice(half * 512 + qq * 64, half * 512 + qq * 64 + 64)
                                self.mm(p3[0:64, co], Xtn[0:64, sc], TTm[0:64, sc])
                            self.tt("dve", TTm[0:64, hs], TTm[0:64, hs], p3[0:64, :], ALU.add)
                        Xc, Xtc, Xn, Xtn = Xn, Xtn, Xc, Xtc
                    if RS == 6:
                        return
                    for half in range(2):
                        pum = self.ps()
                        for cc in range(4):
                            c = half * 4 + cc
                            for e in range(2):
                                co = slice((c * 2 + e) * 64, (c * 2 + e) * 64 + 64)
                                self.mm(pum[0:64, cc * 128 + 64 * e:cc * 128 + 64 * e + 64], MAK[0:64, co],
                                        VTK[0:64, c * 128 + 64 * e:c * 128 + 64 * e + 64])
                        self.cp("act", UM[0:64, half * 512:(half + 1) * 512], pum[0:64, :])
                    if d == 1:
                        self.dma(YC[:, :], self.yfd[jc, tb])
                    for ci in range(8):
                        c = ci if d == 0 else 7 - ci
                        ec = c * 64 + 63 if d == 0 else c * 64
                        cs = slice(c * 64, c * 64 + 64)
                        Z = slice(0, 64)
                        pu = self.ps()
                        for e in range(2):
                            E = slice(64 * e, 64 * e + 64)
                            self.mm(pu[Z, E], HA[e][Z, cs], SBF[Z, E], start=True, stop=True)
                        self.cp("act", U[Z, :], pu[Z, 0:128])
                        pp = self.ps()
                        for e in range(2):
                            E = slice(64 * e, 64 * e + 64)
                            co = slice((c * 2 + e) * 64, (c * 2 + e) * 64 + 64)
                            ve = slice(c * 128 + 64 * e, c * 128 + 64 * e + 64)
                            self.mm(pp[Z, E], TTm[Z, co], U[Z, E], start=True, stop=False)
                            self.mm(pp[Z, E], TTm[Z, co], UM[Z, ve], start=False, stop=True)
                        self.cp("dve", Pm[Z, :], pp[Z, 0:128])
                        py = self.ps()
                        for e in range(2):
                            E = slice(64 * e, 64 * e + 64)
                            co = slice((c * 2 + e) * 64, (c * 2 + e) * 64 + 64)
                            ve = slice(c * 128 + 64 * e, c * 128 + 64 * e + 64)
                            self.mm(py[Z, E], SBF[Z, E], HR[e][Z, cs], start=True, stop=False)
                            self.mm(py[Z, E], Pm[Z, E], MRB[Z, co], start=False, stop=False)
                            self.mm(py[Z, E], VTK[Z, ve], MRK[Z, co], start=False, stop=True)
                        for e in range(2):
                            E = slice(64 * e, 64 * e + 64)
                            self.cp("act" if e else "dve", YH[Z, e, cs], py[Z, E])
                        pst = self.ps()
                        for e in range(2):
                            E = slice(64 * e, 64 * e + 64)
                            ve = slice(c * 128 + 64 * e, c * 128 + 64 * e + 64)
                            self.mm(pst[Z, E], BHT[Z, ve], Pm[Z, E], start=True, stop=False)
                            self.mm(pst[Z, E], KHT[Z, ve], VTK[Z, ve], start=False, stop=True)
                        for e in range(2):
                            E = slice(64 * e, 64 * e + 64)
                            self.stt(S32[Z, E], S32[Z, E], HEP[e][Z, ec:ec + 1], pst[Z, E], ALU.mult, ALU.add)
                        self.cp("act", SBF[Z, :], S32[Z, :])
                    pyb = self.ps()
                    for n4 in range(4):
                        ns = slice(n4 * 128, n4 * 128 + 128)
                        self.mm(pyb[:, ns], SH0, YH[0:64, 0, ns], start=True, stop=False)
                        self.mm(pyb[:, ns], SH1, YH[0:64, 1, ns], start=False, stop=True)
                    if d == 0:
                        self.cp("act", YB[:, :], pyb[:, :])
                    else:
                        self.tt("dve", YB[:, :], pyb[:, :], YC[:, :], ALU.add)
                    if RS == 7:
                        return
                    if d == 0:
                        self.dma(self.yfd[jc, tb], YB[:, :])
                    else:
                        ob = OB[bi % 2]
                        pm = self.ps()
                        for n4 in range(4):
                            self.mm(pm[:, n4 * 128:n4 * 128 + 128], BDF, YB[:, n4 * 128:n4 * 128 + 128])
                        self.stt(YC[:, :], pm[:, :], -1.0 / 64.0, YB[:, :], ALU.mult, ALU.add)
                        self.act(EN[:, :], YC[:, :], AF.Square)
                        pv2 = self.ps()
                        for n4 in range(4):
                            self.mm(pv2[:, n4 * 128:n4 * 128 + 128], BDF, EN[:, n4 * 128:n4 * 128 + 128])
                        self.act(EX[:, :], pv2[:, :], AF.Sqrt, bias=DER[:, 87:88], scale=1.0 / 64.0)
                        self.recip(EX[:, :], EX[:, :])
                        self.tt("dve", YC[:, :], YC[:, :], EX[:, :], ALU.mult)
                        self.ts("dve", YC[:, :], YC[:, :], FM[:, FM_LNW + j:FM_LNW + j + 1], FM[:, FM_LNB + j:FM_LNB + j + 1], ALU.mult, ALU.add)
                        self.stt(BT32[:, :], SH[0][:, :], FM[:, FM_RK + j:FM_RK + j + 1], KFIN[:, :], ALU.mult, ALU.mult)
                        pb = self.ps()
                        for n4 in range(4):
                            self.mm(pb[:, n4 * 128:n4 * 128 + 128], BDF, BT32[:, n4 * 128:n4 * 128 + 128])
                        self.tt("dve", KT32[:, :], pb[:, :], Vv[:, :], ALU.mult)
                        self.tt("dve", YC[:, :], YC[:, :], KT32[:, :], ALU.add)
                        pg = self.ps()
                        self.dma(SIGGt[:, :], self.sigg_d[:, tb])
                        self.mm(pg[:, :], GUP[:, jc], SIGGt[:, :])
                        self.tt("dve", ob[:, :], YC[:, :], pg[:, :], ALU.mult)
                        self.dma(self.obT[jc, tb], ob[:, :])
        self.dbg_src = getattr(self, "dbg_src", {})
        if "obT" in self.dbg_out:
            self.dbg_src["obT"] = (self.obT, (slice(None), slice(None)))


    def layernorm_tile(self, H, g, b, OUT, SUM, NM, JUNK):
        self.act(JUNK[:, :], H[:, :], AF.Identity, accum=SUM[:, 0:1])
        self.ts("dve", NM[:, 0:1], SUM[:, 0:1], -1.0 / D, None, ALU.mult)
        self.act(H[:, :], H[:, :], AF.Identity, bias=NM[:, 0:1])
        self.act(JUNK[:, :], H[:, :], AF.Square, accum=SUM[:, 1:2])
        self.act(NM[:, 1:2], SUM[:, 1:2], AF.Sqrt, bias=self.DER[:, 86:87], scale=1.0 / D)
        self.recip(NM[:, 1:2], NM[:, 1:2])
        self.stt(OUT[:, :], H[:, :], NM[:, 1:2], g, ALU.mult, ALU.mult)
        self.tt("pool", OUT[:, :], OUT[:, :], b, ALU.add)

    def merge(self, l, src):
        sb = self.sb
        CST, XT = self.CST, self.XT
        es_keep = self.es
        with contextlib.ExitStack() as es1:
            self.es = es1
            WS = [sb(f"m_ws{i}", [128, 8, 256], F32) for i in range(2)]
            PA = sb("m_pa", [128, 8, D], BF16)
            PB = sb("m_pb", [128, 8, D], BF16)
            WG = sb("m_wg", [128, 8, 2 * D], BF16)
            k = 0
            for q in range(4):
                self.load_w(WS[k % 2], PA, self.proj_a, l, q * 256, 256, q * 256, "pool" if k % 2 else "dve"); k += 1
                self.load_w(WS[k % 2], PB, self.proj_b, l, q * 256, 256, q * 256, "pool" if k % 2 else "dve"); k += 1
            for q in range(8):
                self.load_w(WS[k % 2], WG, self.w_in, l, GAB + q * 256, 256, q * 256, "pool" if k % 2 else "dve"); k += 1
            OA = sb("m_oa", [128, 8, 512], BF16)
            OBt = sb("m_ob", [128, 8, 512], BF16)
            SA, SB_, M1t, M2t = [sb("m_" + n, [128, 512], F32) for n in ("sa", "sb", "m1", "m2")]
            MG = [sb(f"m_mg{i}", [128, 512], BF16) for i in range(2)]
            for blk in range(NB):
                t0 = blk * 512
                self.dma(OA[:, :, :], V(self.oaT, self.oaT.h[:, t0:t0 + 512].rearrange("(c p) t -> p c t", p=128)))
                self.dma(OBt[:, :, :], V(self.obT, self.obT.h[:, t0:t0 + 512].rearrange("(c p) t -> p c t", p=128)))
                for j in range(8):
                    js = slice(j * 128, j * 128 + 128)
                    pA = self.ps()
                    for c in range(8):
                        self.mm(pA[:, :], PA[:, c, js], OA[:, c, :], start=(c == 0), stop=(c == 7))
                    pB = self.ps()
                    for c in range(8):
                        self.mm(pB[:, :], PB[:, c, js], OBt[:, c, :], start=(c == 0), stop=(c == 7))
                    pga = self.ps()
                    self.proj_fm(pga[:, :], WG, j * 128, 128, t0, 512)
                    pgb = self.ps()
                    self.proj_fm(pgb[:, :], WG, D + j * 128, 128, t0, 512)
                    self.act(SA[:, :], pga[:, :], AF.Sigmoid)
                    self.act(SB_[:, :], pgb[:, :], AF.Sigmoid)
                    self.tt("dve", M1t[:, :], pA[:, :], SA[:, :], ALU.mult)
                    self.tt("dve", M2t[:, :], pB[:, :], SB_[:, :], ALU.mult)
                    mg = MG[j % 2]
                    self.tt("pool", mg[:, :], M1t[:, :], M2t[:, :], ALU.add)
                    self.dma(self.mgT[js, t0:t0 + 512], mg[:, :])
        self.es = es_keep
        self.P.barrier()
        with contextlib.ExitStack() as es2:
            self.es = es2
            WS = [sb(f"m2_ws{i}", [128, 8, 256], F32) for i in range(2)]
            WO = sb("m2_wo", [128, 8, D], BF16)
            for q in range(4):
                self.load_w(WS[q % 2], WO, self.w_out, l, q * 256, 256, q * 256, "pool" if q % 2 else "dve")
            BC = sb("m2_bc", [128, 2 * D], F32)
            self.dma(BC[:, :], self.bc_pack.h[l][:, 0:2 * D] if False else V(self.bc_pack, self.bc_pack.h[l][:, 0:2 * D]))
            MGt = [sb(f"m2_mg{i}", [128, 8, 128], BF16) for i in range(2)]
            XR = [sb(f"m2_xr{i}", [128, D], F32) for i in range(2)]
            H = sb("m2_h", [128, D], F32)
            JUNK = sb("m2_junk", [128, D], F32)
            X1 = [sb(f"m2_x1{i}", [128, D], F32) for i in range(2)]
            SUM = sb("m2_sum", [128, 2], F32)
            NM = sb("m2_nm", [128, 2], F32)
            for i in range(T // 128):
                ts_ = slice(i * 128, i * 128 + 128)
                mgt, xr, x1 = MGt[i % 2], XR[i % 2], X1[i % 2]
                self.dma(mgt[:, :, :], V(self.mgT, self.mgT.h[:, ts_].rearrange("(c p) t -> p c t", p=128)))
                self.dma(xr[:, :], src[ts_, :])
                for half in range(2):
                    hs = slice(half * 512, half * 512 + 512)
                    ph = self.ps()
                    for c in range(8):
                        self.mm(ph[:, :], mgt[:, c, :], WO[:, c, hs], start=(c == 0), stop=(c == 7))
                    self.stt(H[:, hs], xr[:, hs], DN_ALPHA, ph[:, :], ALU.mult, ALU.add)
                self.layernorm_tile(H, BC[:, 0:D], BC[:, D:2 * D], x1, SUM, NM, JUNK)
                self.dma(self.x1d[ts_, :], x1[:, :])
                for half in range(2):
                    p = self.ps()
                    for c in range(4):
                        cc = half * 4 + c
                        self.mm(p[:, c * 128:(c + 1) * 128], x1[:, cc * 128:(cc + 1) * 128], CST[:, C_ID:C_ID + 128])
                    for c in range(4):
                        cc = half * 4 + c
                        self.cp("act" if c % 2 else "dve", XT[:, cc, ts_], p[:, c * 128:(c + 1) * 128])
        self.es = es_keep
        self.dbg_src = getattr(self, "dbg_src", {})
        if "x1" in self.dbg_out:
            self.dbg_src["x1"] = (self.x1d, (slice(None), slice(None)))

    def moe(self, l, dst):
        sb = self.sb
        CST, XT = self.CST, self.XT
        TP = 1024
        NTB = TP // 512
        NTL = TP // 128
        RWS = sb("e_rws", [128, 8, NE], F32)
        RW = sb("e_rw", [128, 8, NE], BF16)
        self.dma(RWS[:, :, :], V(self.router_w, self.router_w.h[l].rearrange("(c p) n -> p c n", p=128)))
        self.cp("dve", RW[:, :, :], RWS[:, :, :])
        BC = sb("e_bc", [128, 2 * D + NE], F32)
        self.dma(BC[:, :], V(self.bc_pack, self.bc_pack.h[l][:, 2 * D:4 * D + NE]))
        B1 = sb("e_b1", [128, NE * 16], F32)
        self.dma(B1[:, :], self.b1_pack[l])
        B2S = sb("e_b2s", [NE, D], F32)
        B2 = sb("e_b2", [NE, D], BF16)
        self.dma(B2S[:, :], self.moe_b2[l])
        self.cp("dve", B2[:, :], B2S[:, :])
        GT = sb("e_gt", [128, T // 128, NE], F32)
        GTT = sb("e_gtt", [NE, T], BF16)
        LG, EXv, MSK = [sb("e_" + n, [128, NE], F32) for n in ("lg", "ex", "msk")]
        M8 = sb("e_m8", [128, 8], F32)
        SS = sb("e_ss", [128, 2], F32)
        for i in range(T // 128):
            ts_ = slice(i * 128, i * 128 + 128)
            pl = self.ps()
            for c in range(8):
                self.mm(pl[:, 0:NE], XT[:, c, ts_], RW[:, c, :], start=(c == 0), stop=(c == 7))
            self.tt("dve", LG[:, :], pl[:, 0:NE], BC[:, 2 * D:2 * D + NE], ALU.add)
            lg_ap, m8_ap = LG[:, :].ap, M8[:, :].ap
            self.P.op("dve", lambda e, o=m8_ap, i_=lg_ap: e.max(out=o, in_=i_), reads=[LG.b], writes=[M8.b])
            self.ts("dve", SS[:, 0:1], M8[:, 0:1], -1.0, None, ALU.mult)
            self.act(EXv[:, :], LG[:, :], AF.Exp, bias=SS[:, 0:1])
            self.ts("dve", MSK[:, :], LG[:, :], M8[:, 3:4], None, ALU.is_ge)
            self.tt("dve", EXv[:, :], EXv[:, :], MSK[:, :], ALU.mult)
            self.act(MSK[:, :], EXv[:, :], AF.Identity, accum=SS[:, 1:2])
            self.recip(SS[:, 1:2], SS[:, 1:2])
            self.ts("dve", GT[:, i, :], EXv[:, :], SS[:, 1:2], None, ALU.mult)
            pt = self.ps()
            self.mm(pt[0:NE, 0:128], GT[:, i, :], CST[:, C_ID:C_ID + 128])
            self.cp("act", GTT[0:NE, ts_], pt[0:NE, 0:128])
        ACC = sb("e_acc", [128, NTL, D], F32)
        ACTT = sb("e_actt", [128, 8, TP], BF16)
        W1S = [sb("e_w1s0", [128, 8, 256], F32)] * 2
        W1B = [sb(f"e_w1b{i}", [128, 8, 256], BF16) for i in range(2)]
        W2S = [sb(f"e_w2s{i}", [128, 512], F32) for i in range(2)]
        W2B = sb("e_w2b", [128, 8, 512], BF16)
        GLU, SIG, LIN = [sb("e_" + n, [128, 512], F32) for n in ("glu", "sig", "lin")]
        XR = sb("e_xr", [128, D], F32)
        H = sb("e_h", [128, D], F32)
        JUNK = XR
        SUM = sb("e_sum", [128, 2], F32)
        NM = sb("e_nm", [128, 2], F32)
        wk = 0
        for ps_i in range(T // TP):
            tp0 = ps_i * TP
            for tl in range(NTL):
                tsl = slice(tp0 + tl * 128, tp0 + tl * 128 + 128)
                for half in range(2):
                    hs = slice(half * 512, half * 512 + 512)
                    pb = self.ps()
                    self.mm(pb[:, :], GTT[0:NE, tsl], B2[0:NE, hs])
                    self.cp("act", ACC[:, tl, hs], pb[:, :])
            for e in range(NE):
                for ft in range(8):
                    w1s, w1b = W1S[wk % 2], W1B[wk % 2]
                    wk += 1
                    src_ap = self.moe_w1.h[l][e][:, ft * 256:(ft + 1) * 256].rearrange("(c p) n -> p c n", p=128)
                    self.dma(w1s[:, :, :], V(self.moe_w1, src_ap))
                    de = w1s.h[:, :, :].rearrange("p c (f two) -> p c f two", two=2)
                    self.cp("pool", w1b[:, :, 0:128], V(w1s, de[:, :, :, 0]))
                    self.cp("pool" if ft % 2 else "dve", w1b[:, :, 128:256], V(w1s, de[:, :, :, 1]))
                    for tb in range(NTB):
                        t0 = tp0 + tb * 512
                        pg = self.ps()
                        self.proj_fm(pg[:, :], w1b, 0, 128, t0, 512)
                        pl = self.ps()
                        self.proj_fm(pl[:, :], w1b, 128, 128, t0, 512)
                        bg = B1[:, e * 16 + ft:e * 16 + ft + 1]
                        bl = B1[:, e * 16 + 8 + ft:e * 16 + 8 + ft + 1]
                        self.ts("dve", GLU[:, :], pg[:, :], bg, SWIGLU_LIMIT, ALU.add, ALU.min)
                        self.act(SIG[:, :], GLU[:, :], AF.Sigmoid, scale=SWIGLU_ALPHA)
                        self.ts("dve", LIN[:, :], pl[:, :], bl, SWIGLU_LIMIT, ALU.add, ALU.min)
                        self.ts("pool", LIN[:, :], LIN[:, :], -SWIGLU_LIMIT, 1.0, ALU.max, ALU.add)
                        self.tt("pool", GLU[:, :], GLU[:, :], SIG[:, :], ALU.mult)
                        self.tt("dve", ACTT[:, ft, tb * 512:(tb + 1) * 512], GLU[:, :], LIN[:, :], ALU.mult)
                for half in range(2):
                    hs = slice(half * 512, half * 512 + 512)
                    for ft in range(8):
                        w2s = W2S[ft % 2]
                        self.dma(w2s[:, :], V(self.moe_w2, self.moe_w2.h[l][e][ft * 128:(ft + 1) * 128, hs]))
                        self.cp("act" if ft % 2 else "pool", W2B[:, ft, :], w2s[:, :])
                    for tl in range(NTL):
                        gi = (tp0 // 128) + tl
                        py = self.ps()
                        for ft in range(8):
                            self.mm(py[:, :], ACTT[:, ft, tl * 128:(tl + 1) * 128], W2B[:, ft, :], start=(ft == 0), stop=(ft == 7))
                        self.stt(ACC[:, tl, hs], py[:, :], GT[:, gi, e:e + 1], ACC[:, tl, hs], ALU.mult, ALU.add)
            for tl in range(NTL):
                tsl = slice(tp0 + tl * 128, tp0 + tl * 128 + 128)
                self.dma(XR[:, :], self.x1d[tsl, :])
                self.stt(H[:, :], XR[:, :], DN_ALPHA, ACC[:, tl, :], ALU.mult, ALU.add)
                self.layernorm_tile(H, BC[:, 0:D], BC[:, D:2 * D], XR, SUM, NM, JUNK)
                self.out_evs.append(self.dma(dst[tsl, :], XR[:, :]))
        self.dbg_src = getattr(self, "dbg_src", {})
        if "x2" in self.dbg_out:
            self.dbg_src["x2"] = (dst, (slice(None), slice(None)))


def make_consts():
    c = np.zeros((128, NCONST), np.float32)
    c[:, C_ID:C_ID + 128] = np.eye(128, dtype=np.float32)
    i = np.arange(64)
    fi = (i[:, None] <= i[None, :]).astype(np.float32)
    fs = (i[:, None] < i[None, :]).astype(np.float32)
    bi = (i[:, None] >= i[None, :]).astype(np.float32)
    bs = (i[:, None] > i[None, :]).astype(np.float32)
    for base, m in ((C_FI, fi), (C_FS, fs), (C_BI, bi), (C_BS, bs)):
        c[0:64, base:base + 512] = np.tile(m, (1, 8))
    r = np.ones(512, np.float32)
    r[::64] = 0.0
    c[:, C_RESET:C_RESET + 512] = r[None, :]
    bd = np.zeros((128, 128), np.float32)
    bd[0:64, 0:64] = 1.0
    bd[64:128, 64:128] = 1.0
    c[:, C_BD:C_BD + 128] = bd
    c[:, C_ONES:C_ONES + 128] = 1.0
    c[0:64, C_I8:C_I8 + 512] = np.tile(np.eye(64, dtype=np.float32), (1, 8))
    c[0:64, C_SH1 + 64:C_SH1 + 128] = np.eye(64, dtype=np.float32)
    return c


def fmcols(v):
    v = np.asarray(v, np.float32).reshape(-1)
    n = (v.size + 127) // 128
    p = np.zeros(n * 128, np.float32)
    p[:v.size] = v
    return p.reshape(n, 128).T


def make_packs(inp):
    fm = np.zeros((L_ALL, 128, NFM), np.float32)
    bc = np.zeros((L_ALL, 128, 4 * D + NE), np.float32)
    b1 = np.zeros((L_ALL, 128, NE * 16), np.float32)
    for l in range(L_ALL):
        mu = np.asarray(inp["rw_mu"][l], np.float32)
        fm[l, :, FM_MU:FM_MU + 24] = fmcols(mu[0:3072])
        fm[l, :, FM_MU + 24] = mu[3072:3200]
        fm[l, 0:64, FM_MUB] = mu[3136:3200]
        fm[l, 0:64, FM_MU + 25] = mu[3200:3264]
        fm[l, :, FM_MU + 26] = mu[3264:3392]
        fm[l, :, FM_W0:FM_W0 + 16] = fmcols(inp["rw_w0"][l])
        fm[l, :, FM_A0:FM_A0 + 8] = fmcols(inp["rw_a0"][l])
        fm[l, :, FM_KK:FM_KK + 8] = fmcols(inp["rw_k_k"][l])
        fm[l, :, FM_KA:FM_KA + 8] = fmcols(inp["rw_k_a"][l])
        fm[l, :, FM_RK:FM_RK + 8] = fmcols(inp["rw_r_k"][l])
        fm[l, :, FM_LNW:FM_LNW + 8] = fmcols(inp["rw_lnx_w"][l])
        fm[l, :, FM_LNB:FM_LNB + 8] = fmcols(inp["rw_lnx_b"][l])
        if l > 0:
            fm[l, :, FM_V0:FM_V0 + 8] = fmcols(inp["rw_v0"][l - 1])
        for i in range(L_ALL):
            fm[l, :, FM_LBL + 8 * i:FM_LBL + 8 * i + 8] = fmcols(inp["hg_lb_logits"][i])
        fm[l, :, FM_NW] = inp["hg_norm_w"][l]
        bc[l, :, 0:D] = inp["ln1_g"][l][None, :]
        bc[l, :, D:2 * D] = inp["ln1_b"][l][None, :]
        bc[l, :, 2 * D:3 * D] = inp["ln2_g"][l][None, :]
        bc[l, :, 3 * D:4 * D] = inp["ln2_b"][l][None, :]
        bc[l, :, 4 * D:] = inp["router_b"][l][None, :]
        bb = np.asarray(inp["moe_b1"][l], np.float32)
        glu = bb[:, 0::2].reshape(NE, 8, 128)
        lin = bb[:, 1::2].reshape(NE, 8, 128)
        pk = np.concatenate([glu, lin], axis=1)
        b1[l] = pk.transpose(2, 0, 1).reshape(128, NE * 16)
    return fm, bc, b1


_CACHE = {}


def run(inputs, n_layers=L_ALL, stop=None, dbg=(), n_cores=4):
    key = (n_layers, stop, tuple(dbg))
    bld = Builder(n_layers=n_layers, stop=stop, dbg=dbg)
    nc = bld.build()
    inp = {k: np.asarray(v) for k, v in inputs.items()}
    fm, bc, b1 = make_packs(inp)
    shared = {
        "w_in": np.ascontiguousarray(inp["w_in"], np.float32),
        "rw_w_up": np.ascontiguousarray(inp["rw_w_up"], np.float32).reshape(L_ALL, 128, D),
        "rw_a_up": np.ascontiguousarray(inp["rw_a_up"], np.float32),
        "rw_g_up": np.ascontiguousarray(inp["rw_g_up"], np.float32),
        "rw_v_down": np.ascontiguousarray(inp["rw_v_down"], np.float32),
        "rw_v_up": np.ascontiguousarray(inp["rw_v_up"], np.float32),
        "proj_a": np.ascontiguousarray(inp["proj_a"], np.float32),
        "proj_b": np.ascontiguousarray(inp["proj_b"], np.float32),
        "w_out": np.ascontiguousarray(inp["w_out"], np.float32),
        "router_w": np.ascontiguousarray(inp["router_w"], np.float32),
        "fm_pack": fm, "bc_pack": bc, "b1_pack": b1, "consts": make_consts(),
    }
    if bld.need_moe:
        for k in ("moe_w1", "moe_w2", "moe_b2"):
            shared[k] = np.ascontiguousarray(inp[k], np.float32)
    in_maps = []
    for c in range(n_cores):
        m = dict(shared)
        m["x"] = np.ascontiguousarray(inp["x"][c % 4], np.float32)
        in_maps.append(m)
    res = run_bass_kernel_spmd(nc, in_maps, core_ids=list(range(n_cores)))
    return res


def kernel(**inputs):
    res = run(inputs)
    out = np.stack([np.asarray(res.results[c]["y"], np.float32) for c in range(4)], axis=0)
    return out
```
